# Optimizing a Trainium2 kernel written in Bass

```python
import math
import jax, jax.numpy as jnp
from jax import lax
import numpy as np

D_MODEL = 1024
BATCH = 4
SEQ = 4096
DEPTH = 4

CHUNK = 64
Q_BLOCK = 128
ROPE_THETA = 500000.0
EPS = 1e-6
NEG_INF = -1e30
TINY = 1e-30
D_FF = 2816
MLA_HEADS = 4
MLA_NOPE = 128
MLA_ROPE = 64
MLA_V = 128
MLA_Q_RANK = 384
MLA_KV_RANK = 256
HG_HEADS = 4
HG_DK = 128
HG_DV = 128
DF_HEADS = 8
DF_DH = 64
DF_ROT = DF_DH // 4
N_EVEN = (DEPTH + 1) // 2
N_ODD = DEPTH // 2
EVEN_SPLITS = (MLA_Q_RANK, MLA_KV_RANK, MLA_ROPE, HG_HEADS * HG_DK, HG_HEADS * HG_DK, HG_HEADS * HG_DV, HG_HEADS * HG_DV)
EVEN_IN = sum(EVEN_SPLITS)
EVEN_MIX = MLA_HEADS * MLA_V + HG_HEADS * HG_DV
ODD_IN = 3 * DF_HEADS * 2 * DF_DH
ODD_MIX = DF_HEADS * 2 * DF_DH

kernel_name = 'hybrid_mla_hgrn2_diffattn_macaron'


def rms_norm(x, g):
    xf = x.astype(jnp.float32)
    y = xf * lax.rsqrt(jnp.mean(xf * xf, axis=-1, keepdims=True) + EPS)
    return (y * g.astype(jnp.float32)).astype(x.dtype)


def rope_tables(positions, dim):
    inv_freq = ROPE_THETA ** (-jnp.arange(0, dim, 2, dtype=jnp.float32) / dim)
    ang = positions.astype(jnp.float32)[..., None] * inv_freq
    return jnp.cos(ang), jnp.sin(ang)


def apply_rope(x, cos, sin):
    half = cos.shape[-1]
    shape = cos.shape[:2] + (1,) * (x.ndim - 3) + (half,)
    c = cos.reshape(shape)
    s = sin.reshape(shape)
    xf = x.astype(jnp.float32)
    x1, x2 = xf[..., :half], xf[..., half:]
    return jnp.concatenate([x1 * c - x2 * s, x2 * c + x1 * s], axis=-1).astype(x.dtype)


def partial_rope(x, cos, sin):
    rot = 2 * cos.shape[-1]
    return jnp.concatenate([apply_rope(x[..., :rot], cos, sin), x[..., rot:]], axis=-1)


def swiglu(x, w_gate, w_up, w_down):
    return (jax.nn.silu(x @ w_gate) * (x @ w_up)) @ w_down


def block_causal_mask(blk, seq):
    q_idx = blk * Q_BLOCK + jnp.arange(Q_BLOCK)
    k_idx = jnp.arange(seq)
    return (k_idx[None, :] // CHUNK) <= (q_idx[:, None] // CHUNK)


def to_query_blocks(t):
    b, s = t.shape[:2]
    return jnp.moveaxis(t.reshape((b, s // Q_BLOCK, Q_BLOCK) + t.shape[2:]), 1, 0)


def from_query_blocks(o):
    nb, b, qb = o.shape[:3]
    return jnp.moveaxis(o, 0, 1).reshape((b, nb * qb) + o.shape[3:])


def chunk_causal_attention(q, k, v, scale):
    seq = k.shape[1]

    def one_block(args):
        qb, blk = args
        s = jnp.einsum('bqhd,bkhd->bhqk', qb, k).astype(jnp.float32) * scale
        s = jnp.where(block_causal_mask(blk, seq), s, NEG_INF)
        p = jax.nn.softmax(s, axis=-1).astype(v.dtype)
        return jnp.einsum('bhqk,bkhd->bqhd', p, v)

    o = lax.map(one_block, (to_query_blocks(q), jnp.arange(seq // Q_BLOCK)))
    return from_query_blocks(o)


def chunk_causal_diff_attention(q, k, v, lam, scale):
    seq = k.shape[1]

    def one_block(args):
        qb, blk = args
        s = jnp.einsum('bqhcd,bkhcd->bchqk', qb, k).astype(jnp.float32) * scale
        s = jnp.where(block_causal_mask(blk, seq), s, NEG_INF)
        p = jax.nn.softmax(s, axis=-1)
        p = (p[:, 0] - lam * p[:, 1]).astype(v.dtype)
        return jnp.einsum('bhqk,bkhd->bqhd', p, v)

    o = lax.map(one_block, (to_query_blocks(q), jnp.arange(seq // Q_BLOCK)))
    return from_query_blocks(o)


def hgrn2_chunkwise(q, k, logf, v):
    b_, s_, h_, dk = q.shape
    dv = v.shape[-1]
    n = s_ // CHUNK

    def to_chunks(t):
        return jnp.moveaxis(t.reshape(b_, n, CHUNK, h_, t.shape[-1]), (1, 3), (0, 2))

    tril = jnp.tril(jnp.ones((CHUNK, CHUNK), dtype=bool))[:, :, None]

    def step(state, inp):
        qc, kc, gc, vc = inp
        bcum = jnp.cumsum(gc, axis=2)
        o_inter = jnp.einsum('bhtk,bhkv->bhtv', qc * jnp.exp(bcum), state)
        rel = bcum[:, :, :, None, :] - bcum[:, :, None, :, :]
        decay = jnp.where(tril, jnp.exp(jnp.where(tril, rel, 0.0)), 0.0)
        attn = jnp.einsum('bhtk,bhsk,bhtsk->bhts', qc, kc, decay)
        o_intra = jnp.einsum('bhts,bhsv->bhtv', attn, vc)
        b_last = bcum[:, :, -1:, :]
        k_dec = kc * jnp.exp(b_last - bcum)
        new_state = jnp.exp(b_last[:, :, 0, :])[..., None] * state + jnp.einsum('bhsk,bhsv->bhkv', k_dec, vc)
        return new_state, o_inter + o_intra

    init = jnp.zeros((b_, h_, dk, dv), jnp.float32)
    _, o = lax.scan(step, init, (to_chunks(q), to_chunks(k), to_chunks(logf), to_chunks(v)))
    return jnp.moveaxis(o, (0, 2), (1, 3)).reshape(b_, s_, h_, dv)


def even_mixer(h, cos_m, sin_m, w_in, g_q, w_uq, g_kv, w_ukv, lb, g_out, w_out):
    b_, s_, _ = h.shape
    z = h @ w_in
    idx = np.cumsum(EVEN_SPLITS)[:-1].tolist()
    c_q, c_kv, k_pe, hq, hf, hi, hg = jnp.split(z, idx, axis=-1)
    q = (rms_norm(c_q, g_q) @ w_uq).reshape(b_, s_, MLA_HEADS, MLA_NOPE + MLA_ROPE)
    q = jnp.concatenate([q[..., :MLA_NOPE], apply_rope(q[..., MLA_NOPE:], cos_m, sin_m)], axis=-1)
    kv = (rms_norm(c_kv, g_kv) @ w_ukv).reshape(b_, s_, MLA_HEADS, MLA_NOPE + MLA_V)
    k_nope, v = kv[..., :MLA_NOPE], kv[..., MLA_NOPE:]
    k_pe = apply_rope(k_pe.reshape(b_, s_, 1, MLA_ROPE), cos_m, sin_m)
    k = jnp.concatenate([k_nope, jnp.broadcast_to(k_pe, (b_, s_, MLA_HEADS, MLA_ROPE))], axis=-1)
    o_a = chunk_causal_attention(q, k, v, (MLA_NOPE + MLA_ROPE) ** -0.5)
    zf = hf.astype(jnp.float32).reshape(b_, s_, HG_HEADS, HG_DK)
    sig = jax.nn.sigmoid(zf)
    logf = jnp.log(jnp.maximum(lb + (1.0 - lb) * sig, TINY))
    kk = (1.0 - lb) * (1.0 - sig)
    qq = hq.astype(jnp.float32).reshape(b_, s_, HG_HEADS, HG_DK)
    vv = hi.astype(jnp.float32).reshape(b_, s_, HG_HEADS, HG_DV)
    o_b = hgrn2_chunkwise(qq, kk, logf, vv)
    gate = jax.nn.silu(hg.astype(jnp.float32).reshape(b_, s_, HG_HEADS, HG_DV))
    o_b = (rms_norm(o_b, g_out) * gate).astype(h.dtype)
    o = jnp.concatenate([o_a.reshape(b_, s_, -1), o_b.reshape(b_, s_, -1)], axis=-1)
    return o @ w_out


def odd_mixer(h, cos_d, sin_d, w_in, lam_p, g_head, w_out, lambda_init):
    b_, s_, _ = h.shape
    z = h @ w_in
    q, k, v = jnp.split(z, 3, axis=-1)
    q = partial_rope(q.reshape(b_, s_, DF_HEADS, 2, DF_DH), cos_d, sin_d)
    k = partial_rope(k.reshape(b_, s_, DF_HEADS, 2, DF_DH), cos_d, sin_d)
    v = v.reshape(b_, s_, DF_HEADS, 2 * DF_DH)
    lp = lam_p.astype(jnp.float32)
    lam = jnp.exp(jnp.sum(lp[0] * lp[1])) - jnp.exp(jnp.sum(lp[2] * lp[3])) + lambda_init
    o = chunk_causal_diff_attention(q, k, v, lam, DF_DH ** -0.5)
    o = rms_norm(o, g_head) * (1.0 - lambda_init)
    return o.reshape(b_, s_, -1) @ w_out


def setup_inputs(seed: int = 0) -> dict:
    key = jax.random.key(seed)
    ks = jax.random.split(key, 20)

    def nrm(k, shape, fan_in):
        return jax.random.normal(k, shape, jnp.float32) * fan_in ** -0.5

    def gain(k, shape):
        return 1.0 + 0.02 * jax.random.normal(k, shape, jnp.float32)

    x = jax.random.normal(ks[0], (BATCH, SEQ, D_MODEL), jnp.float32)
    offsets = jax.random.randint(ks[1], (BATCH, 1), 0, 64, dtype=jnp.int32) * CHUNK
    positions = (offsets + jnp.arange(SEQ, dtype=jnp.int32)[None, :]).astype(jnp.int32)
    return {
        'x': x,
        'positions': positions,
        'norm_g': gain(ks[2], (DEPTH, 3, 2, D_MODEL)),
        'ffn_w_gate': nrm(ks[3], (DEPTH, 2, D_MODEL, D_FF), D_MODEL),
        'ffn_w_up': nrm(ks[4], (DEPTH, 2, D_MODEL, D_FF), D_MODEL),
        'ffn_w_down': nrm(ks[5], (DEPTH, 2, D_FF, D_MODEL), D_FF),
        'ev_w_in': nrm(ks[6], (N_EVEN, D_MODEL, EVEN_IN), D_MODEL),
        'ev_g_q': gain(ks[7], (N_EVEN, MLA_Q_RANK)),
        'ev_w_uq': nrm(ks[8], (N_EVEN, MLA_Q_RANK, MLA_HEADS * (MLA_NOPE + MLA_ROPE)), MLA_Q_RANK),
        'ev_g_kv': gain(ks[9], (N_EVEN, MLA_KV_RANK)),
        'ev_w_ukv': nrm(ks[10], (N_EVEN, MLA_KV_RANK, MLA_HEADS * (MLA_NOPE + MLA_V)), MLA_KV_RANK),
        'ev_lb_logits': jax.random.normal(ks[11], (N_EVEN, HG_HEADS * HG_DK), jnp.float32),
        'ev_g_out': gain(ks[12], (N_EVEN, HG_HEADS, HG_DV)),
        'ev_w_out': nrm(ks[13], (N_EVEN, EVEN_MIX, D_MODEL), EVEN_MIX),
        'od_w_in': nrm(ks[14], (N_ODD, D_MODEL, ODD_IN), D_MODEL),
        'od_lambda': 0.1 * jax.random.normal(ks[15], (N_ODD, 4, DF_DH), jnp.float32),
        'od_g_head': gain(ks[16], (N_ODD, DF_HEADS, 2 * DF_DH)),
        'od_w_out': nrm(ks[17], (N_ODD, ODD_MIX, D_MODEL), ODD_MIX),
    }


def reference(x, positions, norm_g, ffn_w_gate, ffn_w_up, ffn_w_down, ev_w_in, ev_g_q, ev_w_uq, ev_g_kv, ev_w_ukv, ev_lb_logits, ev_g_out, ev_w_out, od_w_in, od_lambda, od_g_head, od_w_out):
    cos_m, sin_m = rope_tables(positions, MLA_ROPE)
    cos_d, sin_d = rope_tables(positions, DF_ROT)
    lb_w = jax.nn.softmax(ev_lb_logits.astype(jnp.float32), axis=0)
    lb_all = jnp.cumsum(lb_w, axis=0) - lb_w[0:1]
    for l in range(DEPTH):
        g = norm_g[l]
        h = swiglu(rms_norm(x, g[0, 0]), ffn_w_gate[l, 0], ffn_w_up[l, 0], ffn_w_down[l, 0])
        x = x + 0.5 * rms_norm(h, g[0, 1])
        h = rms_norm(x, g[1, 0])
        if l % 2 == 0:
            j = l // 2
            lb = lb_all[j].reshape(HG_HEADS, HG_DK)
            m = even_mixer(h, cos_m, sin_m, ev_w_in[j], ev_g_q[j], ev_w_uq[j], ev_g_kv[j], ev_w_ukv[j], lb, ev_g_out[j], ev_w_out[j])
        else:
            j = l // 2
            lambda_init = 0.8 - 0.6 * math.exp(-0.3 * l)
            m = odd_mixer(h, cos_d, sin_d, od_w_in[j], od_lambda[j], od_g_head[j], od_w_out[j], lambda_init)
        x = x + rms_norm(m, g[1, 1])
        h = swiglu(rms_norm(x, g[2, 0]), ffn_w_gate[l, 1], ffn_w_up[l, 1], ffn_w_down[l, 1])
        x = x + 0.5 * rms_norm(h, g[2, 1])
    return x
```

```python
import math
import numpy as np
import ml_dtypes
import concourse.bass as bass
import concourse.mybir as mybir
from concourse.bass_utils import run_bass_kernel_spmd

F32 = mybir.dt.float32
BF16 = mybir.dt.bfloat16
I32 = mybir.dt.int32
AF = mybir.ActivationFunctionType
ALU = mybir.AluOpType
AX = mybir.AxisListType

D_MODEL = 1024
BATCH = 4
SEQ = 4096
DEPTH = 4
CHUNK = 64
ROPE_THETA = 500000.0
EPS = 1e-6
TINY = 1e-30
D_FF = 2816
NJ = D_FF // 128
MLA_NOPE, MLA_ROPE, MLA_V, MLA_Q_RANK, MLA_KV_RANK = 128, 64, 128, 384, 256
DF_DH = 64
DF_ROT = 16
NCORES = 8
TOK = SEQ // 2
NTT = TOK // 128


class View:
    __slots__ = ("tile", "ap")

    def __init__(self, tile, ap):
        self.tile = tile
        self.ap = ap

    def __getitem__(self, idx):
        return View(self.tile, self.ap[idx])

    def rearrange(self, s, **kw):
        return View(self.tile, self.ap.rearrange(s, **kw))

    def bitcast(self, dt):
        return View(self.tile, self.ap.bitcast(dt))


class Tile:
    def __init__(self, name, base_ap):
        self.name = name
        self.base = base_ap
        self.last_w = None
        self.readers = []

    def __getitem__(self, idx):
        return View(self, self.base[idx])

    def v(self):
        return View(self, self.base)


class Op:
    __slots__ = ("eng", "fn", "reads", "writes", "dma", "deps", "signal", "seq",
                 "sem", "val", "prev_dma")

    def __init__(self, eng, fn, reads, writes, dma):
        self.eng = eng
        self.fn = fn
        self.reads = reads
        self.writes = writes
        self.dma = dma
        self.deps = []
        self.signal = False
        self.seq = 0
        self.sem = None
        self.val = 0
        self.prev_dma = None


class Prog:
    ENGS = ("pe", "act", "dve", "pool", "sp")
    NDMASEM = 12

    def __init__(self, nc):
        self.nc = nc
        self.ops = []
        self.n_sb = 0
        self.stack = None
        self.uid = 0

    def open_scope(self):
        import contextlib
        self.stack = contextlib.ExitStack()

    def close_scope(self):
        self.stack.close()
        self.stack = None
        self.ops.append("BARRIER")

    def sb(self, name, shape, dtype):
        if self.stack is not None:
            self.uid += 1
            h = self.stack.enter_context(self.nc.sbuf_tensor("sb%d_%s" % (self.uid, name), list(shape), dtype))
        else:
            h = self.nc.alloc_sbuf_tensor("sb_" + name, list(shape), dtype)
        idx = tuple(slice(None) for _ in shape)
        return Tile(name, h[idx])

    def ps(self, name, shape, dtype=F32):
        h = self.nc.alloc_psum_tensor("ps_" + name, list(shape), dtype)
        idx = tuple(slice(None) for _ in shape)
        return Tile(name, h[idx])

    def dram(self, name, shape, dtype, kind):
        h = self.nc.dram_tensor(name, list(shape), dtype, kind=kind)
        return Tile(name, h.ap())

    def add(self, eng, fn, reads, writes, dma=False):
        rt = []
        for r in reads:
            if isinstance(r, View) and r.tile not in rt:
                rt.append(r.tile)
        wt = []
        for w in writes:
            if isinstance(w, View) and w.tile not in wt:
                wt.append(w.tile)
        op = Op(eng, fn, rt, wt, dma)
        self.ops.append(op)
        return op

    def dma(self, q, out, in_):
        return self.add(q, lambda e: e.dma_start(out=out.ap, in_=in_.ap), [in_], [out], dma=True)

    def matmul(self, out, lhsT, rhs, start, stop):
        return self.add("pe", lambda e: e.matmul(out.ap, lhsT.ap, rhs.ap, start=start, stop=stop),
                        [lhsT, rhs], [out])

    def transpose(self, out, in_, ident):
        return self.add("pe", lambda e: e.transpose(out.ap, in_.ap, ident.ap), [in_, ident], [out])

    def act(self, out, in_, func, bias=None, scale=None, accum=None, eng="act"):
        reads = [in_]
        writes = [out]
        kw = {}
        if bias is not None:
            if isinstance(bias, View):
                reads.append(bias)
                kw["bias"] = bias.ap
            else:
                kw["bias"] = float(bias)
        if scale is not None:
            if isinstance(scale, View):
                reads.append(scale)
                kw["scale"] = scale.ap
            else:
                kw["scale"] = float(scale)
        if accum is not None:
            writes.append(accum)
            kw["accum_out"] = accum.ap
        return self.add(eng, lambda e: e.activation(out.ap, in_.ap, func, **kw), reads, writes)

    def tt(self, eng, out, in0, in1, op):
        return self.add(eng, lambda e: e.tensor_tensor(out.ap, in0.ap, in1.ap, op), [in0, in1], [out])

    def ts(self, eng, out, in0, s1, op0, s2=None, op1=None, accum=None):
        reads = [in0]
        a1 = s1.ap if isinstance(s1, View) else float(s1)
        if isinstance(s1, View):
            reads.append(s1)
        a2 = None
        if s2 is not None:
            a2 = s2.ap if isinstance(s2, View) else float(s2)
            if isinstance(s2, View):
                reads.append(s2)
        writes = [out]
        kw = {}
        if accum is not None:
            writes.append(accum)
            kw["accum_out"] = accum.ap
        o1 = op1 if op1 is not None else ALU.bypass
        return self.add(eng, lambda e: e.tensor_scalar(out.ap, in0.ap, a1, a2, op0, o1, **kw), reads, writes)

    def stt(self, out, in0, scalar, in1, op0, op1, accum=None):
        reads = [in0, in1]
        a = scalar.ap if isinstance(scalar, View) else float(scalar)
        if isinstance(scalar, View):
            reads.append(scalar)
        writes = [out]
        kw = {}
        if accum is not None:
            writes.append(accum)
            kw["accum_out"] = accum.ap
        return self.add("dve", lambda e: e.scalar_tensor_tensor(out.ap, in0.ap, a, in1.ap, op0, op1, **kw),
                        reads, writes)

    def copy(self, eng, out, in_):
        if eng == "act":
            return self.add("act", lambda e: e.copy(out.ap, in_.ap), [in_], [out])
        return self.add(eng, lambda e: e.tensor_copy(out.ap, in_.ap), [in_], [out])

    def memset(self, eng, out, val):
        return self.add(eng, lambda e: e.memset(out.ap, val), [], [out])

    def recip(self, out, in_):
        return self.add("dve", lambda e: e.reciprocal(out.ap, in_.ap), [in_], [out])

    def finalize(self):
        nc = self.nc
        ops = self.ops
        real = []
        fence = []
        last_eng = {}
        dma_q = {}
        for op in ops:
            if isinstance(op, str):
                fence = [o for o in last_eng.values()]
                for q, lst in dma_q.items():
                    fence.extend(lst[-self.NDMASEM:])
                continue
            real.append(op)
            if op.dma:
                dma_q.setdefault(op.eng, []).append(op)
            else:
                last_eng[op.eng] = op
            op.deps.extend(fence)
        ops = real
        self.ops = real
        for op in ops:
            deps = list(op.deps)
            op.deps = []
            raw = set()
            for t in op.reads:
                if t.last_w is not None:
                    deps.append(t.last_w)
                    raw.add(id(t.last_w))
            for t in op.writes:
                if t.last_w is not None:
                    deps.append(t.last_w)
                deps.extend(t.readers)
            seen = set()
            for d in deps:
                if d is op or id(d) in seen:
                    continue
                seen.add(id(d))
                if (not d.dma) and d.eng == op.eng and op.eng == "pe":
                    continue
                op.deps.append(d)
            for t in op.reads:
                t.readers.append(op)
            for t in op.writes:
                t.last_w = op
                t.readers = []
        for op in ops:
            for d in op.deps:
                if not d.dma:
                    d.signal = True
        cnt = {e: 0 for e in self.ENGS}
        dcnt = {e: 0 for e in self.ENGS}
        dma_hist = {e: [] for e in self.ENGS}
        esem = {}
        dsem = {}
        for e in self.ENGS:
            esem[e] = nc.alloc_semaphore("s_" + e)
        for op in ops:
            if op.dma:
                q = op.eng
                if q not in dsem:
                    dsem[q] = [nc.alloc_semaphore("d_%s_%d" % (q, i)) for i in range(self.NDMASEM)]
                i = dcnt[q]
                dcnt[q] += 1
                op.sem = dsem[q][i % self.NDMASEM]
                op.val = 16 * (i // self.NDMASEM + 1)
                if i >= self.NDMASEM:
                    op.prev_dma = dma_hist[q][i - self.NDMASEM]
                dma_hist[q].append(op)
            elif op.signal:
                cnt[op.eng] += 1
                op.seq = cnt[op.eng]
                op.sem = esem[op.eng]
                op.val = op.seq
        per_eng = {e: [] for e in self.ENGS}
        for op in ops:
            per_eng[op.eng].append(op)
        last_dma = {q: h for q, h in dma_hist.items() if h}

        def emit(eng_name, e):
            waited = {}
            for op in per_eng[eng_name]:
                need = {}
                dl = list(op.deps)
                if op.prev_dma is not None:
                    dl.append(op.prev_dma)
                for d in dl:
                    k = d.sem.num
                    if waited.get(k, 0) >= d.val:
                        continue
                    if k not in need or need[k][1] < d.val:
                        need[k] = (d.sem, d.val)
                for k, (s, v) in need.items():
                    e.wait_ge(s, v)
                    waited[k] = v
                ins = op.fn(e)
                if op.dma:
                    ins.then_inc(op.sem, 16)
                elif op.signal:
                    ins.then_inc(op.sem, 1)
            if eng_name in last_dma:
                fin = {}
                for d in last_dma[eng_name]:
                    k = d.sem.num
                    if k not in fin or fin[k][1] < d.val:
                        fin[k] = (d.sem, d.val)
                for k, (s, v) in fin.items():
                    if waited.get(k, 0) < v:
                        e.wait_ge(s, v)

        with nc.Block() as block:
            @block.tensor
            def _(e):
                emit("pe", e)

            @block.scalar
            def _(e):
                emit("act", e)

            @block.vector
            def _(e):
                emit("dve", e)

            @block.gpsimd
            def _(e):
                emit("pool", e)

            @block.sync
            def _(e):
                emit("sp", e)


class Ctx:
    pass


def rms_stats(P, cx, src, d, rstd):
    P.act(cx.junk[:, 0:d], src, AF.Square, accum=cx.ss[:, 0:1])
    P.act(cx.ss[:, 1:2], cx.ss[:, 0:1], AF.Sqrt, bias=cx.epsb[:, 0:1], scale=1.0 / d)
    P.recip(rstd, cx.ss[:, 1:2])


STAGE = 9


def ffn_group(P, cx, xt, W, gpre, gpost, name):
    nt = len(xt)
    nh = nt // 4
    for t in range(nt):
        rms_stats(P, cx, xt[t][:, :], D_MODEL, cx.rstd[:, 0:1])
        P.stt(cx.hn[:, :], xt[t][:, :], cx.rstd[:, 0:1], gpre[:, :], ALU.mult, ALU.mult)
        pt = cx.ptr[t % 2]
        for kc in range(8):
            P.transpose(pt[:, kc * 128:(kc + 1) * 128], cx.hn[:, kc * 128:(kc + 1) * 128], cx.ident[:, :])
        h = t // 4
        P.copy("act", cx.hT[h][:, :, (t % 4) * 128:(t % 4 + 1) * 128],
               pt.rearrange("p (k n) -> p k n", k=8))
    if STAGE < 2:
        return
    for j in range(NJ):
        wg = cx.wg[j % len(cx.wg)]
        wu = cx.wu[j % len(cx.wu)]
        P.dma("pool", wg[:, :, :], W["wg"][j])
        P.dma("pool", wu[:, :, :], W["wu"][j])
        for h in range(nh):
            pg = cx.pg[(j * nh + h) % 2]
            pu = cx.pu[(j * nh + h) % 2]
            for kc in range(8):
                P.matmul(pg[:, :], wg[:, kc, :], cx.hT[h][:, kc, :], kc == 0, kc == 7)
            for kc in range(8):
                P.matmul(pu[:, :], wu[:, kc, :], cx.hT[h][:, kc, :], kc == 0, kc == 7)
            sg = cx.sg[(j * nh + h) % 2]
            P.act(sg[:, :], pg[:, :], AF.Silu)
            P.tt("dve", cx.aT[j][:, h * 512:(h + 1) * 512], sg[:, :], pu[:, :], ALU.mult)
    if STAGE < 3:
        return
    for t in range(nt):
        y = cx.y[t % 2]
        for n in range(2):
            pd = cx.pd[(t * 2 + n) % 2]
            for j in range(NJ):
                P.matmul(pd[:, :], cx.aT[j][:, t * 128:(t + 1) * 128],
                         W["wd"][j][:, n * 512:(n + 1) * 512], j == 0, j == NJ - 1)
            P.copy("act", y[:, n * 512:(n + 1) * 512], pd[:, :])
        norm_residual(P, cx, xt[t], y, gpost, 0.5)


def norm_residual(P, cx, x, y, g, alpha):
    rms_stats(P, cx, y[:, :], D_MODEL, cx.rstd[:, 1:2])
    P.stt(y[:, :], y[:, :], cx.rstd[:, 1:2], g[:, :], ALU.mult, ALU.mult)
    P.stt(x[:, :], y[:, :], float(alpha), x[:, :], ALU.mult, ALU.add)


def load_wd(P, cx, wd_dram):
    for j in range(NJ):
        P.dma("pool", cx.wd[j][:, :], wd_dram[j])


NGRP = SEQ // 1024


def emit_token_phase(P, B, G, step):
    do_post = step > 0
    do_pre = step < DEPTH
    P.open_scope()
    cx = Ctx()
    cx.ident = P.sb("ident_sb", [128, 128], BF16)
    cx.junk = P.sb("junk", [128, D_MODEL], BF16)
    cx.ss = P.sb("ss", [128, 2], F32)
    cx.rstd = P.sb("rstd", [128, 2], F32)
    cx.epsb = P.sb("epsb", [128, 1], F32)
    cx.hn = P.sb("hn", [128, D_MODEL], BF16)
    cx.hT = [P.sb("hT%d" % i, [128, 8, 512], BF16) for i in range(2)]
    cx.wg = [P.sb("wg%d" % i, [128, 8, 128], BF16) for i in range(3)]
    cx.wu = [P.sb("wu%d" % i, [128, 8, 128], BF16) for i in range(3)]
    cx.wd = [P.sb("wd%d" % j, [128, D_MODEL], BF16) for j in range(NJ)]
    cx.aT = [P.sb("aT%d" % j, [128, 1024], BF16) for j in range(NJ)]
    cx.sg = [P.sb("sg%d" % i, [128, 512], F32) for i in range(2)]
    cx.y = [P.sb("y%d" % i, [128, D_MODEL], F32) for i in range(2)]
    gt = [P.sb("g%d" % i, [128, D_MODEL], F32) for i in range(6)]
    xt = [P.sb("x%d" % i, [128, D_MODEL], F32) for i in range(8)]
    if do_post:
        wout = [P.sb("wout%d" % k, [128, D_MODEL], BF16) for k in range(8)]
    if do_pre:
        hTo = P.sb("hTo", [128, 8, 128], BF16)
    cx.pg = [B[0], B[1]]
    cx.pu = [B[2], B[3]]
    cx.pd = [B[4], B[5]]
    cx.ptr = [B[6][:, :].bitcast(BF16), B[7][:, :].bitcast(BF16)]

    P.dma("sp", cx.ident[:, :], G["ident"][:, :])
    P.memset("dve", cx.epsb[:, :], EPS)
    Wpost = Wpre = None
    if do_post:
        l = step - 1
        for i in range(3):
            P.dma("sp", gt[i][:, :], G["gains"][l * 6 + 3 + i])
        for k in range(8):
            P.dma("pool", wout[k][:, :], G["wout%d" % l][k])
        Wpost = {"wg": G["wg%d1" % l], "wu": G["wu%d1" % l], "wd": cx.wd}
    if do_pre:
        l = step
        for i in range(3):
            P.dma("sp", gt[3 + i][:, :], G["gains"][l * 6 + i])
        Wpre = {"wg": G["wg%d0" % l], "wu": G["wu%d0" % l], "wd": cx.wd}

    for grp in range(NGRP):
        for t in range(8):
            src = G["x"][grp * 8 + t] if step == 0 else G["xs%d" % grp][t]
            P.dma("sp", xt[t][:, :], src)
        if do_post:
            l = step - 1
            for h2 in range(2):
                P.dma("sp", cx.hT[h2][:, :, :],
                      G["oT_s"][:, :, grp * 1024 + h2 * 512:grp * 1024 + (h2 + 1) * 512].rearrange("k p n -> p k n"))
            for t in range(8):
                y = cx.y[t % 2]
                for n in range(2):
                    pd = cx.pd[(t * 2 + n) % 2]
                    for k in range(8):
                        P.matmul(pd[:, :], cx.hT[t // 4][:, k, (t % 4) * 128:(t % 4 + 1) * 128],
                                 wout[k][:, n * 512:(n + 1) * 512], k == 0, k == 7)
                    P.copy("act", y[:, n * 512:(n + 1) * 512], pd[:, :])
                norm_residual(P, cx, xt[t], y, gt[0], 1.0)
            load_wd(P, cx, G["wd%d1" % l])
            ffn_group(P, cx, xt, Wpost, gt[1], gt[2], "f2")
        if do_pre:
            l = step
            load_wd(P, cx, G["wd%d0" % l])
            ffn_group(P, cx, xt, Wpre, gt[3], gt[4], "f1")
            for t in range(8):
                rms_stats(P, cx, xt[t][:, :], D_MODEL, cx.rstd[:, 0:1])
                P.stt(cx.hn[:, :], xt[t][:, :], cx.rstd[:, 0:1], gt[5][:, :], ALU.mult, ALU.mult)
                pt = cx.ptr[t % 2]
                for kc in range(8):
                    P.transpose(pt[:, kc * 128:(kc + 1) * 128], cx.hn[:, kc * 128:(kc + 1) * 128], cx.ident[:, :])
                P.copy("act", hTo[:, :, :], pt.rearrange("p (k n) -> p k n", k=8))
                tok0 = grp * 1024 + t * 128
                P.dma("sp", G["hT_s"][:, :, tok0:tok0 + 128].rearrange("k p n -> p k n"), hTo[:, :, :])
        for t in range(8):
            dst = G["xo"][grp * 8 + t] if step == DEPTH else G["xs%d" % grp][t]
            P.dma("sp", dst, xt[t][:, :])
    P.close_scope()


def mm(P, out, lhsT, rhs, start, stop, skip=False):
    if skip:
        return P.add("pe", lambda e: e.matmul(out.ap, lhsT.ap, rhs.ap, start=start, stop=stop,
                                              skip_group_check=True), [lhsT, rhs], [out])
    return P.matmul(out, lhsT, rhs, start, stop)


def build_rope_tables(P, cx, pos_d, freq_col, sgn_col, nrows, Ct, St):
    PI = math.pi
    PIS = 3.1415925
    n = nrows
    for b in range(8):
        sl = slice(b * 512, (b + 1) * 512)
        P.dma("sp", cx.posi[0:n, :], pos_d[0:n, sl])
        P.copy("dve", cx.posf[0:n, :], cx.posi[0:n, :])
        P.ts("dve", cx.ang[0:n, :], cx.posf[0:n, :], freq_col, ALU.mult)
        for shift, dst, sg in ((0.0, St, sgn_col), (0.5 * PI, Ct, None)):
            if shift != 0.0:
                P.ts("dve", cx.ang[0:n, :], cx.ang[0:n, :], shift, ALU.add)
            P.ts("dve", cx.rtmp[0:n, :], cx.ang[0:n, :], 1.0 / (2 * PI), ALU.mult)
            P.copy("dve", cx.ki[0:n, :], cx.rtmp[0:n, :])
            P.copy("dve", cx.rtmp[0:n, :], cx.ki[0:n, :])
            P.stt(cx.rtmp[0:n, :], cx.rtmp[0:n, :], -2 * PI, cx.ang[0:n, :], ALU.mult, ALU.add)
            P.ts("dve", cx.posf[0:n, :], cx.rtmp[0:n, :], PI, ALU.is_gt, 2 * PI, ALU.mult)
            P.tt("dve", cx.rtmp[0:n, :], cx.rtmp[0:n, :], cx.posf[0:n, :], ALU.subtract)
            P.ts("dve", cx.rtmp[0:n, :], cx.rtmp[0:n, :], PIS, ALU.min, -PIS, ALU.max)
            if sg is not None:
                P.act(cx.rtmp[0:n, :], cx.rtmp[0:n, :], AF.Sin)
                P.ts("dve", dst[0:n, sl], cx.rtmp[0:n, :], sg, ALU.mult)
            else:
                P.act(dst[0:n, sl], cx.rtmp[0:n, :], AF.Sin)


def attention(P, cx, qk_pairs, V, scale, out_cb, ncomp=1):
    its = []
    for g in range(8):
        nk = 4 * (g + 1)
        for kt in range(nk):
            for c in range(ncomp):
                its.append((g, kt, c, nk))
    nb = len(cx.pst)

    def front(i):
        g, kt, c, nk = its[i]
        a = kt - 4 * g
        q0 = 128 * a if a > 0 else 0
        st = cx.pst[i % nb]
        pT = cx.pT[i % nb]
        prs = qk_pairs[c]
        for n_, (kT, qT) in enumerate(prs):
            P.matmul(st[:, q0:512], kT[:, kt * 128:(kt + 1) * 128],
                     qT[:, g * 512 + q0:(g + 1) * 512], n_ == 0, n_ == len(prs) - 1)
        P.act(pT[:, q0:512], st[:, q0:512], AF.Exp, scale=scale)
        if a >= 0:
            P.memset("dve", pT[64:128, q0:q0 + 64], 0.0)

    def back(i):
        g, kt, c, nk = its[i]
        a = kt - 4 * g
        q0 = 128 * a if a > 0 else 0
        pT = cx.pT[i % nb]
        mm(P, cx.pso[c][:, q0:512], V[:, kt, :], pT[:, q0:512], kt == 0, kt == nk - 1, skip=True)
        mm(P, cx.psd[c][:, q0:512], cx.ones[:, :], pT[:, q0:512], kt == 0, kt == nk - 1, skip=True)
        if kt == nk - 1 and c == ncomp - 1:
            out_cb(g)

    SK = nb - 1
    n = len(its)
    for i in range(n + SK):
        if i < n:
            front(i)
        if i - SK >= 0:
            back(i - SK)


def colnorm(P, cx, raws, sqs, ps_ss, d, rstd_out):
    for i, sq in enumerate(sqs):
        P.matmul(ps_ss, cx.ones[:, :], sq, i == 0, i == len(sqs) - 1)
    P.act(rstd_out, ps_ss, AF.Sqrt, bias=cx.epsb[:, 0:1], scale=1.0 / d)
    P.recip(rstd_out, rstd_out)


def emit_even_mixer(P, B, G, j):
    P.open_scope()
    cx = Ctx()
    hT_d = G["hT_s"]
    oT_d = G["oT_s"]
    pos_d = G["pos"]
    cols_d = G["colsE%d" % j]
    ones_d, identf_d, mask_d, rmask_d = G["ones"], G["identf"], G["mask"], G["rmask"]
    wlat_d, whg_d, wuq_d, wukv_d = G["wlat%d" % j], G["whg%d" % j], G["wuq%d" % j], G["wukv%d" % j]

    hT = P.sb("hT_sb", [128, 8, SEQ], BF16)
    colsr = [P.sb("cols_sb%d" % r, [128, 16], F32) for r in range(2)]
    cols = colsr[0]
    cx.ones = P.sb("ones_sb", [128, 128], BF16)
    identf = P.sb("identf_sb", [128, 128], F32)
    mask = P.sb("mask_sb", [64, 512], F32)
    rmask = P.sb("rmask_sb", [128, 512], F32)
    wlat = P.sb("wlat_sb", [128, 8, 768], BF16)
    whg = P.sb("whg_sb", [128, 8, 1024], BF16)
    wuq = P.sb("wuq_sb", [128, 3, 512], BF16)
    wukv = P.sb("wukv_sb", [128, 2, 512], BF16)
    cx.epsb = P.sb("epsb", [128, 1], F32)
    cx.ki = P.sb("ki", [128, 512], I32)
    cx.posi = P.sb("posi", [128, 512], I32)
    sig = P.sb("sig", [128, 512], F32)
    ff = P.sb("ff", [128, 512], F32)
    kk = P.sb("kk", [128, 512], F32)
    bcum = P.sb("bcum", [128, 512], F32)
    eb = P.sb("eb", [128, 512], F32)
    enb = P.sb("enb", [128, 512], F32)
    kdT = P.sb("kdT", [128, 512], F32)
    gate = P.sb("gate", [128, 512], F32)
    cx.posf = sig
    cx.ang = ff
    cx.rtmp = kk
    Ct = P.sb("Ct", [64, SEQ], BF16)
    St = P.sb("St", [64, SEQ], BF16)
    cqn = P.sb("cqn", [128, 3, SEQ], BF16)
    ckvn = P.sb("ckvn", [128, 2, SEQ], BF16)
    kpeT = P.sb("kpeT", [64, SEQ], BF16)
    raw = [bcum, eb, enb, kdT, gate]
    sq = [P.sb("sq%d" % i, [128, 512], BF16) for i in range(5)]
    rstdA = P.sb("rstdA", [128, 512], F32)
    rstdB = ff
    t1 = kk
    t2 = sig
    qnT = hT[:, 0, :]
    knT = hT[:, 1, :]
    qrT = hT[0:64, 2, :]
    V = hT[:, 3, :].rearrange("p (t n) -> p t n", t=32)
    cx.pT = [P.sb("pT%d" % i, [128, 512], BF16) for i in range(3)]
    osb = [P.sb("osb%d" % i, [128, 512], BF16) for i in range(2)]

    for r in range(2):
        P.dma("sp", colsr[r][:, :], cols_d[r])
    P.dma("sp", cx.ones[:, :], ones_d[:, :])
    P.dma("sp", identf[:, :], identf_d[:, :])
    P.dma("sp", mask[:, :], mask_d[:, :])
    P.dma("sp", rmask[:, :], rmask_d[:, :])
    P.memset("dve", cx.epsb[:, :], EPS)
    P.dma("pool", wlat[:, :, :], wlat_d[:, :, :].rearrange("k p n -> p k n"))
    for k in range(8):
        P.dma("sp", hT[:, k, :], hT_d[k])
    build_rope_tables(P, cx, pos_d, cols[0:64, 5:6], cols[0:64, 6:7], 64, Ct, St)

    for b in range(8):
        sl = slice(b * 512, (b + 1) * 512)
        for oc in range(5):
            pb = B[oc % 4]
            for k in range(8):
                P.matmul(pb[:, :], wlat[:, k, oc * 128:(oc + 1) * 128], hT[:, k, sl], k == 0, k == 7)
            P.copy("act", raw[oc][:, :], pb[:, :])
            P.act(sq[oc][:, :], pb[:, :], AF.Square)
        colnorm(P, cx, raw[0:3], [s[:, :] for s in sq[0:3]], B[4][:, :], MLA_Q_RANK, rstdA[:, :])
        colnorm(P, cx, raw[3:5], [s[:, :] for s in sq[3:5]], B[5][:, :], MLA_KV_RANK, rstdB[:, :])
        for c in range(3):
            P.stt(cqn[:, c, sl], raw[c][:, :], cols[:, c:c + 1], rstdA[:, :], ALU.mult, ALU.mult)
        for c in range(2):
            P.stt(ckvn[:, c, sl], raw[3 + c][:, :], cols[:, 3 + c:4 + c], rstdB[:, :], ALU.mult, ALU.mult)
        for k in range(8):
            P.matmul(B[6][0:64, :], wlat[:, k, 640:704], hT[:, k, sl], k == 0, k == 7)
        for k in range(8):
            P.matmul(B[7][0:64, :], wlat[:, k, 704:768], hT[:, k, sl], k == 0, k == 7)
        P.tt("dve", t1[0:64, :], B[6][0:64, :], Ct[:, sl], ALU.mult)
        P.tt("dve", t2[0:64, :], B[7][0:64, :], St[:, sl], ALU.mult)
        P.tt("dve", kpeT[:, sl], t1[0:64, :], t2[0:64, :], ALU.add)

    lbc = P.sb("lbc", [128, 4], F32)
    state = P.sb("state", [128, 128], F32)
    state_bf = P.sb("state_bf", [128, 128], BF16)
    qd = P.sb("qd", [128, 512], BF16)
    ktl = P.sb("ktl", [128, 512], BF16)
    kdec = P.sb("kdec", [64, 8, 128], BF16)
    V64 = P.sb("V64", [64, 8, 128], BF16)
    attn = P.sb("attn", [64, 512], BF16)
    oraw = bcum
    osq = P.sb("osq", [128, 512], BF16)
    for r in range(2):
        P.dma("pool", whg[:, :, :], whg_d[r].rearrange("k p n -> p k n"))
        for h in range(2):
            wofs = h * 512
            if j == 0:
                P.memset("dve", lbc[:, 0:1], 0.0)
            else:
                P.tt("dve", lbc[:, 3:4], colsr[r][:, 9 + h:10 + h], colsr[r][:, 7 + h:8 + h], ALU.subtract)
                P.act(lbc[:, 0:1], lbc[:, 3:4], AF.Sigmoid)
            P.ts("dve", lbc[:, 1:2], lbc[:, 0:1], -1.0, ALU.mult, 1.0, ALU.add)
            P.ts("dve", lbc[:, 2:3], lbc[:, 1:2], -1.0, ALU.mult)
            P.memset("dve", state[:, :], 0.0)
            P.memset("dve", state_bf[:, :], 0.0)
            for b in range(8):
                sl = slice(b * 512, (b + 1) * 512)
                for i, pb in enumerate((B[0], B[1], B[2])):
                    for k in range(8):
                        P.matmul(pb[:, :], whg[:, k, wofs + i * 128:wofs + (i + 1) * 128], hT[:, k, sl], k == 0, k == 7)
                for c in range(8):
                    pb = B[3 + c // 4]
                    tok = slice(b * 512 + c * 64, b * 512 + (c + 1) * 64)
                    for k in range(8):
                        mm(P, pb[0:64, (c % 4) * 128:(c % 4 + 1) * 128], hT[:, k, tok],
                           whg[:, k, wofs + 384:wofs + 512], (c % 4 == 0 and k == 0), k == 7, skip=True)
                P.copy("act", V64[:, 0:4, :], B[3][0:64, :].rearrange("p (c n) -> p c n", c=4))
                P.copy("act", V64[:, 4:8, :], B[4][0:64, :].rearrange("p (c n) -> p c n", c=4))
                P.act(sig[:, :], B[1][:, :], AF.Sigmoid)
                P.act(gate[:, :], B[2][:, :], AF.Silu)
                P.ts("dve", ff[:, :], sig[:, :], lbc[:, 1:2], ALU.mult, lbc[:, 0:1], ALU.add)
                P.ts("dve", ff[:, :], ff[:, :], TINY, ALU.max)
                P.act(ff[:, :], ff[:, :], AF.Ln)
                P.ts("dve", kk[:, :], sig[:, :], lbc[:, 2:3], ALU.mult, lbc[:, 1:2], ALU.add)
                P.add("dve", lambda e: e.tensor_tensor_scan(bcum[:, :].ap, rmask[:, :].ap, ff[:, :].ap, 0.0,
                                                            ALU.mult, ALU.add), [rmask[:, :], ff[:, :]], [bcum[:, :]])
                P.act(eb[:, :], bcum[:, :], AF.Exp)
                P.act(enb[:, :], bcum[:, :], AF.Exp, scale=-1.0)
                P.tt("dve", qd[:, :], B[0][:, :], eb[:, :], ALU.mult)
                P.tt("dve", ktl[:, :], kk[:, :], enb[:, :], ALU.mult)
                P.tt("dve", kdT[:, :], kk[:, :], enb[:, :], ALU.mult)
                for c in range(8):
                    cs = slice(c * 64, (c + 1) * 64)
                    P.ts("dve", kdT[:, cs], kdT[:, cs], eb[:, c * 64 + 63:c * 64 + 64], ALU.mult)
                for half in range(2):
                    pb = B[5]
                    for c4 in range(4):
                        c = half * 4 + c4
                        P.transpose(pb[0:64, c4 * 128:(c4 + 1) * 128], kdT[:, c * 64:(c + 1) * 64], identf[:, :])
                    P.copy("act", kdec[:, half * 4:(half + 1) * 4, :], pb[0:64, :].rearrange("p (c n) -> p c n", c=4))
                for c in range(8):
                    cs = slice(c * 64, (c + 1) * 64)
                    mm(P, B[6][0:64, cs], ktl[:, cs], qd[:, cs], c == 0, True, skip=True)
                P.tt("dve", attn[:, :], B[6][0:64, :], mask[:, :], ALU.mult)
                for c in range(8):
                    cs = slice(c * 64, (c + 1) * 64)
                    mm(P, B[7][:, cs], V64[:, c, :], attn[:, cs], c == 0, False, skip=True)
                    mm(P, B[7][:, cs], state_bf[:, :], qd[:, cs], False, True, skip=True)
                    su = B[3] if c % 2 == 0 else B[4]
                    P.matmul(su[:, 0:128], kdec[:, c, :], V64[:, c, :], True, True)
                    P.stt(state[:, :], state[:, :], eb[:, c * 64 + 63:c * 64 + 64], su[:, 0:128], ALU.mult, ALU.add)
                    P.copy("act", state_bf[:, :], state[:, :])
                P.copy("act", oraw[:, :], B[7][:, :])
                P.act(osq[:, :], B[7][:, :], AF.Square)
                colnorm(P, cx, None, [osq[:, :]], B[6][:, :], 128, rstdA[:, :])
                P.stt(oraw[:, :], oraw[:, :], colsr[r][:, 11 + h:12 + h], rstdA[:, :], ALU.mult, ALU.mult)
                o = osb[b % 2]
                P.tt("dve", o[:, :], oraw[:, :], gate[:, :], ALU.mult)
                P.dma("sp", oT_d[4 + 2 * r + h][:, sl], o[:, :])
    cx.pst = [B[0], B[1], B[5]]
    cx.pso = [B[2]]
    cx.psd = [B[3]]
    scale = (MLA_NOPE + MLA_ROPE) ** -0.5
    for r in range(2):
        P.dma("pool", wuq[:, :, :], wuq_d[r].rearrange("k p n -> p k n"))
        P.dma("pool", wukv[:, :, :], wukv_d[r].rearrange("k p n -> p k n"))
        for h in range(2):
            for b in range(8):
                sl = slice(b * 512, (b + 1) * 512)
                for k in range(3):
                    P.matmul(B[4][:, :], wuq[:, k, h * 256:h * 256 + 128], cqn[:, k, sl], k == 0, k == 2)
                P.copy("act", qnT[:, sl], B[4][:, :])
                for k in range(3):
                    P.matmul(B[6][0:64, :], wuq[:, k, h * 256 + 128:h * 256 + 192], cqn[:, k, sl], k == 0, k == 2)
                for k in range(3):
                    P.matmul(B[7][0:64, :], wuq[:, k, h * 256 + 192:h * 256 + 256], cqn[:, k, sl], k == 0, k == 2)
                P.tt("dve", t1[0:64, :], B[6][0:64, :], Ct[:, sl], ALU.mult)
                P.tt("dve", t2[0:64, :], B[7][0:64, :], St[:, sl], ALU.mult)
                P.tt("dve", qrT[:, sl], t1[0:64, :], t2[0:64, :], ALU.add)
                for k in range(2):
                    P.matmul(B[5][:, :], wukv[:, k, h * 256:h * 256 + 128], ckvn[:, k, sl], k == 0, k == 1)
                P.copy("act", knT[:, sl], B[5][:, :])
                for tt_ in range(4):
                    tok = slice(b * 512 + tt_ * 128, b * 512 + (tt_ + 1) * 128)
                    for k in range(2):
                        mm(P, B[4][:, tt_ * 128:(tt_ + 1) * 128], ckvn[:, k, tok],
                           wukv[:, k, h * 256 + 128:h * 256 + 256], (tt_ == 0 and k == 0), k == 1, skip=True)
                P.copy("act", V[:, b * 4:(b + 1) * 4, :], B[4][:, :].rearrange("p (t n) -> p t n", t=4))

            def out_cb(g, h=h, r=r):
                o = osb[g % 2]
                P.recip(t1[:, :], cx.psd[0][:, :])
                P.tt("dve", o[:, :], cx.pso[0][:, :], t1[:, :], ALU.mult)
                P.dma("sp", oT_d[2 * r + h][:, g * 512:(g + 1) * 512], o[:, :])

            attention(P, cx, [[(knT, qnT), (kpeT, qrT)]], V, scale, out_cb)

    P.close_scope()


BF = ml_dtypes.bfloat16


def to_hT(h):
    s = h.shape[0]
    return np.ascontiguousarray(h.reshape(s, 8, 128).transpose(1, 2, 0))


def kchunk(w):
    return np.ascontiguousarray(w.reshape(w.shape[0] // 128, 128, w.shape[1]))


def const_tables():
    ones = np.ones((128, 128), dtype=BF)
    identf = np.eye(128, dtype=np.float32)
    mask = np.zeros((64, 512), dtype=np.float32)
    for c in range(8):
        mask[:, c * 64:(c + 1) * 64] = np.triu(np.ones((64, 64), dtype=np.float32))
    rmask = np.ones((128, 512), dtype=np.float32)
    rmask[:, ::64] = 0.0
    return ones, identf, mask, rmask


def inv_freq(dim):
    return (np.float32(ROPE_THETA) ** (-(np.arange(0, dim, 2, dtype=np.float32)) / np.float32(dim))).astype(np.float32)


def even_mixer_inputs(inp, j, r, h_T, pos):
    w_in = inp["ev_w_in"][j]
    kpe = w_in[:, 640:704]
    kpe_sw = np.concatenate([kpe[:, 32:64], kpe[:, 0:32]], axis=1)
    wlat = np.concatenate([w_in[:, 0:640], kpe, kpe_sw], axis=1)
    hg_parts = []
    for i in range(2):
        gh = 2 * r + i
        hg_parts += [w_in[:, 704 + gh * 128:704 + (gh + 1) * 128], w_in[:, 1216 + gh * 128:1216 + (gh + 1) * 128],
                     w_in[:, 2240 + gh * 128:2240 + (gh + 1) * 128], w_in[:, 1728 + gh * 128:1728 + (gh + 1) * 128]]
    whg = np.concatenate(hg_parts, axis=1)
    w_uq = inp["ev_w_uq"][j]
    w_ukv = inp["ev_w_ukv"][j]
    uq_parts, ukv_parts = [], []
    for i in range(2):
        gh = 2 * r + i
        rp = w_uq[:, gh * 192 + 128:gh * 192 + 192]
        uq_parts += [w_uq[:, gh * 192:gh * 192 + 128], rp, np.concatenate([rp[:, 32:64], rp[:, 0:32]], axis=1)]
        ukv_parts += [w_ukv[:, gh * 256:gh * 256 + 256]]
    wuq = np.concatenate(uq_parts, axis=1)
    wukv = np.concatenate(ukv_parts, axis=1)
    cols = np.zeros((128, 16), dtype=np.float32)
    cols[:, 0:3] = inp["ev_g_q"][j].reshape(3, 128).T
    cols[:, 3:5] = inp["ev_g_kv"][j].reshape(2, 128).T
    f = inv_freq(MLA_ROPE)
    cols[0:32, 5] = f
    cols[32:64, 5] = f
    cols[0:32, 6] = -1.0
    cols[32:64, 6] = 1.0
    for i in range(2):
        gh = 2 * r + i
        cols[:, 7 + i] = inp["ev_lb_logits"][0][gh * 128:(gh + 1) * 128]
        cols[:, 9 + i] = inp["ev_lb_logits"][1][gh * 128:(gh + 1) * 128]
        cols[:, 11 + i] = inp["ev_g_out"][j][gh]
    ones, identf, mask, rmask = const_tables()
    return {"cols": cols, "ones": ones, "identf": identf, "mask": mask, "rmask": rmask,
            "wlat": kchunk(np.ascontiguousarray(wlat)), "whg": kchunk(np.ascontiguousarray(whg)),
            "wuq": kchunk(np.ascontiguousarray(wuq)), "wukv": kchunk(np.ascontiguousarray(wukv))}


def emit_odd_mixer(P, B, G, layer):
    lambda_init = 0.8 - 0.6 * math.exp(-0.3 * layer)
    jj = layer // 2
    P.open_scope()
    cx = Ctx()
    hT_d = G["hT_s"]
    oT_d = G["oT_s"]
    pos_d = G["pos"]
    cols_d = G["colsO%d" % jj]
    lamp_d = G["lamp%d" % jj]
    ones_d = G["ones"]
    w_d = G["wodd%d" % jj]

    hT = P.sb("hT_sb", [128, 8, SEQ], BF16)
    w = P.sb("w_sb", [128, 8, 2560], BF16)
    colsr = [P.sb("cols_sb%d" % r, [128, 16], F32) for r in range(2)]
    cols = colsr[0]
    lamp = P.sb("lamp_sb", [128, 256], F32)
    lam = P.sb("lam", [128, 8], F32)
    cx.ones = P.sb("ones_sb", [128, 128], BF16)
    cx.epsb = P.sb("epsb", [128, 1], F32)
    cx.ki = P.sb("ki", [128, 512], I32)
    cx.posi = P.sb("posi", [128, 512], I32)
    cx.posf = P.sb("posf", [128, 512], F32)
    cx.ang = P.sb("ang", [128, 512], F32)
    cx.rtmp = P.sb("rtmp", [128, 512], F32)
    t1 = cx.posf
    t2 = cx.ang
    oraw = cx.rtmp
    Ct = P.sb("Ct", [128, SEQ], BF16)
    St = P.sb("St", [128, SEQ], BF16)
    qT = P.sb("qT", [128, SEQ], BF16)
    kT = P.sb("kT", [128, SEQ], BF16)
    V = P.sb("V", [128, 32, 128], BF16)
    cx.pT = [P.sb("pT%d" % i, [128, 512], BF16) for i in range(3)]
    osb = [P.sb("osb%d" % i, [128, 512], BF16) for i in range(2)]
    osq = P.sb("osq", [128, 512], BF16)
    rstd = P.sb("rstd", [128, 512], F32)

    for r in range(2):
        P.dma("sp", colsr[r][:, :], cols_d[r])
    P.dma("sp", lamp[:, :], lamp_d[:, :])
    P.dma("sp", cx.ones[:, :], ones_d[:, :])
    P.memset("dve", cx.epsb[:, :], EPS)
    for k in range(8):
        P.dma("sp", hT[:, k, :], hT_d[k])
    P.stt(lamp[:, 0:64], lamp[:, 0:64], 1.0, lamp[:, 64:128], ALU.mult, ALU.mult, accum=lam[:, 0:1])
    P.stt(lamp[:, 128:192], lamp[:, 128:192], 1.0, lamp[:, 192:256], ALU.mult, ALU.mult, accum=lam[:, 1:2])
    P.act(lam[:, 2:3], lam[:, 0:1], AF.Exp)
    P.act(lam[:, 3:4], lam[:, 1:2], AF.Exp)
    P.tt("dve", lam[:, 4:5], lam[:, 3:4], lam[:, 2:3], ALU.subtract)
    P.ts("dve", lam[:, 5:6], lam[:, 4:5], -lambda_init, ALU.add)
    for r in range(2):
        P.ts("dve", colsr[r][:, 6:10], colsr[r][:, 2:6], 1.0 - lambda_init, ALU.mult)
    build_rope_tables(P, cx, pos_d, cols[:, 0:1], cols[:, 1:2], 128, Ct, St)

    cx.pst = [B[0], B[1], B[7]]
    cx.pso = [B[2], B[3]]
    cx.psd = [B[4], B[5]]
    scale = DF_DH ** -0.5
    for r in range(2):
        for k in range(8):
            P.dma("pool", w[:, k, :], w_d[r][k])
        for hh in range(4):
            wo = hh * 640
            for b in range(8):
                sl = slice(b * 512, (b + 1) * 512)
                for (dst, o0) in ((qT, 0), (kT, 256)):
                    for k in range(8):
                        P.matmul(B[6][:, :], w[:, k, wo + o0:wo + o0 + 128], hT[:, k, sl], k == 0, k == 7)
                    for k in range(8):
                        P.matmul(B[7][:, :], w[:, k, wo + o0 + 128:wo + o0 + 256], hT[:, k, sl], k == 0, k == 7)
                    P.tt("dve", t1[:, :], B[6][:, :], Ct[:, sl], ALU.mult)
                    P.tt("dve", t2[:, :], B[7][:, :], St[:, sl], ALU.mult)
                    P.tt("dve", dst[:, sl], t1[:, :], t2[:, :], ALU.add)
                for tt_ in range(4):
                    tok = slice(b * 512 + tt_ * 128, b * 512 + (tt_ + 1) * 128)
                    for k in range(8):
                        mm(P, B[6][:, tt_ * 128:(tt_ + 1) * 128], hT[:, k, tok],
                           w[:, k, wo + 512:wo + 640], (tt_ == 0 and k == 0), k == 7, skip=True)
                P.copy("act", V[:, b * 4:(b + 1) * 4, :], B[6][:, :].rearrange("p (t n) -> p t n", t=4))

            def out_cb(g, hh=hh, r=r):
                o = osb[g % 2]
                P.recip(t1[:, :], cx.psd[0][:, :])
                P.recip(t2[:, :], cx.psd[1][:, :])
                P.tt("dve", t1[:, :], cx.pso[0][:, :], t1[:, :], ALU.mult)
                P.tt("dve", t2[:, :], cx.pso[1][:, :], t2[:, :], ALU.mult)
                P.stt(oraw[:, :], t2[:, :], lam[:, 5:6], t1[:, :], ALU.mult, ALU.add)
                P.act(osq[:, :], oraw[:, :], AF.Square)
                colnorm(P, cx, None, [osq[:, :]], B[6][:, :], 128, rstd[:, :])
                P.stt(o[:, :], oraw[:, :], colsr[r][:, 6 + hh:7 + hh], rstd[:, :], ALU.mult, ALU.mult)
                P.dma("sp", oT_d[4 * r + hh][:, g * 512:(g + 1) * 512], o[:, :])

            pairs = [[(kT[0:64, :], qT[0:64, :])], [(kT[64:128, :], qT[64:128, :])]]
            attention(P, cx, pairs, V, scale, out_cb, ncomp=2)
    P.close_scope()


def odd_mixer_inputs(inp, jj, r, h_T, pos):
    w_in = inp["od_w_in"][jj]

    def sw(wc):
        out = wc.copy()
        for c0 in range(0, wc.shape[1], 64):
            out[:, c0:c0 + 8] = wc[:, c0 + 8:c0 + 16]
            out[:, c0 + 8:c0 + 16] = wc[:, c0:c0 + 8]
        return out

    parts = []
    for hh in range(4):
        gh = 4 * r + hh
        q = w_in[:, gh * 128:(gh + 1) * 128]
        k = w_in[:, 1024 + gh * 128:1024 + (gh + 1) * 128]
        v = w_in[:, 2048 + gh * 128:2048 + (gh + 1) * 128]
        parts += [q, sw(q), k, sw(k), v]
    w = np.concatenate(parts, axis=1)
    cols = np.zeros((128, 16), dtype=np.float32)
    f = inv_freq(DF_ROT)
    for blk in (0, 64):
        cols[blk:blk + 8, 0] = f
        cols[blk + 8:blk + 16, 0] = f
        cols[blk:blk + 8, 1] = -1.0
        cols[blk + 8:blk + 16, 1] = 1.0
    for hh in range(4):
        cols[:, 2 + hh] = inp["od_g_head"][jj][4 * r + hh]
    lamp = np.ascontiguousarray(np.broadcast_to(inp["od_lambda"][jj].reshape(1, 256), (128, 256)))
    ones = np.ones((128, 128), dtype=BF)
    return {"cols": cols, "lamp": lamp, "ones": ones, "w": kchunk(np.ascontiguousarray(w))}


def build_fused():
    nc = bass.Bass("TRN2", target_bir_lowering=False)
    P = Prog(nc)
    G = {}

    def ein(name, shape, dt=F32):
        G[name] = P.dram(name, shape, dt, "ExternalInput")

    ein("x", [SEQ // 128, 128, D_MODEL])
    ein("pos", [128, SEQ], I32)
    ein("ident", [128, 128], BF16)
    ein("ones", [128, 128], BF16)
    ein("identf", [128, 128])
    ein("mask", [64, 512])
    ein("rmask", [128, 512])
    ein("gains", [DEPTH * 6, 128, D_MODEL])
    for l in range(DEPTH):
        for i in range(2):
            ein("wg%d%d" % (l, i), [NJ, 128, 8, 128])
            ein("wu%d%d" % (l, i), [NJ, 128, 8, 128])
            ein("wd%d%d" % (l, i), [NJ, 128, D_MODEL])
        ein("wout%d" % l, [8, 128, D_MODEL])
    for j in range(DEPTH // 2):
        ein("wlat%d" % j, [8, 128, 768])
        ein("whg%d" % j, [2, 8, 128, 1024])
        ein("wuq%d" % j, [2, 3, 128, 512])
        ein("wukv%d" % j, [2, 2, 128, 512])
        ein("colsE%d" % j, [2, 128, 16])
        ein("wodd%d" % j, [2, 8, 128, 2560])
        ein("colsO%d" % j, [2, 128, 16])
        ein("lamp%d" % j, [128, 256])
    for g in range(NGRP):
        G["xs%d" % g] = P.dram("xs%d" % g, [8, 128, D_MODEL], F32, "Internal")
    G["hT_s"] = P.dram("hT_s", [8, 128, SEQ], BF16, "Internal")
    G["oT_s"] = P.dram("oT_s", [8, 128, SEQ], BF16, "Internal")
    G["xo"] = P.dram("xo", [SEQ // 128, 128, D_MODEL], F32, "ExternalOutput")
    B = [P.ps("B%d" % i, [128, 512]) for i in range(8)]
    for step in range(DEPTH + 1):
        emit_token_phase(P, B, G, step)
        if step < DEPTH:
            if step % 2 == 0:
                emit_even_mixer(P, B, G, step // 2)
            else:
                emit_odd_mixer(P, B, G, step)
    P.finalize()
    return nc


def rearr_ffn_w(W):
    return np.ascontiguousarray(W.reshape(8, 128, NJ, 128).transpose(2, 1, 0, 3))


def fused_common_inputs(inp):
    ones, identf, mask, rmask = const_tables()
    C = {"ident": np.eye(128, dtype=BF), "ones": ones, "identf": identf, "mask": mask, "rmask": rmask}
    g = inp["norm_g"].reshape(DEPTH * 6, 1, D_MODEL)
    C["gains"] = np.ascontiguousarray(np.broadcast_to(g, (DEPTH * 6, 128, D_MODEL)))
    for l in range(DEPTH):
        for i in range(2):
            C["wg%d%d" % (l, i)] = rearr_ffn_w(inp["ffn_w_gate"][l, i])
            C["wu%d%d" % (l, i)] = rearr_ffn_w(inp["ffn_w_up"][l, i])
            C["wd%d%d" % (l, i)] = np.ascontiguousarray(inp["ffn_w_down"][l, i].reshape(NJ, 128, D_MODEL))
        w_out = inp["ev_w_out"][l // 2] if l % 2 == 0 else inp["od_w_out"][l // 2]
        C["wout%d" % l] = kchunk(np.ascontiguousarray(w_out))
    for j in range(DEPTH // 2):
        e = [even_mixer_inputs(inp, j, r, None, None) for r in range(2)]
        C["wlat%d" % j] = e[0]["wlat"]
        for nm in ("whg", "wuq", "wukv"):
            C["%s%d" % (nm, j)] = np.ascontiguousarray(np.stack([e[0][nm], e[1][nm]], axis=0))
        C["colsE%d" % j] = np.ascontiguousarray(np.stack([e[0]["cols"], e[1]["cols"]], axis=0))
        o = [odd_mixer_inputs(inp, j, r, None, None) for r in range(2)]
        C["wodd%d" % j] = np.ascontiguousarray(np.stack([o[0]["w"], o[1]["w"]], axis=0))
        C["colsO%d" % j] = np.ascontiguousarray(np.stack([o[0]["cols"], o[1]["cols"]], axis=0))
        C["lamp%d" % j] = o[0]["lamp"]
    return C


_NC_CACHE = {}


def kernel(x, positions, norm_g, ffn_w_gate, ffn_w_up, ffn_w_down, ev_w_in, ev_g_q, ev_w_uq, ev_g_kv,
           ev_w_ukv, ev_lb_logits, ev_g_out, ev_w_out, od_w_in, od_lambda, od_g_head, od_w_out):
    inp = dict(x=x, positions=positions, norm_g=norm_g, ffn_w_gate=ffn_w_gate, ffn_w_up=ffn_w_up,
               ffn_w_down=ffn_w_down, ev_w_in=ev_w_in, ev_g_q=ev_g_q, ev_w_uq=ev_w_uq, ev_g_kv=ev_g_kv,
               ev_w_ukv=ev_w_ukv, ev_lb_logits=ev_lb_logits, ev_g_out=ev_g_out, ev_w_out=ev_w_out,
               od_w_in=od_w_in, od_lambda=od_lambda, od_g_head=od_g_head, od_w_out=od_w_out)
    inp = {k: np.asarray(v) for k, v in inp.items()}
    if "nc" not in _NC_CACHE:
        _NC_CACHE["nc"] = build_fused()
    nc = _NC_CACHE["nc"]
    C = fused_common_inputs(inp)
    cores = list(range(NCORES))
    in_maps = []
    for c in cores:
        b = c // 2
        m = dict(C)
        m["x"] = np.ascontiguousarray(inp["x"][b].astype(np.float32, copy=False)).reshape(SEQ // 128, 128, D_MODEL)
        m["pos"] = np.ascontiguousarray(np.broadcast_to(inp["positions"][b].astype(np.int32)[None, :], (128, SEQ)))
        in_maps.append(m)
    res = run_bass_kernel_spmd(nc, in_maps, core_ids=cores).results
    out = np.zeros((BATCH, SEQ, D_MODEL), dtype=np.float32)
    for b in range(BATCH):
        out[b] = np.asarray(res[2 * b]["xo"]).reshape(SEQ, D_MODEL)
    return out
```

```python
import math
import numpy as np
import ml_dtypes
import concourse.bass as bass
import concourse.mybir as mybir
from concourse.bass_utils import run_bass_kernel_spmd

F32 = mybir.dt.float32
BF16 = mybir.dt.bfloat16
I32 = mybir.dt.int32
AF = mybir.ActivationFunctionType
ALU = mybir.AluOpType
AX = mybir.AxisListType

D_MODEL = 1024
BATCH = 4
SEQ = 4096
DEPTH = 4
CHUNK = 64
ROPE_THETA = 500000.0
EPS = 1e-6
TINY = 1e-30
D_FF = 2816
NJ = D_FF // 128
MLA_NOPE, MLA_ROPE, MLA_V, MLA_Q_RANK, MLA_KV_RANK = 128, 64, 128, 384, 256
DF_DH = 64
DF_ROT = 16
NCORES = 8
TOK = SEQ // 2
NTT = TOK // 128


class View:
    __slots__ = ("tile", "ap")

    def __init__(self, tile, ap):
        self.tile = tile
        self.ap = ap

    def __getitem__(self, idx):
        return View(self.tile, self.ap[idx])

    def rearrange(self, s, **kw):
        return View(self.tile, self.ap.rearrange(s, **kw))

    def bitcast(self, dt):
        return View(self.tile, self.ap.bitcast(dt))


class Tile:
    def __init__(self, name, base_ap):
        self.name = name
        self.base = base_ap
        self.last_w = None
        self.readers = []

    def __getitem__(self, idx):
        return View(self, self.base[idx])

    def v(self):
        return View(self, self.base)


class Op:
    __slots__ = ("eng", "fn", "reads", "writes", "dma", "deps", "signal", "seq",
                 "sem", "val", "prev_dma")

    def __init__(self, eng, fn, reads, writes, dma):
        self.eng = eng
        self.fn = fn
        self.reads = reads
        self.writes = writes
        self.dma = dma
        self.deps = []
        self.signal = False
        self.seq = 0
        self.sem = None
        self.val = 0
        self.prev_dma = None


class Prog:
    ENGS = ("pe", "act", "dve", "pool", "sp")
    NDMASEM = 12

    def __init__(self, nc):
        self.nc = nc
        self.ops = []
        self.n_sb = 0
        self.stack = None
        self.uid = 0

    def open_scope(self):
        import contextlib
        self.stack = contextlib.ExitStack()

    def close_scope(self):
        self.stack.close()
        self.stack = None
        self.ops.append("BARRIER")

    def sb(self, name, shape, dtype):
        if self.stack is not None:
            self.uid += 1
            h = self.stack.enter_context(self.nc.sbuf_tensor("sb%d_%s" % (self.uid, name), list(shape), dtype))
        else:
            h = self.nc.alloc_sbuf_tensor("sb_" + name, list(shape), dtype)
        idx = tuple(slice(None) for _ in shape)
        return Tile(name, h[idx])

    def ps(self, name, shape, dtype=F32):
        h = self.nc.alloc_psum_tensor("ps_" + name, list(shape), dtype)
        idx = tuple(slice(None) for _ in shape)
        return Tile(name, h[idx])

    def dram(self, name, shape, dtype, kind):
        h = self.nc.dram_tensor(name, list(shape), dtype, kind=kind)
        return Tile(name, h.ap())

    def add(self, eng, fn, reads, writes, dma=False):
        rt = []
        for r in reads:
            if isinstance(r, View) and r.tile not in rt:
                rt.append(r.tile)
        wt = []
        for w in writes:
            if isinstance(w, View) and w.tile not in wt:
                wt.append(w.tile)
        op = Op(eng, fn, rt, wt, dma)
        self.ops.append(op)
        return op

    def dma(self, q, out, in_):
        return self.add(q, lambda e: e.dma_start(out=out.ap, in_=in_.ap), [in_], [out], dma=True)

    def matmul(self, out, lhsT, rhs, start, stop):
        return self.add("pe", lambda e: e.matmul(out.ap, lhsT.ap, rhs.ap, start=start, stop=stop),
                        [lhsT, rhs], [out])

    def transpose(self, out, in_, ident):
        return self.add("pe", lambda e: e.transpose(out.ap, in_.ap, ident.ap), [in_, ident], [out])

    def act(self, out, in_, func, bias=None, scale=None, accum=None, eng="act"):
        reads = [in_]
        writes = [out]
        kw = {}
        if bias is not None:
            if isinstance(bias, View):
                reads.append(bias)
                kw["bias"] = bias.ap
            else:
                kw["bias"] = float(bias)
        if scale is not None:
            if isinstance(scale, View):
                reads.append(scale)
                kw["scale"] = scale.ap
            else:
                kw["scale"] = float(scale)
        if accum is not None:
            writes.append(accum)
            kw["accum_out"] = accum.ap
        return self.add(eng, lambda e: e.activation(out.ap, in_.ap, func, **kw), reads, writes)

    def tt(self, eng, out, in0, in1, op):
        return self.add(eng, lambda e: e.tensor_tensor(out.ap, in0.ap, in1.ap, op), [in0, in1], [out])

    def ts(self, eng, out, in0, s1, op0, s2=None, op1=None, accum=None):
        reads = [in0]
        a1 = s1.ap if isinstance(s1, View) else float(s1)
        if isinstance(s1, View):
            reads.append(s1)
        a2 = None
        if s2 is not None:
            a2 = s2.ap if isinstance(s2, View) else float(s2)
            if isinstance(s2, View):
                reads.append(s2)
        writes = [out]
        kw = {}
        if accum is not None:
            writes.append(accum)
            kw["accum_out"] = accum.ap
        o1 = op1 if op1 is not None else ALU.bypass
        return self.add(eng, lambda e: e.tensor_scalar(out.ap, in0.ap, a1, a2, op0, o1, **kw), reads, writes)

    def stt(self, out, in0, scalar, in1, op0, op1, accum=None):
        reads = [in0, in1]
        a = scalar.ap if isinstance(scalar, View) else float(scalar)
        if isinstance(scalar, View):
            reads.append(scalar)
        writes = [out]
        kw = {}
        if accum is not None:
            writes.append(accum)
            kw["accum_out"] = accum.ap
        return self.add("dve", lambda e: e.scalar_tensor_tensor(out.ap, in0.ap, a, in1.ap, op0, op1, **kw),
                        reads, writes)

    def copy(self, eng, out, in_):
        if eng == "act":
            return self.add("act", lambda e: e.copy(out.ap, in_.ap), [in_], [out])
        return self.add(eng, lambda e: e.tensor_copy(out.ap, in_.ap), [in_], [out])

    def memset(self, eng, out, val):
        return self.add(eng, lambda e: e.memset(out.ap, val), [], [out])

    def recip(self, out, in_):
        return self.add("dve", lambda e: e.reciprocal(out.ap, in_.ap), [in_], [out])

    def finalize(self):
        nc = self.nc
        ops = self.ops
        real = []
        fence = []
        last_eng = {}
        dma_q = {}
        for op in ops:
            if isinstance(op, str):
                fence = [o for o in last_eng.values()]
                for q, lst in dma_q.items():
                    fence.extend(lst[-self.NDMASEM:])
                continue
            real.append(op)
            if op.dma:
                dma_q.setdefault(op.eng, []).append(op)
            else:
                last_eng[op.eng] = op
            op.deps.extend(fence)
        ops = real
        self.ops = real
        for op in ops:
            deps = list(op.deps)
            op.deps = []
            raw = set()
            for t in op.reads:
                if t.last_w is not None:
                    deps.append(t.last_w)
                    raw.add(id(t.last_w))
            for t in op.writes:
                if t.last_w is not None:
                    deps.append(t.last_w)
                deps.extend(t.readers)
            seen = set()
            for d in deps:
                if d is op or id(d) in seen:
                    continue
                seen.add(id(d))
                if (not d.dma) and d.eng == op.eng and op.eng == "pe":
                    continue
                op.deps.append(d)
            for t in op.reads:
                t.readers.append(op)
            for t in op.writes:
                t.last_w = op
                t.readers = []
        for op in ops:
            for d in op.deps:
                if not d.dma:
                    d.signal = True
        cnt = {e: 0 for e in self.ENGS}
        dcnt = {e: 0 for e in self.ENGS}
        dma_hist = {e: [] for e in self.ENGS}
        esem = {}
        dsem = {}
        for e in self.ENGS:
            esem[e] = nc.alloc_semaphore("s_" + e)
        for op in ops:
            if op.dma:
                q = op.eng
                if q not in dsem:
                    dsem[q] = [nc.alloc_semaphore("d_%s_%d" % (q, i)) for i in range(self.NDMASEM)]
                i = dcnt[q]
                dcnt[q] += 1
                op.sem = dsem[q][i % self.NDMASEM]
                op.val = 16 * (i // self.NDMASEM + 1)
                if i >= self.NDMASEM:
                    op.prev_dma = dma_hist[q][i - self.NDMASEM]
                dma_hist[q].append(op)
            elif op.signal:
                cnt[op.eng] += 1
                op.seq = cnt[op.eng]
                op.sem = esem[op.eng]
                op.val = op.seq
        per_eng = {e: [] for e in self.ENGS}
        for op in ops:
            per_eng[op.eng].append(op)
        last_dma = {q: h for q, h in dma_hist.items() if h}

        def emit(eng_name, e):
            waited = {}
            for op in per_eng[eng_name]:
                need = {}
                dl = list(op.deps)
                if op.prev_dma is not None:
                    dl.append(op.prev_dma)
                for d in dl:
                    k = d.sem.num
                    if waited.get(k, 0) >= d.val:
                        continue
                    if k not in need or need[k][1] < d.val:
                        need[k] = (d.sem, d.val)
                for k, (s, v) in need.items():
                    e.wait_ge(s, v)
                    waited[k] = v
                ins = op.fn(e)
                if op.dma:
                    ins.then_inc(op.sem, 16)
                elif op.signal:
                    ins.then_inc(op.sem, 1)
            if eng_name in last_dma:
                fin = {}
                for d in last_dma[eng_name]:
                    k = d.sem.num
                    if k not in fin or fin[k][1] < d.val:
                        fin[k] = (d.sem, d.val)
                for k, (s, v) in fin.items():
                    if waited.get(k, 0) < v:
                        e.wait_ge(s, v)

        with nc.Block() as block:
            @block.tensor
            def _(e):
                emit("pe", e)

            @block.scalar
            def _(e):
                emit("act", e)

            @block.vector
            def _(e):
                emit("dve", e)

            @block.gpsimd
            def _(e):
                emit("pool", e)

            @block.sync
            def _(e):
                emit("sp", e)


class Ctx:
    pass


def rms_stats(P, cx, src, d, rstd):
    P.act(cx.junk[:, 0:d], src, AF.Square, accum=cx.ss[:, 0:1])
    P.act(cx.ss[:, 1:2], cx.ss[:, 0:1], AF.Sqrt, bias=cx.epsb[:, 0:1], scale=1.0 / d)
    P.recip(rstd, cx.ss[:, 1:2])


STAGE = 9


def ffn_group(P, cx, xt, W, gpre, gpost, name):
    nt = len(xt)
    nh = nt // 4
    for t in range(nt):
        rms_stats(P, cx, xt[t][:, :], D_MODEL, cx.rstd[:, 0:1])
        P.stt(cx.hn[:, :], xt[t][:, :], cx.rstd[:, 0:1], gpre[:, :], ALU.mult, ALU.mult)
        pt = cx.ptr[t % 2]
        for kc in range(8):
            P.transpose(pt[:, kc * 128:(kc + 1) * 128], cx.hn[:, kc * 128:(kc + 1) * 128], cx.ident[:, :])
        h = t // 4
        P.copy("act", cx.hT[h][:, :, (t % 4) * 128:(t % 4 + 1) * 128],
               pt.rearrange("p (k n) -> p k n", k=8))
    if STAGE < 2:
        return
    for j in range(NJ):
        wg = cx.wg[j % len(cx.wg)]
        wu = cx.wu[j % len(cx.wu)]
        P.dma("pool", wg[:, :, :], W["wg"][j])
        P.dma("pool", wu[:, :, :], W["wu"][j])
        P.dma("pool", cx.wd[j][:, :], W["wd_d"][j])
        for h in range(nh):
            pg = cx.pg[(j * nh + h) % 2]
            pu = cx.pu[(j * nh + h) % 2]
            for kc in range(8):
                P.matmul(pg[:, :], wg[:, kc, :], cx.hT[h][:, kc, :], kc == 0, kc == 7)
            for kc in range(8):
                P.matmul(pu[:, :], wu[:, kc, :], cx.hT[h][:, kc, :], kc == 0, kc == 7)
            sg = cx.sg[(j * nh + h) % 2]
            P.act(sg[:, :], pg[:, :], AF.Silu)
            P.tt("dve", cx.aT[j][:, h * 512:(h + 1) * 512], sg[:, :], pu[:, :], ALU.mult)
    if STAGE < 3:
        return
    for t in range(nt):
        y = cx.y[t % 2]
        for n in range(2):
            pd = cx.pd[(t * 2 + n) % 2]
            for j in range(NJ):
                P.matmul(pd[:, :], cx.aT[j][:, t * 128:(t + 1) * 128],
                         W["wd"][j][:, n * 512:(n + 1) * 512], j == 0, j == NJ - 1)
            P.copy("act", y[:, n * 512:(n + 1) * 512], pd[:, :])
        norm_residual(P, cx, xt[t], y, gpost, 0.5)


def norm_residual(P, cx, x, y, g, alpha):
    rms_stats(P, cx, y[:, :], D_MODEL, cx.rstd[:, 1:2])
    P.stt(y[:, :], y[:, :], cx.rstd[:, 1:2], g[:, :], ALU.mult, ALU.mult)
    P.stt(x[:, :], y[:, :], float(alpha), x[:, :], ALU.mult, ALU.add)


def load_wd(P, cx, wd_dram):
    for j in range(NJ):
        P.dma("pool", cx.wd[j][:, :], wd_dram[j])


NGRP = SEQ // 1024


def emit_token_phase(P, B, G, step):
    do_post = step > 0
    do_pre = step < DEPTH
    P.open_scope()
    cx = Ctx()
    cx.ident = P.sb("ident_sb", [128, 128], BF16)
    cx.junk = P.sb("junk", [128, D_MODEL], BF16)
    cx.ss = P.sb("ss", [128, 2], F32)
    cx.rstd = P.sb("rstd", [128, 2], F32)
    cx.epsb = P.sb("epsb", [128, 1], F32)
    cx.hn = P.sb("hn", [128, D_MODEL], BF16)
    cx.hT = [P.sb("hT%d" % i, [128, 8, 512], BF16) for i in range(2)]
    cx.wg = [P.sb("wg%d" % i, [128, 8, 128], BF16) for i in range(3)]
    cx.wu = [P.sb("wu%d" % i, [128, 8, 128], BF16) for i in range(3)]
    cx.wd = [P.sb("wd%d" % j, [128, D_MODEL], BF16) for j in range(NJ)]
    cx.aT = [P.sb("aT%d" % j, [128, 1024], BF16) for j in range(NJ)]
    cx.sg = [P.sb("sg%d" % i, [128, 512], F32) for i in range(2)]
    cx.y = [P.sb("y%d" % i, [128, D_MODEL], F32) for i in range(2)]
    gt = [P.sb("g%d" % i, [128, D_MODEL], F32) for i in range(6)]
    xt = [P.sb("x%d" % i, [128, D_MODEL], F32) for i in range(8)]
    if do_post:
        wout = [P.sb("wout%d" % k, [128, D_MODEL], BF16) for k in range(8)]
    if do_pre:
        hTo = P.sb("hTo", [128, 8, 128], BF16)
    cx.pg = [B[0], B[1]]
    cx.pu = [B[2], B[3]]
    cx.pd = [B[4], B[5]]
    cx.ptr = [B[6][:, :].bitcast(BF16), B[7][:, :].bitcast(BF16)]

    P.dma("sp", cx.ident[:, :], G["ident"][:, :])
    P.memset("dve", cx.epsb[:, :], EPS)
    Wpost = Wpre = None
    if do_post:
        l = step - 1
        for i in range(3):
            P.dma("sp", gt[i][:, :], G["gains"][l * 6 + 3 + i])
        for k in range(8):
            P.dma("pool", wout[k][:, :], G["wout%d" % l][k])
        Wpost = {"wg": G["wg%d1" % l], "wu": G["wu%d1" % l], "wd": cx.wd, "wd_d": G["wd%d1" % l]}
    if do_pre:
        l = step
        for i in range(3):
            P.dma("sp", gt[3 + i][:, :], G["gains"][l * 6 + i])
        Wpre = {"wg": G["wg%d0" % l], "wu": G["wu%d0" % l], "wd": cx.wd, "wd_d": G["wd%d0" % l]}

    for grp in range(NGRP):
        for t in range(8):
            src = G["x"][grp * 8 + t] if step == 0 else G["xs%d" % grp][t]
            P.dma("sp", xt[t][:, :], src)
        if do_post:
            l = step - 1
            for h2 in range(2):
                P.dma("sp", cx.hT[h2][:, :, :],
                      G["oT_s"][:, :, grp * 1024 + h2 * 512:grp * 1024 + (h2 + 1) * 512].rearrange("k p n -> p k n"))
            for t in range(8):
                y = cx.y[t % 2]
                for n in range(2):
                    pd = cx.pd[(t * 2 + n) % 2]
                    for k in range(8):
                        P.matmul(pd[:, :], cx.hT[t // 4][:, k, (t % 4) * 128:(t % 4 + 1) * 128],
                                 wout[k][:, n * 512:(n + 1) * 512], k == 0, k == 7)
                    P.copy("act", y[:, n * 512:(n + 1) * 512], pd[:, :])
                norm_residual(P, cx, xt[t], y, gt[0], 1.0)
            ffn_group(P, cx, xt, Wpost, gt[1], gt[2], "f2")
        if do_pre:
            l = step
            ffn_group(P, cx, xt, Wpre, gt[3], gt[4], "f1")
            for t in range(8):
                rms_stats(P, cx, xt[t][:, :], D_MODEL, cx.rstd[:, 0:1])
                P.stt(cx.hn[:, :], xt[t][:, :], cx.rstd[:, 0:1], gt[5][:, :], ALU.mult, ALU.mult)
                pt = cx.ptr[t % 2]
                for kc in range(8):
                    P.transpose(pt[:, kc * 128:(kc + 1) * 128], cx.hn[:, kc * 128:(kc + 1) * 128], cx.ident[:, :])
                P.copy("act", hTo[:, :, :], pt.rearrange("p (k n) -> p k n", k=8))
                tok0 = grp * 1024 + t * 128
                P.dma("sp", G["hT_s"][:, :, tok0:tok0 + 128].rearrange("k p n -> p k n"), hTo[:, :, :])
        for t in range(8):
            dst = G["xo"][grp * 8 + t] if step == DEPTH else G["xs%d" % grp][t]
            P.dma("sp", dst, xt[t][:, :])
    P.close_scope()


def mm(P, out, lhsT, rhs, start, stop, skip=False):
    if skip:
        return P.add("pe", lambda e: e.matmul(out.ap, lhsT.ap, rhs.ap, start=start, stop=stop,
                                              skip_group_check=True), [lhsT, rhs], [out])
    return P.matmul(out, lhsT, rhs, start, stop)


def build_rope_tables(P, cx, pos_d, freq_col, sgn_col, nrows, Ct, St):
    PI = math.pi
    PIS = 3.1415925
    n = nrows
    for b in range(8):
        sl = slice(b * 512, (b + 1) * 512)
        P.dma("sp", cx.posi[0:n, :], pos_d[0:n, sl])
        P.copy("dve", cx.posf[0:n, :], cx.posi[0:n, :])
        P.ts("dve", cx.ang[0:n, :], cx.posf[0:n, :], freq_col, ALU.mult)
        for shift, dst, sg in ((0.0, St, sgn_col), (0.5 * PI, Ct, None)):
            if shift != 0.0:
                P.ts("dve", cx.ang[0:n, :], cx.ang[0:n, :], shift, ALU.add)
            P.ts("dve", cx.rtmp[0:n, :], cx.ang[0:n, :], 1.0 / (2 * PI), ALU.mult)
            P.copy("dve", cx.ki[0:n, :], cx.rtmp[0:n, :])
            P.copy("dve", cx.rtmp[0:n, :], cx.ki[0:n, :])
            P.stt(cx.rtmp[0:n, :], cx.rtmp[0:n, :], -2 * PI, cx.ang[0:n, :], ALU.mult, ALU.add)
            P.ts("dve", cx.posf[0:n, :], cx.rtmp[0:n, :], PI, ALU.is_gt, 2 * PI, ALU.mult)
            P.tt("dve", cx.rtmp[0:n, :], cx.rtmp[0:n, :], cx.posf[0:n, :], ALU.subtract)
            P.ts("dve", cx.rtmp[0:n, :], cx.rtmp[0:n, :], PIS, ALU.min, -PIS, ALU.max)
            if sg is not None:
                P.act(cx.rtmp[0:n, :], cx.rtmp[0:n, :], AF.Sin)
                P.ts("dve", dst[0:n, sl], cx.rtmp[0:n, :], sg, ALU.mult)
            else:
                P.act(dst[0:n, sl], cx.rtmp[0:n, :], AF.Sin)


def attention(P, cx, qk_pairs, V, scale, out_cb, ncomp=1):
    its = []
    for g in range(8):
        nk = 4 * (g + 1)
        for kt in range(nk):
            for c in range(ncomp):
                its.append((g, kt, c, nk))
    nb = len(cx.pst)

    def front(i):
        g, kt, c, nk = its[i]
        a = kt - 4 * g
        q0 = 128 * a if a > 0 else 0
        st = cx.pst[i % nb]
        pT = cx.pT[i % nb]
        prs = qk_pairs[c]
        for n_, (kT, qT) in enumerate(prs):
            P.matmul(st[:, q0:512], kT[:, kt * 128:(kt + 1) * 128],
                     qT[:, g * 512 + q0:(g + 1) * 512], n_ == 0, n_ == len(prs) - 1)
        P.act(pT[:, q0:512], st[:, q0:512], AF.Exp, scale=scale)
        if a >= 0:
            P.memset("dve", pT[64:128, q0:q0 + 64], 0.0)

    def back(i):
        g, kt, c, nk = its[i]
        a = kt - 4 * g
        q0 = 128 * a if a > 0 else 0
        pT = cx.pT[i % nb]
        mm(P, cx.pso[c][:, q0:512], V[:, kt, :], pT[:, q0:512], kt == 0, kt == nk - 1, skip=True)
        mm(P, cx.psd[c][:, q0:512], cx.ones[:, :], pT[:, q0:512], kt == 0, kt == nk - 1, skip=True)
        if kt == nk - 1 and c == ncomp - 1:
            out_cb(g)

    SK = nb - 1
    n = len(its)
    for i in range(n + SK):
        if i < n:
            front(i)
        if i - SK >= 0:
            back(i - SK)


def colnorm(P, cx, raws, sqs, ps_ss, d, rstd_out):
    for i, sq in enumerate(sqs):
        P.matmul(ps_ss, cx.ones[:, :], sq, i == 0, i == len(sqs) - 1)
    P.act(rstd_out, ps_ss, AF.Sqrt, bias=cx.epsb[:, 0:1], scale=1.0 / d)
    P.recip(rstd_out, rstd_out)


def emit_even_mixer(P, B, G, j):
    P.open_scope()
    cx = Ctx()
    hT_d = G["hT_s"]
    oT_d = G["oT_s"]
    pos_d = G["pos"]
    cols_d = G["colsE%d" % j]
    ones_d, identf_d, mask_d, rmask_d = G["ones"], G["identf"], G["mask"], G["rmask"]
    wlat_d, whg_d, wuq_d, wukv_d = G["wlat%d" % j], G["whg%d" % j], G["wuq%d" % j], G["wukv%d" % j]

    hT = P.sb("hT_sb", [128, 8, SEQ], BF16)
    colsr = [P.sb("cols_sb%d" % r, [128, 16], F32) for r in range(2)]
    cols = colsr[0]
    cx.ones = P.sb("ones_sb", [128, 128], BF16)
    identf = P.sb("identf_sb", [128, 128], F32)
    mask = P.sb("mask_sb", [64, 512], F32)
    rmask = P.sb("rmask_sb", [128, 512], F32)
    wlat = P.sb("wlat_sb", [128, 8, 768], BF16)
    whg = P.sb("whg_sb", [128, 8, 1024], BF16)
    wuq = P.sb("wuq_sb", [128, 3, 512], BF16)
    wukv = P.sb("wukv_sb", [128, 2, 512], BF16)
    cx.epsb = P.sb("epsb", [128, 1], F32)
    cx.ki = P.sb("ki", [128, 512], I32)
    cx.posi = P.sb("posi", [128, 512], I32)
    sig = P.sb("sig", [128, 512], F32)
    ff = P.sb("ff", [128, 512], F32)
    kk = P.sb("kk", [128, 512], F32)
    bcum = P.sb("bcum", [128, 512], F32)
    eb = P.sb("eb", [128, 512], F32)
    enb = P.sb("enb", [128, 512], F32)
    kdT = P.sb("kdT", [128, 512], F32)
    gate = P.sb("gate", [128, 512], F32)
    cx.posf = sig
    cx.ang = ff
    cx.rtmp = kk
    Ct = P.sb("Ct", [64, SEQ], BF16)
    St = P.sb("St", [64, SEQ], BF16)
    cqn = P.sb("cqn", [128, 3, SEQ], BF16)
    ckvn = P.sb("ckvn", [128, 2, SEQ], BF16)
    kpeT = P.sb("kpeT", [64, SEQ], BF16)
    raw = [bcum, eb, enb, kdT, gate]
    sq = [P.sb("sq%d" % i, [128, 512], BF16) for i in range(5)]
    rstdA = P.sb("rstdA", [128, 512], F32)
    rstdB = ff
    t1 = kk
    t2 = sig
    qnT = hT[:, 0, :]
    knT = hT[:, 1, :]
    qrT = hT[0:64, 2, :]
    V = hT[:, 3, :].rearrange("p (t n) -> p t n", t=32)
    cx.pT = [P.sb("pT%d" % i, [128, 512], BF16) for i in range(3)]
    osb = [P.sb("osb%d" % i, [128, 512], BF16) for i in range(2)]

    for r in range(2):
        P.dma("sp", colsr[r][:, :], cols_d[r])
    P.dma("sp", cx.ones[:, :], ones_d[:, :])
    P.dma("sp", identf[:, :], identf_d[:, :])
    P.dma("sp", mask[:, :], mask_d[:, :])
    P.dma("sp", rmask[:, :], rmask_d[:, :])
    P.memset("dve", cx.epsb[:, :], EPS)
    P.dma("pool", wlat[:, :, :], wlat_d[:, :, :].rearrange("k p n -> p k n"))
    for k in range(8):
        P.dma("sp", hT[:, k, :], hT_d[k])
    build_rope_tables(P, cx, pos_d, cols[0:64, 5:6], cols[0:64, 6:7], 64, Ct, St)

    for b in range(8):
        sl = slice(b * 512, (b + 1) * 512)
        for oc in range(5):
            pb = B[oc % 4]
            for k in range(8):
                P.matmul(pb[:, :], wlat[:, k, oc * 128:(oc + 1) * 128], hT[:, k, sl], k == 0, k == 7)
            P.copy("act", raw[oc][:, :], pb[:, :])
            P.act(sq[oc][:, :], pb[:, :], AF.Square)
        colnorm(P, cx, raw[0:3], [s[:, :] for s in sq[0:3]], B[4][:, :], MLA_Q_RANK, rstdA[:, :])
        colnorm(P, cx, raw[3:5], [s[:, :] for s in sq[3:5]], B[5][:, :], MLA_KV_RANK, rstdB[:, :])
        for c in range(3):
            P.stt(cqn[:, c, sl], raw[c][:, :], cols[:, c:c + 1], rstdA[:, :], ALU.mult, ALU.mult)
        for c in range(2):
            P.stt(ckvn[:, c, sl], raw[3 + c][:, :], cols[:, 3 + c:4 + c], rstdB[:, :], ALU.mult, ALU.mult)
        for k in range(8):
            P.matmul(B[6][0:64, :], wlat[:, k, 640:704], hT[:, k, sl], k == 0, k == 7)
        for k in range(8):
            P.matmul(B[7][0:64, :], wlat[:, k, 704:768], hT[:, k, sl], k == 0, k == 7)
        P.tt("dve", t1[0:64, :], B[6][0:64, :], Ct[:, sl], ALU.mult)
        P.tt("dve", t2[0:64, :], B[7][0:64, :], St[:, sl], ALU.mult)
        P.tt("dve", kpeT[:, sl], t1[0:64, :], t2[0:64, :], ALU.add)

    lbc = P.sb("lbc", [128, 4], F32)
    state = P.sb("state", [128, 128], F32)
    state_bf = P.sb("state_bf", [128, 128], BF16)
    qd = P.sb("qd", [128, 512], BF16)
    ktl = P.sb("ktl", [128, 512], BF16)
    kdec = P.sb("kdec", [64, 8, 128], BF16)
    V64 = P.sb("V64", [64, 8, 128], BF16)
    attn = P.sb("attn", [64, 512], BF16)
    oraw = bcum
    osq = P.sb("osq", [128, 512], BF16)
    for r in range(2):
        P.dma("pool", whg[:, :, :], whg_d[r].rearrange("k p n -> p k n"))
        for h in range(2):
            wofs = h * 512
            if j == 0:
                P.memset("dve", lbc[:, 0:1], 0.0)
            else:
                P.tt("dve", lbc[:, 3:4], colsr[r][:, 9 + h:10 + h], colsr[r][:, 7 + h:8 + h], ALU.subtract)
                P.act(lbc[:, 0:1], lbc[:, 3:4], AF.Sigmoid)
            P.ts("dve", lbc[:, 1:2], lbc[:, 0:1], -1.0, ALU.mult, 1.0, ALU.add)
            P.ts("dve", lbc[:, 2:3], lbc[:, 1:2], -1.0, ALU.mult)
            P.memset("dve", state[:, :], 0.0)
            P.memset("dve", state_bf[:, :], 0.0)
            for b in range(8):
                sl = slice(b * 512, (b + 1) * 512)
                for i, pb in enumerate((B[0], B[1], B[2])):
                    for k in range(8):
                        P.matmul(pb[:, :], whg[:, k, wofs + i * 128:wofs + (i + 1) * 128], hT[:, k, sl], k == 0, k == 7)
                for c in range(8):
                    pb = B[3 + c // 4]
                    tok = slice(b * 512 + c * 64, b * 512 + (c + 1) * 64)
                    for k in range(8):
                        mm(P, pb[0:64, (c % 4) * 128:(c % 4 + 1) * 128], hT[:, k, tok],
                           whg[:, k, wofs + 384:wofs + 512], (c % 4 == 0 and k == 0), k == 7, skip=True)
                P.copy("act", V64[:, 0:4, :], B[3][0:64, :].rearrange("p (c n) -> p c n", c=4))
                P.copy("act", V64[:, 4:8, :], B[4][0:64, :].rearrange("p (c n) -> p c n", c=4))
                P.act(sig[:, :], B[1][:, :], AF.Sigmoid)
                P.act(gate[:, :], B[2][:, :], AF.Silu)
                P.ts("dve", ff[:, :], sig[:, :], lbc[:, 1:2], ALU.mult, lbc[:, 0:1], ALU.add)
                P.ts("dve", ff[:, :], ff[:, :], TINY, ALU.max)
                P.act(ff[:, :], ff[:, :], AF.Ln)
                P.ts("dve", kk[:, :], sig[:, :], lbc[:, 2:3], ALU.mult, lbc[:, 1:2], ALU.add)
                P.add("dve", lambda e: e.tensor_tensor_scan(bcum[:, :].ap, rmask[:, :].ap, ff[:, :].ap, 0.0,
                                                            ALU.mult, ALU.add), [rmask[:, :], ff[:, :]], [bcum[:, :]])
                P.act(eb[:, :], bcum[:, :], AF.Exp)
                P.act(enb[:, :], bcum[:, :], AF.Exp, scale=-1.0)
                P.tt("dve", qd[:, :], B[0][:, :], eb[:, :], ALU.mult)
                P.tt("dve", ktl[:, :], kk[:, :], enb[:, :], ALU.mult)
                P.tt("dve", kdT[:, :], kk[:, :], enb[:, :], ALU.mult)
                for c in range(8):
                    cs = slice(c * 64, (c + 1) * 64)
                    P.ts("dve", kdT[:, cs], kdT[:, cs], eb[:, c * 64 + 63:c * 64 + 64], ALU.mult)
                for half in range(2):
                    pb = B[5]
                    for c4 in range(4):
                        c = half * 4 + c4
                        P.transpose(pb[0:64, c4 * 128:(c4 + 1) * 128], kdT[:, c * 64:(c + 1) * 64], identf[:, :])
                    P.copy("act", kdec[:, half * 4:(half + 1) * 4, :], pb[0:64, :].rearrange("p (c n) -> p c n", c=4))
                for c in range(8):
                    cs = slice(c * 64, (c + 1) * 64)
                    mm(P, B[6][0:64, cs], ktl[:, cs], qd[:, cs], c == 0, True, skip=True)
                P.tt("dve", attn[:, :], B[6][0:64, :], mask[:, :], ALU.mult)
                for c in range(8):
                    cs = slice(c * 64, (c + 1) * 64)
                    mm(P, B[7][:, cs], V64[:, c, :], attn[:, cs], c == 0, False, skip=True)
                    mm(P, B[7][:, cs], state_bf[:, :], qd[:, cs], False, True, skip=True)
                    su = B[3] if c % 2 == 0 else B[4]
                    P.matmul(su[:, 0:128], kdec[:, c, :], V64[:, c, :], True, True)
                    P.stt(state[:, :], state[:, :], eb[:, c * 64 + 63:c * 64 + 64], su[:, 0:128], ALU.mult, ALU.add)
                    P.copy("act", state_bf[:, :], state[:, :])
                P.copy("act", oraw[:, :], B[7][:, :])
                P.act(osq[:, :], B[7][:, :], AF.Square)
                colnorm(P, cx, None, [osq[:, :]], B[6][:, :], 128, rstdA[:, :])
                P.stt(oraw[:, :], oraw[:, :], colsr[r][:, 11 + h:12 + h], rstdA[:, :], ALU.mult, ALU.mult)
                o = osb[b % 2]
                P.tt("dve", o[:, :], oraw[:, :], gate[:, :], ALU.mult)
                P.dma("sp", oT_d[4 + 2 * r + h][:, sl], o[:, :])
    cx.pst = [B[0], B[1], B[5]]
    cx.pso = [B[2]]
    cx.psd = [B[3]]
    scale = (MLA_NOPE + MLA_ROPE) ** -0.5
    for r in range(2):
        P.dma("pool", wuq[:, :, :], wuq_d[r].rearrange("k p n -> p k n"))
        P.dma("pool", wukv[:, :, :], wukv_d[r].rearrange("k p n -> p k n"))
        for h in range(2):
            for b in range(8):
                sl = slice(b * 512, (b + 1) * 512)
                for k in range(3):
                    P.matmul(B[4][:, :], wuq[:, k, h * 256:h * 256 + 128], cqn[:, k, sl], k == 0, k == 2)
                P.copy("act", qnT[:, sl], B[4][:, :])
                for k in range(3):
                    P.matmul(B[6][0:64, :], wuq[:, k, h * 256 + 128:h * 256 + 192], cqn[:, k, sl], k == 0, k == 2)
                for k in range(3):
                    P.matmul(B[7][0:64, :], wuq[:, k, h * 256 + 192:h * 256 + 256], cqn[:, k, sl], k == 0, k == 2)
                P.tt("dve", t1[0:64, :], B[6][0:64, :], Ct[:, sl], ALU.mult)
                P.tt("dve", t2[0:64, :], B[7][0:64, :], St[:, sl], ALU.mult)
                P.tt("dve", qrT[:, sl], t1[0:64, :], t2[0:64, :], ALU.add)
                for k in range(2):
                    P.matmul(B[5][:, :], wukv[:, k, h * 256:h * 256 + 128], ckvn[:, k, sl], k == 0, k == 1)
                P.copy("act", knT[:, sl], B[5][:, :])
                for tt_ in range(4):
                    tok = slice(b * 512 + tt_ * 128, b * 512 + (tt_ + 1) * 128)
                    for k in range(2):
                        mm(P, B[4][:, tt_ * 128:(tt_ + 1) * 128], ckvn[:, k, tok],
                           wukv[:, k, h * 256 + 128:h * 256 + 256], (tt_ == 0 and k == 0), k == 1, skip=True)
                P.copy("act", V[:, b * 4:(b + 1) * 4, :], B[4][:, :].rearrange("p (t n) -> p t n", t=4))

            def out_cb(g, h=h, r=r):
                o = osb[g % 2]
                P.recip(t1[:, :], cx.psd[0][:, :])
                P.tt("dve", o[:, :], cx.pso[0][:, :], t1[:, :], ALU.mult)
                P.dma("sp", oT_d[2 * r + h][:, g * 512:(g + 1) * 512], o[:, :])

            attention(P, cx, [[(knT, qnT), (kpeT, qrT)]], V, scale, out_cb)

    P.close_scope()


BF = ml_dtypes.bfloat16


def to_hT(h):
    s = h.shape[0]
    return np.ascontiguousarray(h.reshape(s, 8, 128).transpose(1, 2, 0))


def kchunk(w):
    return np.ascontiguousarray(w.reshape(w.shape[0] // 128, 128, w.shape[1]))


def const_tables():
    ones = np.ones((128, 128), dtype=BF)
    identf = np.eye(128, dtype=np.float32)
    mask = np.zeros((64, 512), dtype=np.float32)
    for c in range(8):
        mask[:, c * 64:(c + 1) * 64] = np.triu(np.ones((64, 64), dtype=np.float32))
    rmask = np.ones((128, 512), dtype=np.float32)
    rmask[:, ::64] = 0.0
    return ones, identf, mask, rmask


def inv_freq(dim):
    return (np.float32(ROPE_THETA) ** (-(np.arange(0, dim, 2, dtype=np.float32)) / np.float32(dim))).astype(np.float32)


def even_mixer_inputs(inp, j, r, h_T, pos):
    w_in = inp["ev_w_in"][j]
    kpe = w_in[:, 640:704]
    kpe_sw = np.concatenate([kpe[:, 32:64], kpe[:, 0:32]], axis=1)
    wlat = np.concatenate([w_in[:, 0:640], kpe, kpe_sw], axis=1)
    hg_parts = []
    for i in range(2):
        gh = 2 * r + i
        hg_parts += [w_in[:, 704 + gh * 128:704 + (gh + 1) * 128], w_in[:, 1216 + gh * 128:1216 + (gh + 1) * 128],
                     w_in[:, 2240 + gh * 128:2240 + (gh + 1) * 128], w_in[:, 1728 + gh * 128:1728 + (gh + 1) * 128]]
    whg = np.concatenate(hg_parts, axis=1)
    w_uq = inp["ev_w_uq"][j]
    w_ukv = inp["ev_w_ukv"][j]
    uq_parts, ukv_parts = [], []
    for i in range(2):
        gh = 2 * r + i
        rp = w_uq[:, gh * 192 + 128:gh * 192 + 192]
        uq_parts += [w_uq[:, gh * 192:gh * 192 + 128], rp, np.concatenate([rp[:, 32:64], rp[:, 0:32]], axis=1)]
        ukv_parts += [w_ukv[:, gh * 256:gh * 256 + 256]]
    wuq = np.concatenate(uq_parts, axis=1)
    wukv = np.concatenate(ukv_parts, axis=1)
    cols = np.zeros((128, 16), dtype=np.float32)
    cols[:, 0:3] = inp["ev_g_q"][j].reshape(3, 128).T
    cols[:, 3:5] = inp["ev_g_kv"][j].reshape(2, 128).T
    f = inv_freq(MLA_ROPE)
    cols[0:32, 5] = f
    cols[32:64, 5] = f
    cols[0:32, 6] = -1.0
    cols[32:64, 6] = 1.0
    for i in range(2):
        gh = 2 * r + i
        cols[:, 7 + i] = inp["ev_lb_logits"][0][gh * 128:(gh + 1) * 128]
        cols[:, 9 + i] = inp["ev_lb_logits"][1][gh * 128:(gh + 1) * 128]
        cols[:, 11 + i] = inp["ev_g_out"][j][gh]
    ones, identf, mask, rmask = const_tables()
    return {"cols": cols, "ones": ones, "identf": identf, "mask": mask, "rmask": rmask,
            "wlat": kchunk(np.ascontiguousarray(wlat)), "whg": kchunk(np.ascontiguousarray(whg)),
            "wuq": kchunk(np.ascontiguousarray(wuq)), "wukv": kchunk(np.ascontiguousarray(wukv))}


def emit_odd_mixer(P, B, G, layer):
    lambda_init = 0.8 - 0.6 * math.exp(-0.3 * layer)
    jj = layer // 2
    P.open_scope()
    cx = Ctx()
    hT_d = G["hT_s"]
    oT_d = G["oT_s"]
    pos_d = G["pos"]
    cols_d = G["colsO%d" % jj]
    lamp_d = G["lamp%d" % jj]
    ones_d = G["ones"]
    w_d = G["wodd%d" % jj]

    hT = P.sb("hT_sb", [128, 8, SEQ], BF16)
    w = P.sb("w_sb", [128, 8, 2560], BF16)
    colsr = [P.sb("cols_sb%d" % r, [128, 16], F32) for r in range(2)]
    cols = colsr[0]
    lamp = P.sb("lamp_sb", [128, 256], F32)
    lam = P.sb("lam", [128, 8], F32)
    cx.ones = P.sb("ones_sb", [128, 128], BF16)
    cx.epsb = P.sb("epsb", [128, 1], F32)
    cx.ki = P.sb("ki", [128, 512], I32)
    cx.posi = P.sb("posi", [128, 512], I32)
    cx.posf = P.sb("posf", [128, 512], F32)
    cx.ang = P.sb("ang", [128, 512], F32)
    cx.rtmp = P.sb("rtmp", [128, 512], F32)
    t1 = cx.posf
    t2 = cx.ang
    oraw = cx.rtmp
    Ct = P.sb("Ct", [128, SEQ], BF16)
    St = P.sb("St", [128, SEQ], BF16)
    qT = P.sb("qT", [128, SEQ], BF16)
    kT = P.sb("kT", [128, SEQ], BF16)
    V = P.sb("V", [128, 32, 128], BF16)
    cx.pT = [P.sb("pT%d" % i, [128, 512], BF16) for i in range(3)]
    osb = [P.sb("osb%d" % i, [128, 512], BF16) for i in range(2)]
    osq = P.sb("osq", [128, 512], BF16)
    rstd = P.sb("rstd", [128, 512], F32)

    for r in range(2):
        P.dma("sp", colsr[r][:, :], cols_d[r])
    P.dma("sp", lamp[:, :], lamp_d[:, :])
    P.dma("sp", cx.ones[:, :], ones_d[:, :])
    P.memset("dve", cx.epsb[:, :], EPS)
    for k in range(8):
        P.dma("sp", hT[:, k, :], hT_d[k])
    P.stt(lamp[:, 0:64], lamp[:, 0:64], 1.0, lamp[:, 64:128], ALU.mult, ALU.mult, accum=lam[:, 0:1])
    P.stt(lamp[:, 128:192], lamp[:, 128:192], 1.0, lamp[:, 192:256], ALU.mult, ALU.mult, accum=lam[:, 1:2])
    P.act(lam[:, 2:3], lam[:, 0:1], AF.Exp)
    P.act(lam[:, 3:4], lam[:, 1:2], AF.Exp)
    P.tt("dve", lam[:, 4:5], lam[:, 3:4], lam[:, 2:3], ALU.subtract)
    P.ts("dve", lam[:, 5:6], lam[:, 4:5], -lambda_init, ALU.add)
    for r in range(2):
        P.ts("dve", colsr[r][:, 6:10], colsr[r][:, 2:6], 1.0 - lambda_init, ALU.mult)
    build_rope_tables(P, cx, pos_d, cols[:, 0:1], cols[:, 1:2], 128, Ct, St)

    cx.pst = [B[0], B[1], B[7]]
    cx.pso = [B[2], B[3]]
    cx.psd = [B[4], B[5]]
    scale = DF_DH ** -0.5
    for r in range(2):
        for k in range(8):
            P.dma("pool", w[:, k, :], w_d[r][k])
        for hh in range(4):
            wo = hh * 640
            for b in range(8):
                sl = slice(b * 512, (b + 1) * 512)
                for (dst, o0) in ((qT, 0), (kT, 256)):
                    for k in range(8):
                        P.matmul(B[6][:, :], w[:, k, wo + o0:wo + o0 + 128], hT[:, k, sl], k == 0, k == 7)
                    for k in range(8):
                        P.matmul(B[7][:, :], w[:, k, wo + o0 + 128:wo + o0 + 256], hT[:, k, sl], k == 0, k == 7)
                    P.tt("dve", t1[:, :], B[6][:, :], Ct[:, sl], ALU.mult)
                    P.tt("dve", t2[:, :], B[7][:, :], St[:, sl], ALU.mult)
                    P.tt("dve", dst[:, sl], t1[:, :], t2[:, :], ALU.add)
                for tt_ in range(4):
                    tok = slice(b * 512 + tt_ * 128, b * 512 + (tt_ + 1) * 128)
                    for k in range(8):
                        mm(P, B[6][:, tt_ * 128:(tt_ + 1) * 128], hT[:, k, tok],
                           w[:, k, wo + 512:wo + 640], (tt_ == 0 and k == 0), k == 7, skip=True)
                P.copy("act", V[:, b * 4:(b + 1) * 4, :], B[6][:, :].rearrange("p (t n) -> p t n", t=4))

            def out_cb(g, hh=hh, r=r):
                o = osb[g % 2]
                P.recip(t1[:, :], cx.psd[0][:, :])
                P.recip(t2[:, :], cx.psd[1][:, :])
                P.tt("dve", t1[:, :], cx.pso[0][:, :], t1[:, :], ALU.mult)
                P.tt("dve", t2[:, :], cx.pso[1][:, :], t2[:, :], ALU.mult)
                P.stt(oraw[:, :], t2[:, :], lam[:, 5:6], t1[:, :], ALU.mult, ALU.add)
                P.act(osq[:, :], oraw[:, :], AF.Square)
                colnorm(P, cx, None, [osq[:, :]], B[6][:, :], 128, rstd[:, :])
                P.stt(o[:, :], oraw[:, :], colsr[r][:, 6 + hh:7 + hh], rstd[:, :], ALU.mult, ALU.mult)
                P.dma("sp", oT_d[4 * r + hh][:, g * 512:(g + 1) * 512], o[:, :])

            pairs = [[(kT[0:64, :], qT[0:64, :])], [(kT[64:128, :], qT[64:128, :])]]
            attention(P, cx, pairs, V, scale, out_cb, ncomp=2)
    P.close_scope()


def odd_mixer_inputs(inp, jj, r, h_T, pos):
    w_in = inp["od_w_in"][jj]

    def sw(wc):
        out = wc.copy()
        for c0 in range(0, wc.shape[1], 64):
            out[:, c0:c0 + 8] = wc[:, c0 + 8:c0 + 16]
            out[:, c0 + 8:c0 + 16] = wc[:, c0:c0 + 8]
        return out

    parts = []
    for hh in range(4):
        gh = 4 * r + hh
        q = w_in[:, gh * 128:(gh + 1) * 128]
        k = w_in[:, 1024 + gh * 128:1024 + (gh + 1) * 128]
        v = w_in[:, 2048 + gh * 128:2048 + (gh + 1) * 128]
        parts += [q, sw(q), k, sw(k), v]
    w = np.concatenate(parts, axis=1)
    cols = np.zeros((128, 16), dtype=np.float32)
    f = inv_freq(DF_ROT)
    for blk in (0, 64):
        cols[blk:blk + 8, 0] = f
        cols[blk + 8:blk + 16, 0] = f
        cols[blk:blk + 8, 1] = -1.0
        cols[blk + 8:blk + 16, 1] = 1.0
    for hh in range(4):
        cols[:, 2 + hh] = inp["od_g_head"][jj][4 * r + hh]
    lamp = np.ascontiguousarray(np.broadcast_to(inp["od_lambda"][jj].reshape(1, 256), (128, 256)))
    ones = np.ones((128, 128), dtype=BF)
    return {"cols": cols, "lamp": lamp, "ones": ones, "w": kchunk(np.ascontiguousarray(w))}


def build_fused():
    nc = bass.Bass("TRN2", target_bir_lowering=False)
    P = Prog(nc)
    G = {}

    def ein(name, shape, dt=F32):
        G[name] = P.dram(name, shape, dt, "ExternalInput")

    ein("x", [SEQ // 128, 128, D_MODEL])
    ein("pos", [128, SEQ], I32)
    ein("ident", [128, 128], BF16)
    ein("ones", [128, 128], BF16)
    ein("identf", [128, 128])
    ein("mask", [64, 512])
    ein("rmask", [128, 512])
    ein("gains", [DEPTH * 6, 128, D_MODEL])
    for l in range(DEPTH):
        for i in range(2):
            ein("wg%d%d" % (l, i), [NJ, 128, 8, 128])
            ein("wu%d%d" % (l, i), [NJ, 128, 8, 128])
            ein("wd%d%d" % (l, i), [NJ, 128, D_MODEL])
        ein("wout%d" % l, [8, 128, D_MODEL])
    for j in range(DEPTH // 2):
        ein("wlat%d" % j, [8, 128, 768])
        ein("whg%d" % j, [2, 8, 128, 1024])
        ein("wuq%d" % j, [2, 3, 128, 512])
        ein("wukv%d" % j, [2, 2, 128, 512])
        ein("colsE%d" % j, [2, 128, 16])
        ein("wodd%d" % j, [2, 8, 128, 2560])
        ein("colsO%d" % j, [2, 128, 16])
        ein("lamp%d" % j, [128, 256])
    for g in range(NGRP):
        G["xs%d" % g] = P.dram("xs%d" % g, [8, 128, D_MODEL], F32, "Internal")
    G["hT_s"] = P.dram("hT_s", [8, 128, SEQ], BF16, "Internal")
    G["oT_s"] = P.dram("oT_s", [8, 128, SEQ], BF16, "Internal")
    G["xo"] = P.dram("xo", [SEQ // 128, 128, D_MODEL], F32, "ExternalOutput")
    B = [P.ps("B%d" % i, [128, 512]) for i in range(8)]
    for step in range(DEPTH + 1):
        emit_token_phase(P, B, G, step)
        if step < DEPTH:
            if step % 2 == 0:
                emit_even_mixer(P, B, G, step // 2)
            else:
                emit_odd_mixer(P, B, G, step)
    P.finalize()
    return nc


def rearr_ffn_w(W):
    return np.ascontiguousarray(W.reshape(8, 128, NJ, 128).transpose(2, 1, 0, 3))


def fused_common_inputs(inp):
    ones, identf, mask, rmask = const_tables()
    C = {"ident": np.eye(128, dtype=BF), "ones": ones, "identf": identf, "mask": mask, "rmask": rmask}
    g = inp["norm_g"].reshape(DEPTH * 6, 1, D_MODEL)
    C["gains"] = np.ascontiguousarray(np.broadcast_to(g, (DEPTH * 6, 128, D_MODEL)))
    for l in range(DEPTH):
        for i in range(2):
            C["wg%d%d" % (l, i)] = rearr_ffn_w(inp["ffn_w_gate"][l, i])
            C["wu%d%d" % (l, i)] = rearr_ffn_w(inp["ffn_w_up"][l, i])
            C["wd%d%d" % (l, i)] = np.ascontiguousarray(inp["ffn_w_down"][l, i].reshape(NJ, 128, D_MODEL))
        w_out = inp["ev_w_out"][l // 2] if l % 2 == 0 else inp["od_w_out"][l // 2]
        C["wout%d" % l] = kchunk(np.ascontiguousarray(w_out))
    for j in range(DEPTH // 2):
        e = [even_mixer_inputs(inp, j, r, None, None) for r in range(2)]
        C["wlat%d" % j] = e[0]["wlat"]
        for nm in ("whg", "wuq", "wukv"):
            C["%s%d" % (nm, j)] = np.ascontiguousarray(np.stack([e[0][nm], e[1][nm]], axis=0))
        C["colsE%d" % j] = np.ascontiguousarray(np.stack([e[0]["cols"], e[1]["cols"]], axis=0))
        o = [odd_mixer_inputs(inp, j, r, None, None) for r in range(2)]
        C["wodd%d" % j] = np.ascontiguousarray(np.stack([o[0]["w"], o[1]["w"]], axis=0))
        C["colsO%d" % j] = np.ascontiguousarray(np.stack([o[0]["cols"], o[1]["cols"]], axis=0))
        C["lamp%d" % j] = o[0]["lamp"]
    return C


_NC_CACHE = {}


def kernel(x, positions, norm_g, ffn_w_gate, ffn_w_up, ffn_w_down, ev_w_in, ev_g_q, ev_w_uq, ev_g_kv,
           ev_w_ukv, ev_lb_logits, ev_g_out, ev_w_out, od_w_in, od_lambda, od_g_head, od_w_out):
    inp = dict(x=x, positions=positions, norm_g=norm_g, ffn_w_gate=ffn_w_gate, ffn_w_up=ffn_w_up,
               ffn_w_down=ffn_w_down, ev_w_in=ev_w_in, ev_g_q=ev_g_q, ev_w_uq=ev_w_uq, ev_g_kv=ev_g_kv,
               ev_w_ukv=ev_w_ukv, ev_lb_logits=ev_lb_logits, ev_g_out=ev_g_out, ev_w_out=ev_w_out,
               od_w_in=od_w_in, od_lambda=od_lambda, od_g_head=od_g_head, od_w_out=od_w_out)
    inp = {k: np.asarray(v) for k, v in inp.items()}
    if "nc" not in _NC_CACHE:
        _NC_CACHE["nc"] = build_fused()
    nc = _NC_CACHE["nc"]
    C = fused_common_inputs(inp)
    cores = list(range(NCORES))
    in_maps = []
    for c in cores:
        b = c // 2
        m = dict(C)
        m["x"] = np.ascontiguousarray(inp["x"][b].astype(np.float32, copy=False)).reshape(SEQ // 128, 128, D_MODEL)
        m["pos"] = np.ascontiguousarray(np.broadcast_to(inp["positions"][b].astype(np.int32)[None, :], (128, SEQ)))
        in_maps.append(m)
    res = run_bass_kernel_spmd(nc, in_maps, core_ids=cores).results
    out = np.zeros((BATCH, SEQ, D_MODEL), dtype=np.float32)
    for b in range(BATCH):
        out[b] = np.asarray(res[2 * b]["xo"]).reshape(SEQ, D_MODEL)
    return out
```

```python
import math
import numpy as np
import ml_dtypes
import concourse.bass as bass
import concourse.mybir as mybir
from concourse.bass_utils import run_bass_kernel_spmd

F32 = mybir.dt.float32
BF16 = mybir.dt.bfloat16
I32 = mybir.dt.int32
AF = mybir.ActivationFunctionType
ALU = mybir.AluOpType
AX = mybir.AxisListType

D_MODEL = 1024
BATCH = 4
SEQ = 4096
DEPTH = 4
CHUNK = 64
ROPE_THETA = 500000.0
EPS = 1e-6
TINY = 1e-30
D_FF = 2816
NJ = D_FF // 128
MLA_NOPE, MLA_ROPE, MLA_V, MLA_Q_RANK, MLA_KV_RANK = 128, 64, 128, 384, 256
DF_DH = 64
DF_ROT = 16
NCORES = 8
TOK = SEQ // 2
NTT = TOK // 128


class View:
    __slots__ = ("tile", "ap")

    def __init__(self, tile, ap):
        self.tile = tile
        self.ap = ap

    def __getitem__(self, idx):
        return View(self.tile, self.ap[idx])

    def rearrange(self, s, **kw):
        return View(self.tile, self.ap.rearrange(s, **kw))

    def bitcast(self, dt):
        return View(self.tile, self.ap.bitcast(dt))


class Tile:
    def __init__(self, name, base_ap):
        self.name = name
        self.base = base_ap
        self.last_w = None
        self.readers = []

    def __getitem__(self, idx):
        return View(self, self.base[idx])

    def v(self):
        return View(self, self.base)


class Op:
    __slots__ = ("eng", "fn", "reads", "writes", "dma", "deps", "signal", "seq",
                 "sem", "val", "prev_dma")

    def __init__(self, eng, fn, reads, writes, dma):
        self.eng = eng
        self.fn = fn
        self.reads = reads
        self.writes = writes
        self.dma = dma
        self.deps = []
        self.signal = False
        self.seq = 0
        self.sem = None
        self.val = 0
        self.prev_dma = None


class Prog:
    ENGS = ("pe", "act", "dve", "pool", "sp")
    NDMASEM = 12

    def __init__(self, nc):
        self.nc = nc
        self.ops = []
        self.n_sb = 0
        self.stack = None
        self.uid = 0

    def open_scope(self):
        import contextlib
        self.stack = contextlib.ExitStack()

    def close_scope(self):
        self.stack.close()
        self.stack = None
        self.ops.append("BARRIER")

    def sb(self, name, shape, dtype):
        if self.stack is not None:
            self.uid += 1
            h = self.stack.enter_context(self.nc.sbuf_tensor("sb%d_%s" % (self.uid, name), list(shape), dtype))
        else:
            h = self.nc.alloc_sbuf_tensor("sb_" + name, list(shape), dtype)
        idx = tuple(slice(None) for _ in shape)
        return Tile(name, h[idx])

    def ps(self, name, shape, dtype=F32):
        h = self.nc.alloc_psum_tensor("ps_" + name, list(shape), dtype)
        idx = tuple(slice(None) for _ in shape)
        return Tile(name, h[idx])

    def dram(self, name, shape, dtype, kind):
        h = self.nc.dram_tensor(name, list(shape), dtype, kind=kind)
        return Tile(name, h.ap())

    def add(self, eng, fn, reads, writes, dma=False):
        rt = []
        for r in reads:
            if isinstance(r, View) and r.tile not in rt:
                rt.append(r.tile)
        wt = []
        for w in writes:
            if isinstance(w, View) and w.tile not in wt:
                wt.append(w.tile)
        op = Op(eng, fn, rt, wt, dma)
        self.ops.append(op)
        return op

    def dma(self, q, out, in_):
        return self.add(q, lambda e: e.dma_start(out=out.ap, in_=in_.ap), [in_], [out], dma=True)

    def matmul(self, out, lhsT, rhs, start, stop):
        return self.add("pe", lambda e: e.matmul(out.ap, lhsT.ap, rhs.ap, start=start, stop=stop),
                        [lhsT, rhs], [out])

    def transpose(self, out, in_, ident):
        return self.add("pe", lambda e: e.transpose(out.ap, in_.ap, ident.ap), [in_, ident], [out])

    def act(self, out, in_, func, bias=None, scale=None, accum=None, eng="act"):
        reads = [in_]
        writes = [out]
        kw = {}
        if bias is not None:
            if isinstance(bias, View):
                reads.append(bias)
                kw["bias"] = bias.ap
            else:
                kw["bias"] = float(bias)
        if scale is not None:
            if isinstance(scale, View):
                reads.append(scale)
                kw["scale"] = scale.ap
            else:
                kw["scale"] = float(scale)
        if accum is not None:
            writes.append(accum)
            kw["accum_out"] = accum.ap
        return self.add(eng, lambda e: e.activation(out.ap, in_.ap, func, **kw), reads, writes)

    def tt(self, eng, out, in0, in1, op):
        return self.add(eng, lambda e: e.tensor_tensor(out.ap, in0.ap, in1.ap, op), [in0, in1], [out])

    def ts(self, eng, out, in0, s1, op0, s2=None, op1=None, accum=None):
        reads = [in0]
        a1 = s1.ap if isinstance(s1, View) else float(s1)
        if isinstance(s1, View):
            reads.append(s1)
        a2 = None
        if s2 is not None:
            a2 = s2.ap if isinstance(s2, View) else float(s2)
            if isinstance(s2, View):
                reads.append(s2)
        writes = [out]
        kw = {}
        if accum is not None:
            writes.append(accum)
            kw["accum_out"] = accum.ap
        o1 = op1 if op1 is not None else ALU.bypass
        return self.add(eng, lambda e: e.tensor_scalar(out.ap, in0.ap, a1, a2, op0, o1, **kw), reads, writes)

    def stt(self, out, in0, scalar, in1, op0, op1, accum=None):
        reads = [in0, in1]
        a = scalar.ap if isinstance(scalar, View) else float(scalar)
        if isinstance(scalar, View):
            reads.append(scalar)
        writes = [out]
        kw = {}
        if accum is not None:
            writes.append(accum)
            kw["accum_out"] = accum.ap
        return self.add("dve", lambda e: e.scalar_tensor_tensor(out.ap, in0.ap, a, in1.ap, op0, op1, **kw),
                        reads, writes)

    def copy(self, eng, out, in_):
        if eng == "act":
            return self.add("act", lambda e: e.copy(out.ap, in_.ap), [in_], [out])
        return self.add(eng, lambda e: e.tensor_copy(out.ap, in_.ap), [in_], [out])

    def memset(self, eng, out, val):
        return self.add(eng, lambda e: e.memset(out.ap, val), [], [out])

    def recip(self, out, in_):
        return self.add("dve", lambda e: e.reciprocal(out.ap, in_.ap), [in_], [out])

    def finalize(self):
        nc = self.nc
        ops = self.ops
        real = []
        fence = []
        last_eng = {}
        dma_q = {}
        for op in ops:
            if isinstance(op, str):
                fence = [o for o in last_eng.values()]
                for q, lst in dma_q.items():
                    fence.extend(lst[-self.NDMASEM:])
                continue
            real.append(op)
            if op.dma:
                dma_q.setdefault(op.eng, []).append(op)
            else:
                last_eng[op.eng] = op
            op.deps.extend(fence)
        ops = real
        self.ops = real
        for op in ops:
            deps = list(op.deps)
            op.deps = []
            raw = set()
            for t in op.reads:
                if t.last_w is not None:
                    deps.append(t.last_w)
                    raw.add(id(t.last_w))
            for t in op.writes:
                if t.last_w is not None:
                    deps.append(t.last_w)
                deps.extend(t.readers)
            seen = set()
            for d in deps:
                if d is op or id(d) in seen:
                    continue
                seen.add(id(d))
                if (not d.dma) and d.eng == op.eng and op.eng == "pe":
                    continue
                op.deps.append(d)
            for t in op.reads:
                t.readers.append(op)
            for t in op.writes:
                t.last_w = op
                t.readers = []
        for op in ops:
            for d in op.deps:
                if not d.dma:
                    d.signal = True
        cnt = {e: 0 for e in self.ENGS}
        dcnt = {e: 0 for e in self.ENGS}
        dma_hist = {e: [] for e in self.ENGS}
        esem = {}
        dsem = {}
        for e in self.ENGS:
            esem[e] = nc.alloc_semaphore("s_" + e)
        for op in ops:
            if op.dma:
                q = op.eng
                if q not in dsem:
                    dsem[q] = [nc.alloc_semaphore("d_%s_%d" % (q, i)) for i in range(self.NDMASEM)]
                i = dcnt[q]
                dcnt[q] += 1
                op.sem = dsem[q][i % self.NDMASEM]
                op.val = 16 * (i // self.NDMASEM + 1)
                if i >= self.NDMASEM:
                    op.prev_dma = dma_hist[q][i - self.NDMASEM]
                dma_hist[q].append(op)
            elif op.signal:
                cnt[op.eng] += 1
                op.seq = cnt[op.eng]
                op.sem = esem[op.eng]
                op.val = op.seq
        per_eng = {e: [] for e in self.ENGS}
        for op in ops:
            per_eng[op.eng].append(op)
        last_dma = {q: h for q, h in dma_hist.items() if h}

        def emit(eng_name, e):
            waited = {}
            for op in per_eng[eng_name]:
                need = {}
                dl = list(op.deps)
                if op.prev_dma is not None:
                    dl.append(op.prev_dma)
                for d in dl:
                    k = d.sem.num
                    if waited.get(k, 0) >= d.val:
                        continue
                    if k not in need or need[k][1] < d.val:
                        need[k] = (d.sem, d.val)
                for k, (s, v) in need.items():
                    e.wait_ge(s, v)
                    waited[k] = v
                ins = op.fn(e)
                if op.dma:
                    ins.then_inc(op.sem, 16)
                elif op.signal:
                    ins.then_inc(op.sem, 1)
            if eng_name in last_dma:
                fin = {}
                for d in last_dma[eng_name]:
                    k = d.sem.num
                    if k not in fin or fin[k][1] < d.val:
                        fin[k] = (d.sem, d.val)
                for k, (s, v) in fin.items():
                    if waited.get(k, 0) < v:
                        e.wait_ge(s, v)

        with nc.Block() as block:
            @block.tensor
            def _(e):
                emit("pe", e)

            @block.scalar
            def _(e):
                emit("act", e)

            @block.vector
            def _(e):
                emit("dve", e)

            @block.gpsimd
            def _(e):
                emit("pool", e)

            @block.sync
            def _(e):
                emit("sp", e)


class Ctx:
    pass


def rms_stats(P, cx, src, d, rstd):
    P.act(cx.junk[:, 0:d], src, AF.Square, accum=cx.ss[:, 0:1])
    P.act(cx.ss[:, 1:2], cx.ss[:, 0:1], AF.Sqrt, bias=cx.epsb[:, 0:1], scale=1.0 / d)
    P.recip(rstd, cx.ss[:, 1:2])


def norm_T_stage(P, cx, xt, g, sink):
    nt = len(xt)

    def stats(t):
        rs = cx.rstdN[:, t:t + 1]
        P.act(cx.junk[:, :], xt[t][:, :], AF.Square, accum=cx.ssN[:, 2 * t:2 * t + 1])
        P.act(cx.ssN[:, 2 * t + 1:2 * t + 2], cx.ssN[:, 2 * t:2 * t + 1], AF.Sqrt,
              bias=cx.epsb[:, 0:1], scale=1.0 / D_MODEL)
        P.recip(rs, cx.ssN[:, 2 * t + 1:2 * t + 2])
        P.stt(cx.hnb[t % 2][:, :], xt[t][:, :], rs, g[:, :], ALU.mult, ALU.mult)

    def trans(t):
        pt = cx.ptr[t % 2]
        hn = cx.hnb[t % 2]
        for kc in range(8):
            P.transpose(pt[:, kc * 128:(kc + 1) * 128], hn[:, kc * 128:(kc + 1) * 128], cx.ident[:, :])
        sink(t, pt)

    for t in range(nt + 1):
        if t < nt:
            stats(t)
        if t >= 1:
            trans(t - 1)


STAGE = 9


def ffn_group(P, cx, xt, W, gpre, gpost, name):
    nt = len(xt)
    nh = nt // 4
    def sink(t, pt):
        P.copy("act", cx.hT[t // 4][:, :, (t % 4) * 128:(t % 4 + 1) * 128],
               pt.rearrange("p (k n) -> p k n", k=8))

    norm_T_stage(P, cx, xt, gpre, sink)
    if STAGE < 2:
        return
    for j in range(NJ):
        wg = cx.wg[j % len(cx.wg)]
        wu = cx.wu[j % len(cx.wu)]
        P.dma("pool", wg[:, :, :], W["wg"][j])
        P.dma("pool", wu[:, :, :], W["wu"][j])
        P.dma("pool", cx.wd[j][:, :], W["wd_d"][j])
        for h in range(nh):
            pg = cx.pg[(j * nh + h) % 2]
            pu = cx.pu[(j * nh + h) % 2]
            for kc in range(8):
                P.matmul(pg[:, :], wg[:, kc, :], cx.hT[h][:, kc, :], kc == 0, kc == 7)
            for kc in range(8):
                P.matmul(pu[:, :], wu[:, kc, :], cx.hT[h][:, kc, :], kc == 0, kc == 7)
            sg = cx.sg[(j * nh + h) % 2]
            P.act(sg[:, :], pg[:, :], AF.Silu)
            P.tt("dve", cx.aT[j][:, h * 512:(h + 1) * 512], sg[:, :], pu[:, :], ALU.mult)
    if STAGE < 3:
        return
    for t in range(nt):
        y = cx.y[t % 2]
        for n in range(2):
            pd = cx.pd[(t * 2 + n) % 2]
            for j in range(NJ):
                P.matmul(pd[:, :], cx.aT[j][:, t * 128:(t + 1) * 128],
                         W["wd"][j][:, n * 512:(n + 1) * 512], j == 0, j == NJ - 1)
            P.copy("act", y[:, n * 512:(n + 1) * 512], pd[:, :])
        norm_residual(P, cx, xt[t], y, gpost, 0.5)


def norm_residual(P, cx, x, y, g, alpha):
    rms_stats(P, cx, y[:, :], D_MODEL, cx.rstd[:, 1:2])
    P.stt(y[:, :], y[:, :], cx.rstd[:, 1:2], g[:, :], ALU.mult, ALU.mult)
    P.stt(x[:, :], y[:, :], float(alpha), x[:, :], ALU.mult, ALU.add)


def load_wd(P, cx, wd_dram):
    for j in range(NJ):
        P.dma("pool", cx.wd[j][:, :], wd_dram[j])


NGRP = SEQ // 1024


def emit_token_phase(P, B, G, step):
    do_post = step > 0
    do_pre = step < DEPTH
    P.open_scope()
    cx = Ctx()
    cx.ident = P.sb("ident_sb", [128, 128], BF16)
    cx.ss = P.sb("ss", [128, 2], F32)
    cx.rstd = P.sb("rstd", [128, 2], F32)
    cx.epsb = P.sb("epsb", [128, 1], F32)
    cx.hnb = [P.sb("hn%d" % i, [128, D_MODEL], BF16) for i in range(2)]
    cx.ssN = P.sb("ssN", [128, 32], F32)
    cx.rstdN = P.sb("rstdN", [128, 16], F32)
    cx.hT = [P.sb("hT%d" % i, [128, 8, 512], BF16) for i in range(2)]
    cx.wg = [P.sb("wg%d" % i, [128, 8, 128], BF16) for i in range(3)]
    cx.wu = [P.sb("wu%d" % i, [128, 8, 128], BF16) for i in range(3)]
    cx.wd = [P.sb("wd%d" % j, [128, D_MODEL], BF16) for j in range(NJ)]
    cx.aT = [P.sb("aT%d" % j, [128, 1024], BF16) for j in range(NJ)]
    cx.sg = [P.sb("sg%d" % i, [128, 512], F32) for i in range(2)]
    cx.junk = cx.sg[0][:, :].bitcast(BF16)
    cx.y = [P.sb("y%d" % i, [128, D_MODEL], F32) for i in range(2)]
    gt = [P.sb("g%d" % i, [128, D_MODEL], F32) for i in range(6)]
    xt = [P.sb("x%d" % i, [128, D_MODEL], F32) for i in range(8)]
    if do_post:
        wout = [P.sb("wout%d" % k, [128, D_MODEL], BF16) for k in range(8)]
    if do_pre:
        hTo = [P.sb("hTo0", [128, 8, 128], BF16),
               cx.sg[1][:, :].bitcast(BF16).rearrange("p (k n) -> p k n", k=8)]
    cx.pg = [B[0], B[1]]
    cx.pu = [B[2], B[3]]
    cx.pd = [B[4], B[5]]
    cx.ptr = [B[6][:, :].bitcast(BF16), B[7][:, :].bitcast(BF16)]

    P.dma("sp", cx.ident[:, :], G["ident"][:, :])
    P.memset("dve", cx.epsb[:, :], EPS)
    Wpost = Wpre = None
    if do_post:
        l = step - 1
        for i in range(3):
            P.dma("sp", gt[i][:, :], G["gains"][l * 6 + 3 + i])
        for k in range(8):
            P.dma("pool", wout[k][:, :], G["wout%d" % l][k])
        Wpost = {"wg": G["wg%d1" % l], "wu": G["wu%d1" % l], "wd": cx.wd, "wd_d": G["wd%d1" % l]}
    if do_pre:
        l = step
        for i in range(3):
            P.dma("sp", gt[3 + i][:, :], G["gains"][l * 6 + i])
        Wpre = {"wg": G["wg%d0" % l], "wu": G["wu%d0" % l], "wd": cx.wd, "wd_d": G["wd%d0" % l]}

    for grp in range(NGRP):
        for t in range(8):
            src = G["x"][grp * 8 + t] if step == 0 else G["xs%d" % grp][t]
            P.dma("sp", xt[t][:, :], src)
        if do_post:
            l = step - 1
            for h2 in range(2):
                P.dma("sp", cx.hT[h2][:, :, :],
                      G["oT_s"][:, :, grp * 1024 + h2 * 512:grp * 1024 + (h2 + 1) * 512].rearrange("k p n -> p k n"))
            for t in range(8):
                y = cx.y[t % 2]
                for n in range(2):
                    pd = cx.pd[(t * 2 + n) % 2]
                    for k in range(8):
                        P.matmul(pd[:, :], cx.hT[t // 4][:, k, (t % 4) * 128:(t % 4 + 1) * 128],
                                 wout[k][:, n * 512:(n + 1) * 512], k == 0, k == 7)
                    P.copy("act", y[:, n * 512:(n + 1) * 512], pd[:, :])
                norm_residual(P, cx, xt[t], y, gt[0], 1.0)
            ffn_group(P, cx, xt, Wpost, gt[1], gt[2], "f2")
        if do_pre:
            l = step
            ffn_group(P, cx, xt, Wpre, gt[3], gt[4], "f1")
            def hsink(t, pt, grp=grp):
                ho = hTo[t % 2]
                P.copy("act", ho[:, :, :], pt.rearrange("p (k n) -> p k n", k=8))
                tok0 = grp * 1024 + t * 128
                P.dma("sp", G["hT_s"][:, :, tok0:tok0 + 128].rearrange("k p n -> p k n"), ho[:, :, :])

            norm_T_stage(P, cx, xt, gt[5], hsink)
        for t in range(8):
            dst = G["xo"][grp * 8 + t] if step == DEPTH else G["xs%d" % grp][t]
            P.dma("sp", dst, xt[t][:, :])
    P.close_scope()


def mm(P, out, lhsT, rhs, start, stop, skip=False):
    if skip:
        return P.add("pe", lambda e: e.matmul(out.ap, lhsT.ap, rhs.ap, start=start, stop=stop,
                                              skip_group_check=True), [lhsT, rhs], [out])
    return P.matmul(out, lhsT, rhs, start, stop)


def build_rope_tables(P, cx, pos_d, freq_col, sgn_col, nrows, Ct, St):
    PI = math.pi
    PIS = 3.1415925
    n = nrows
    for b in range(8):
        sl = slice(b * 512, (b + 1) * 512)
        P.dma("sp", cx.posi[0:n, :], pos_d[0:n, sl])
        P.copy("dve", cx.posf[0:n, :], cx.posi[0:n, :])
        P.ts("dve", cx.ang[0:n, :], cx.posf[0:n, :], freq_col, ALU.mult)
        for shift, dst, sg in ((0.0, St, sgn_col), (0.5 * PI, Ct, None)):
            if shift != 0.0:
                P.ts("dve", cx.ang[0:n, :], cx.ang[0:n, :], shift, ALU.add)
            P.ts("dve", cx.rtmp[0:n, :], cx.ang[0:n, :], 1.0 / (2 * PI), ALU.mult)
            P.copy("dve", cx.ki[0:n, :], cx.rtmp[0:n, :])
            P.copy("dve", cx.rtmp[0:n, :], cx.ki[0:n, :])
            P.stt(cx.rtmp[0:n, :], cx.rtmp[0:n, :], -2 * PI, cx.ang[0:n, :], ALU.mult, ALU.add)
            P.ts("dve", cx.posf[0:n, :], cx.rtmp[0:n, :], PI, ALU.is_gt, 2 * PI, ALU.mult)
            P.tt("dve", cx.rtmp[0:n, :], cx.rtmp[0:n, :], cx.posf[0:n, :], ALU.subtract)
            P.ts("dve", cx.rtmp[0:n, :], cx.rtmp[0:n, :], PIS, ALU.min, -PIS, ALU.max)
            if sg is not None:
                P.act(cx.rtmp[0:n, :], cx.rtmp[0:n, :], AF.Sin)
                P.ts("dve", dst[0:n, sl], cx.rtmp[0:n, :], sg, ALU.mult)
            else:
                P.act(dst[0:n, sl], cx.rtmp[0:n, :], AF.Sin)


def attention(P, cx, qk_pairs, V, scale, out_cb, ncomp=1):
    its = []
    for g in range(8):
        nk = 4 * (g + 1)
        for kt in range(nk):
            for c in range(ncomp):
                its.append((g, kt, c, nk))
    nb = len(cx.pst)

    def front(i):
        g, kt, c, nk = its[i]
        a = kt - 4 * g
        q0 = 128 * a if a > 0 else 0
        st = cx.pst[i % nb]
        pT = cx.pT[i % nb]
        prs = qk_pairs[c]
        for n_, (kT, qT) in enumerate(prs):
            P.matmul(st[:, q0:512], kT[:, kt * 128:(kt + 1) * 128],
                     qT[:, g * 512 + q0:(g + 1) * 512], n_ == 0, n_ == len(prs) - 1)
        P.act(pT[:, q0:512], st[:, q0:512], AF.Exp, scale=scale)
        if a >= 0:
            P.memset("dve", pT[64:128, q0:q0 + 64], 0.0)

    def back(i):
        g, kt, c, nk = its[i]
        a = kt - 4 * g
        q0 = 128 * a if a > 0 else 0
        pT = cx.pT[i % nb]
        mm(P, cx.pso[c][:, q0:512], V[:, kt, :], pT[:, q0:512], kt == 0, kt == nk - 1, skip=True)
        mm(P, cx.psd[c][:, q0:512], cx.ones[:, :], pT[:, q0:512], kt == 0, kt == nk - 1, skip=True)
        if kt == nk - 1 and c == ncomp - 1:
            out_cb(g)

    SK = nb - 1
    n = len(its)
    for i in range(n + SK):
        if i < n:
            front(i)
        if i - SK >= 0:
            back(i - SK)


def colnorm(P, cx, raws, sqs, ps_ss, d, rstd_out):
    for i, sq in enumerate(sqs):
        P.matmul(ps_ss, cx.ones[:, :], sq, i == 0, i == len(sqs) - 1)
    P.act(rstd_out, ps_ss, AF.Sqrt, bias=cx.epsb[:, 0:1], scale=1.0 / d)
    P.recip(rstd_out, rstd_out)


def emit_even_mixer(P, B, G, j):
    P.open_scope()
    cx = Ctx()
    hT_d = G["hT_s"]
    oT_d = G["oT_s"]
    pos_d = G["pos"]
    cols_d = G["colsE%d" % j]
    ones_d, identf_d, mask_d, rmask_d = G["ones"], G["identf"], G["mask"], G["rmask"]
    wlat_d, whg_d, wuq_d, wukv_d = G["wlat%d" % j], G["whg%d" % j], G["wuq%d" % j], G["wukv%d" % j]

    hT = P.sb("hT_sb", [128, 8, SEQ], BF16)
    colsr = [P.sb("cols_sb%d" % r, [128, 16], F32) for r in range(2)]
    cols = colsr[0]
    cx.ones = P.sb("ones_sb", [128, 128], BF16)
    identf = P.sb("identf_sb", [128, 128], F32)
    mask = P.sb("mask_sb", [64, 512], F32)
    rmask = P.sb("rmask_sb", [128, 512], F32)
    wlat = P.sb("wlat_sb", [128, 8, 768], BF16)
    whg = P.sb("whg_sb", [128, 8, 1024], BF16)
    wuq = P.sb("wuq_sb", [128, 3, 512], BF16)
    wukv = P.sb("wukv_sb", [128, 2, 512], BF16)
    cx.epsb = P.sb("epsb", [128, 1], F32)
    cx.ki = P.sb("ki", [128, 512], I32)
    cx.posi = P.sb("posi", [128, 512], I32)
    sig = P.sb("sig", [128, 512], F32)
    ff = P.sb("ff", [128, 512], F32)
    kk = P.sb("kk", [128, 512], F32)
    bcum = P.sb("bcum", [128, 512], F32)
    eb = P.sb("eb", [128, 512], F32)
    enb = P.sb("enb", [128, 512], F32)
    kdT = P.sb("kdT", [128, 512], F32)
    gate = P.sb("gate", [128, 512], F32)
    cx.posf = sig
    cx.ang = ff
    cx.rtmp = kk
    Ct = P.sb("Ct", [64, SEQ], BF16)
    St = P.sb("St", [64, SEQ], BF16)
    cqn = P.sb("cqn", [128, 3, SEQ], BF16)
    ckvn = P.sb("ckvn", [128, 2, SEQ], BF16)
    kpeT = P.sb("kpeT", [64, SEQ], BF16)
    raw = [bcum, eb, enb, kdT, gate]
    sq = [P.sb("sq%d" % i, [128, 512], BF16) for i in range(5)]
    rstdA = P.sb("rstdA", [128, 512], F32)
    rstdB = ff
    t1 = kk
    t2 = sig
    qnT = hT[:, 0, :]
    knT = hT[:, 1, :]
    qrT = hT[0:64, 2, :]
    V = hT[:, 3, :].rearrange("p (t n) -> p t n", t=32)
    cx.pT = [P.sb("pT%d" % i, [128, 512], BF16) for i in range(3)]
    osb = [P.sb("osb%d" % i, [128, 512], BF16) for i in range(2)]

    for r in range(2):
        P.dma("sp", colsr[r][:, :], cols_d[r])
    P.dma("sp", cx.ones[:, :], ones_d[:, :])
    P.dma("sp", identf[:, :], identf_d[:, :])
    P.dma("sp", mask[:, :], mask_d[:, :])
    P.dma("sp", rmask[:, :], rmask_d[:, :])
    P.memset("dve", cx.epsb[:, :], EPS)
    P.dma("pool", wlat[:, :, :], wlat_d[:, :, :].rearrange("k p n -> p k n"))
    for k in range(8):
        P.dma("sp", hT[:, k, :], hT_d[k])
    build_rope_tables(P, cx, pos_d, cols[0:64, 5:6], cols[0:64, 6:7], 64, Ct, St)

    for b in range(8):
        sl = slice(b * 512, (b + 1) * 512)
        for oc in range(5):
            pb = B[oc % 4]
            for k in range(8):
                P.matmul(pb[:, :], wlat[:, k, oc * 128:(oc + 1) * 128], hT[:, k, sl], k == 0, k == 7)
            P.copy("act", raw[oc][:, :], pb[:, :])
            P.act(sq[oc][:, :], pb[:, :], AF.Square)
        colnorm(P, cx, raw[0:3], [s[:, :] for s in sq[0:3]], B[4][:, :], MLA_Q_RANK, rstdA[:, :])
        colnorm(P, cx, raw[3:5], [s[:, :] for s in sq[3:5]], B[5][:, :], MLA_KV_RANK, rstdB[:, :])
        for c in range(3):
            P.stt(cqn[:, c, sl], raw[c][:, :], cols[:, c:c + 1], rstdA[:, :], ALU.mult, ALU.mult)
        for c in range(2):
            P.stt(ckvn[:, c, sl], raw[3 + c][:, :], cols[:, 3 + c:4 + c], rstdB[:, :], ALU.mult, ALU.mult)
        for k in range(8):
            P.matmul(B[6][0:64, :], wlat[:, k, 640:704], hT[:, k, sl], k == 0, k == 7)
        for k in range(8):
            P.matmul(B[7][0:64, :], wlat[:, k, 704:768], hT[:, k, sl], k == 0, k == 7)
        P.tt("dve", t1[0:64, :], B[6][0:64, :], Ct[:, sl], ALU.mult)
        P.tt("dve", t2[0:64, :], B[7][0:64, :], St[:, sl], ALU.mult)
        P.tt("dve", kpeT[:, sl], t1[0:64, :], t2[0:64, :], ALU.add)

    lbc = P.sb("lbc", [128, 4], F32)
    state = P.sb("state", [128, 128], F32)
    state_bf = P.sb("state_bf", [128, 128], BF16)
    qd = P.sb("qd", [128, 512], BF16)
    ktl = P.sb("ktl", [128, 512], BF16)
    kdec = P.sb("kdec", [64, 8, 128], BF16)
    V64 = P.sb("V64", [64, 8, 128], BF16)
    attn = P.sb("attn", [64, 512], BF16)
    oraw = bcum
    osq = P.sb("osq", [128, 512], BF16)
    for r in range(2):
        P.dma("pool", whg[:, :, :], whg_d[r].rearrange("k p n -> p k n"))
        for h in range(2):
            wofs = h * 512
            if j == 0:
                P.memset("dve", lbc[:, 0:1], 0.0)
            else:
                P.tt("dve", lbc[:, 3:4], colsr[r][:, 9 + h:10 + h], colsr[r][:, 7 + h:8 + h], ALU.subtract)
                P.act(lbc[:, 0:1], lbc[:, 3:4], AF.Sigmoid)
            P.ts("dve", lbc[:, 1:2], lbc[:, 0:1], -1.0, ALU.mult, 1.0, ALU.add)
            P.ts("dve", lbc[:, 2:3], lbc[:, 1:2], -1.0, ALU.mult)
            P.memset("dve", state[:, :], 0.0)
            P.memset("dve", state_bf[:, :], 0.0)
            for b in range(8):
                sl = slice(b * 512, (b + 1) * 512)
                for i, pb in enumerate((B[0], B[1], B[2])):
                    for k in range(8):
                        P.matmul(pb[:, :], whg[:, k, wofs + i * 128:wofs + (i + 1) * 128], hT[:, k, sl], k == 0, k == 7)
                for c in range(8):
                    pb = B[3 + c // 4]
                    tok = slice(b * 512 + c * 64, b * 512 + (c + 1) * 64)
                    for k in range(8):
                        mm(P, pb[0:64, (c % 4) * 128:(c % 4 + 1) * 128], hT[:, k, tok],
                           whg[:, k, wofs + 384:wofs + 512], (c % 4 == 0 and k == 0), k == 7, skip=True)
                P.copy("act", V64[:, 0:4, :], B[3][0:64, :].rearrange("p (c n) -> p c n", c=4))
                P.copy("act", V64[:, 4:8, :], B[4][0:64, :].rearrange("p (c n) -> p c n", c=4))
                P.act(sig[:, :], B[1][:, :], AF.Sigmoid)
                P.act(gate[:, :], B[2][:, :], AF.Silu)
                P.ts("dve", ff[:, :], sig[:, :], lbc[:, 1:2], ALU.mult, lbc[:, 0:1], ALU.add)
                P.ts("dve", ff[:, :], ff[:, :], TINY, ALU.max)
                P.act(ff[:, :], ff[:, :], AF.Ln)
                P.ts("dve", kk[:, :], sig[:, :], lbc[:, 2:3], ALU.mult, lbc[:, 1:2], ALU.add)
                P.add("dve", lambda e: e.tensor_tensor_scan(bcum[:, :].ap, rmask[:, :].ap, ff[:, :].ap, 0.0,
                                                            ALU.mult, ALU.add), [rmask[:, :], ff[:, :]], [bcum[:, :]])
                P.act(eb[:, :], bcum[:, :], AF.Exp)
                P.act(enb[:, :], bcum[:, :], AF.Exp, scale=-1.0)
                P.tt("dve", qd[:, :], B[0][:, :], eb[:, :], ALU.mult)
                P.tt("dve", ktl[:, :], kk[:, :], enb[:, :], ALU.mult)
                P.tt("dve", kdT[:, :], kk[:, :], enb[:, :], ALU.mult)
                for c in range(8):
                    cs = slice(c * 64, (c + 1) * 64)
                    P.ts("dve", kdT[:, cs], kdT[:, cs], eb[:, c * 64 + 63:c * 64 + 64], ALU.mult)
                for half in range(2):
                    pb = B[5]
                    for c4 in range(4):
                        c = half * 4 + c4
                        P.transpose(pb[0:64, c4 * 128:(c4 + 1) * 128], kdT[:, c * 64:(c + 1) * 64], identf[:, :])
                    P.copy("act", kdec[:, half * 4:(half + 1) * 4, :], pb[0:64, :].rearrange("p (c n) -> p c n", c=4))
                for c in range(8):
                    cs = slice(c * 64, (c + 1) * 64)
                    mm(P, B[6][0:64, cs], ktl[:, cs], qd[:, cs], c == 0, True, skip=True)
                P.tt("dve", attn[:, :], B[6][0:64, :], mask[:, :], ALU.mult)
                for c in range(8):
                    cs = slice(c * 64, (c + 1) * 64)
                    mm(P, B[7][:, cs], V64[:, c, :], attn[:, cs], c == 0, False, skip=True)
                    mm(P, B[7][:, cs], state_bf[:, :], qd[:, cs], False, True, skip=True)
                    su = B[3] if c % 2 == 0 else B[4]
                    P.matmul(su[:, 0:128], kdec[:, c, :], V64[:, c, :], True, True)
                    P.stt(state[:, :], state[:, :], eb[:, c * 64 + 63:c * 64 + 64], su[:, 0:128], ALU.mult, ALU.add)
                    P.copy("act", state_bf[:, :], state[:, :])
                P.copy("act", oraw[:, :], B[7][:, :])
                P.act(osq[:, :], B[7][:, :], AF.Square)
                colnorm(P, cx, None, [osq[:, :]], B[6][:, :], 128, rstdA[:, :])
                P.stt(oraw[:, :], oraw[:, :], colsr[r][:, 11 + h:12 + h], rstdA[:, :], ALU.mult, ALU.mult)
                o = osb[b % 2]
                P.tt("dve", o[:, :], oraw[:, :], gate[:, :], ALU.mult)
                P.dma("sp", oT_d[4 + 2 * r + h][:, sl], o[:, :])
    cx.pst = [B[0], B[1], B[5]]
    cx.pso = [B[2]]
    cx.psd = [B[3]]
    scale = (MLA_NOPE + MLA_ROPE) ** -0.5
    for r in range(2):
        P.dma("pool", wuq[:, :, :], wuq_d[r].rearrange("k p n -> p k n"))
        P.dma("pool", wukv[:, :, :], wukv_d[r].rearrange("k p n -> p k n"))
        for h in range(2):
            for b in range(8):
                sl = slice(b * 512, (b + 1) * 512)
                for k in range(3):
                    P.matmul(B[4][:, :], wuq[:, k, h * 256:h * 256 + 128], cqn[:, k, sl], k == 0, k == 2)
                P.copy("act", qnT[:, sl], B[4][:, :])
                for k in range(3):
                    P.matmul(B[6][0:64, :], wuq[:, k, h * 256 + 128:h * 256 + 192], cqn[:, k, sl], k == 0, k == 2)
                for k in range(3):
                    P.matmul(B[7][0:64, :], wuq[:, k, h * 256 + 192:h * 256 + 256], cqn[:, k, sl], k == 0, k == 2)
                P.tt("dve", t1[0:64, :], B[6][0:64, :], Ct[:, sl], ALU.mult)
                P.tt("dve", t2[0:64, :], B[7][0:64, :], St[:, sl], ALU.mult)
                P.tt("dve", qrT[:, sl], t1[0:64, :], t2[0:64, :], ALU.add)
                for k in range(2):
                    P.matmul(B[5][:, :], wukv[:, k, h * 256:h * 256 + 128], ckvn[:, k, sl], k == 0, k == 1)
                P.copy("act", knT[:, sl], B[5][:, :])
                for tt_ in range(4):
                    tok = slice(b * 512 + tt_ * 128, b * 512 + (tt_ + 1) * 128)
                    for k in range(2):
                        mm(P, B[4][:, tt_ * 128:(tt_ + 1) * 128], ckvn[:, k, tok],
                           wukv[:, k, h * 256 + 128:h * 256 + 256], (tt_ == 0 and k == 0), k == 1, skip=True)
                P.copy("act", V[:, b * 4:(b + 1) * 4, :], B[4][:, :].rearrange("p (t n) -> p t n", t=4))

            def out_cb(g, h=h, r=r):
                o = osb[g % 2]
                P.recip(t1[:, :], cx.psd[0][:, :])
                P.tt("dve", o[:, :], cx.pso[0][:, :], t1[:, :], ALU.mult)
                P.dma("sp", oT_d[2 * r + h][:, g * 512:(g + 1) * 512], o[:, :])

            attention(P, cx, [[(knT, qnT), (kpeT, qrT)]], V, scale, out_cb)

    P.close_scope()


BF = ml_dtypes.bfloat16


def to_hT(h):
    s = h.shape[0]
    return np.ascontiguousarray(h.reshape(s, 8, 128).transpose(1, 2, 0))


def kchunk(w):
    return np.ascontiguousarray(w.reshape(w.shape[0] // 128, 128, w.shape[1]))


def const_tables():
    ones = np.ones((128, 128), dtype=BF)
    identf = np.eye(128, dtype=np.float32)
    mask = np.zeros((64, 512), dtype=np.float32)
    for c in range(8):
        mask[:, c * 64:(c + 1) * 64] = np.triu(np.ones((64, 64), dtype=np.float32))
    rmask = np.ones((128, 512), dtype=np.float32)
    rmask[:, ::64] = 0.0
    return ones, identf, mask, rmask


def inv_freq(dim):
    return (np.float32(ROPE_THETA) ** (-(np.arange(0, dim, 2, dtype=np.float32)) / np.float32(dim))).astype(np.float32)


def even_mixer_inputs(inp, j, r, h_T, pos):
    w_in = inp["ev_w_in"][j]
    kpe = w_in[:, 640:704]
    kpe_sw = np.concatenate([kpe[:, 32:64], kpe[:, 0:32]], axis=1)
    wlat = np.concatenate([w_in[:, 0:640], kpe, kpe_sw], axis=1)
    hg_parts = []
    for i in range(2):
        gh = 2 * r + i
        hg_parts += [w_in[:, 704 + gh * 128:704 + (gh + 1) * 128], w_in[:, 1216 + gh * 128:1216 + (gh + 1) * 128],
                     w_in[:, 2240 + gh * 128:2240 + (gh + 1) * 128], w_in[:, 1728 + gh * 128:1728 + (gh + 1) * 128]]
    whg = np.concatenate(hg_parts, axis=1)
    w_uq = inp["ev_w_uq"][j]
    w_ukv = inp["ev_w_ukv"][j]
    uq_parts, ukv_parts = [], []
    for i in range(2):
        gh = 2 * r + i
        rp = w_uq[:, gh * 192 + 128:gh * 192 + 192]
        uq_parts += [w_uq[:, gh * 192:gh * 192 + 128], rp, np.concatenate([rp[:, 32:64], rp[:, 0:32]], axis=1)]
        ukv_parts += [w_ukv[:, gh * 256:gh * 256 + 256]]
    wuq = np.concatenate(uq_parts, axis=1)
    wukv = np.concatenate(ukv_parts, axis=1)
    cols = np.zeros((128, 16), dtype=np.float32)
    cols[:, 0:3] = inp["ev_g_q"][j].reshape(3, 128).T
    cols[:, 3:5] = inp["ev_g_kv"][j].reshape(2, 128).T
    f = inv_freq(MLA_ROPE)
    cols[0:32, 5] = f
    cols[32:64, 5] = f
    cols[0:32, 6] = -1.0
    cols[32:64, 6] = 1.0
    for i in range(2):
        gh = 2 * r + i
        cols[:, 7 + i] = inp["ev_lb_logits"][0][gh * 128:(gh + 1) * 128]
        cols[:, 9 + i] = inp["ev_lb_logits"][1][gh * 128:(gh + 1) * 128]
        cols[:, 11 + i] = inp["ev_g_out"][j][gh]
    ones, identf, mask, rmask = const_tables()
    return {"cols": cols, "ones": ones, "identf": identf, "mask": mask, "rmask": rmask,
            "wlat": kchunk(np.ascontiguousarray(wlat)), "whg": kchunk(np.ascontiguousarray(whg)),
            "wuq": kchunk(np.ascontiguousarray(wuq)), "wukv": kchunk(np.ascontiguousarray(wukv))}


def emit_odd_mixer(P, B, G, layer):
    lambda_init = 0.8 - 0.6 * math.exp(-0.3 * layer)
    jj = layer // 2
    P.open_scope()
    cx = Ctx()
    hT_d = G["hT_s"]
    oT_d = G["oT_s"]
    pos_d = G["pos"]
    cols_d = G["colsO%d" % jj]
    lamp_d = G["lamp%d" % jj]
    ones_d = G["ones"]
    w_d = G["wodd%d" % jj]

    hT = P.sb("hT_sb", [128, 8, SEQ], BF16)
    w = P.sb("w_sb", [128, 8, 2560], BF16)
    colsr = [P.sb("cols_sb%d" % r, [128, 16], F32) for r in range(2)]
    cols = colsr[0]
    lamp = P.sb("lamp_sb", [128, 256], F32)
    lam = P.sb("lam", [128, 8], F32)
    cx.ones = P.sb("ones_sb", [128, 128], BF16)
    cx.epsb = P.sb("epsb", [128, 1], F32)
    cx.ki = P.sb("ki", [128, 512], I32)
    cx.posi = P.sb("posi", [128, 512], I32)
    cx.posf = P.sb("posf", [128, 512], F32)
    cx.ang = P.sb("ang", [128, 512], F32)
    cx.rtmp = P.sb("rtmp", [128, 512], F32)
    t1 = cx.posf
    t2 = cx.ang
    oraw = cx.rtmp
    Ct = P.sb("Ct", [128, SEQ], BF16)
    St = P.sb("St", [128, SEQ], BF16)
    qT = P.sb("qT", [128, SEQ], BF16)
    kT = P.sb("kT", [128, SEQ], BF16)
    V = P.sb("V", [128, 32, 128], BF16)
    cx.pT = [P.sb("pT%d" % i, [128, 512], BF16) for i in range(3)]
    osb = [P.sb("osb%d" % i, [128, 512], BF16) for i in range(2)]
    osq = P.sb("osq", [128, 512], BF16)
    rstd = P.sb("rstd", [128, 512], F32)

    for r in range(2):
        P.dma("sp", colsr[r][:, :], cols_d[r])
    P.dma("sp", lamp[:, :], lamp_d[:, :])
    P.dma("sp", cx.ones[:, :], ones_d[:, :])
    P.memset("dve", cx.epsb[:, :], EPS)
    for k in range(8):
        P.dma("sp", hT[:, k, :], hT_d[k])
    P.stt(lamp[:, 0:64], lamp[:, 0:64], 1.0, lamp[:, 64:128], ALU.mult, ALU.mult, accum=lam[:, 0:1])
    P.stt(lamp[:, 128:192], lamp[:, 128:192], 1.0, lamp[:, 192:256], ALU.mult, ALU.mult, accum=lam[:, 1:2])
    P.act(lam[:, 2:3], lam[:, 0:1], AF.Exp)
    P.act(lam[:, 3:4], lam[:, 1:2], AF.Exp)
    P.tt("dve", lam[:, 4:5], lam[:, 3:4], lam[:, 2:3], ALU.subtract)
    P.ts("dve", lam[:, 5:6], lam[:, 4:5], -lambda_init, ALU.add)
    for r in range(2):
        P.ts("dve", colsr[r][:, 6:10], colsr[r][:, 2:6], 1.0 - lambda_init, ALU.mult)
    build_rope_tables(P, cx, pos_d, cols[:, 0:1], cols[:, 1:2], 128, Ct, St)

    cx.pst = [B[0], B[1], B[7]]
    cx.pso = [B[2], B[3]]
    cx.psd = [B[4], B[5]]
    scale = DF_DH ** -0.5
    for r in range(2):
        for k in range(8):
            P.dma("pool", w[:, k, :], w_d[r][k])
        for hh in range(4):
            wo = hh * 640
            for b in range(8):
                sl = slice(b * 512, (b + 1) * 512)
                for (dst, o0) in ((qT, 0), (kT, 256)):
                    for k in range(8):
                        P.matmul(B[6][:, :], w[:, k, wo + o0:wo + o0 + 128], hT[:, k, sl], k == 0, k == 7)
                    for k in range(8):
                        P.matmul(B[7][:, :], w[:, k, wo + o0 + 128:wo + o0 + 256], hT[:, k, sl], k == 0, k == 7)
                    P.tt("dve", t1[:, :], B[6][:, :], Ct[:, sl], ALU.mult)
                    P.tt("dve", t2[:, :], B[7][:, :], St[:, sl], ALU.mult)
                    P.tt("dve", dst[:, sl], t1[:, :], t2[:, :], ALU.add)
                for tt_ in range(4):
                    tok = slice(b * 512 + tt_ * 128, b * 512 + (tt_ + 1) * 128)
                    for k in range(8):
                        mm(P, B[6][:, tt_ * 128:(tt_ + 1) * 128], hT[:, k, tok],
                           w[:, k, wo + 512:wo + 640], (tt_ == 0 and k == 0), k == 7, skip=True)
                P.copy("act", V[:, b * 4:(b + 1) * 4, :], B[6][:, :].rearrange("p (t n) -> p t n", t=4))

            def out_cb(g, hh=hh, r=r):
                o = osb[g % 2]
                P.recip(t1[:, :], cx.psd[0][:, :])
                P.recip(t2[:, :], cx.psd[1][:, :])
                P.tt("dve", t1[:, :], cx.pso[0][:, :], t1[:, :], ALU.mult)
                P.tt("dve", t2[:, :], cx.pso[1][:, :], t2[:, :], ALU.mult)
                P.stt(oraw[:, :], t2[:, :], lam[:, 5:6], t1[:, :], ALU.mult, ALU.add)
                P.act(osq[:, :], oraw[:, :], AF.Square)
                colnorm(P, cx, None, [osq[:, :]], B[6][:, :], 128, rstd[:, :])
                P.stt(o[:, :], oraw[:, :], colsr[r][:, 6 + hh:7 + hh], rstd[:, :], ALU.mult, ALU.mult)
                P.dma("sp", oT_d[4 * r + hh][:, g * 512:(g + 1) * 512], o[:, :])

            pairs = [[(kT[0:64, :], qT[0:64, :])], [(kT[64:128, :], qT[64:128, :])]]
            attention(P, cx, pairs, V, scale, out_cb, ncomp=2)
    P.close_scope()


def odd_mixer_inputs(inp, jj, r, h_T, pos):
    w_in = inp["od_w_in"][jj]

    def sw(wc):
        out = wc.copy()
        for c0 in range(0, wc.shape[1], 64):
            out[:, c0:c0 + 8] = wc[:, c0 + 8:c0 + 16]
            out[:, c0 + 8:c0 + 16] = wc[:, c0:c0 + 8]
        return out

    parts = []
    for hh in range(4):
        gh = 4 * r + hh
        q = w_in[:, gh * 128:(gh + 1) * 128]
        k = w_in[:, 1024 + gh * 128:1024 + (gh + 1) * 128]
        v = w_in[:, 2048 + gh * 128:2048 + (gh + 1) * 128]
        parts += [q, sw(q), k, sw(k), v]
    w = np.concatenate(parts, axis=1)
    cols = np.zeros((128, 16), dtype=np.float32)
    f = inv_freq(DF_ROT)
    for blk in (0, 64):
        cols[blk:blk + 8, 0] = f
        cols[blk + 8:blk + 16, 0] = f
        cols[blk:blk + 8, 1] = -1.0
        cols[blk + 8:blk + 16, 1] = 1.0
    for hh in range(4):
        cols[:, 2 + hh] = inp["od_g_head"][jj][4 * r + hh]
    lamp = np.ascontiguousarray(np.broadcast_to(inp["od_lambda"][jj].reshape(1, 256), (128, 256)))
    ones = np.ones((128, 128), dtype=BF)
    return {"cols": cols, "lamp": lamp, "ones": ones, "w": kchunk(np.ascontiguousarray(w))}


def build_fused():
    nc = bass.Bass("TRN2", target_bir_lowering=False)
    P = Prog(nc)
    G = {}

    def ein(name, shape, dt=F32):
        G[name] = P.dram(name, shape, dt, "ExternalInput")

    ein("x", [SEQ // 128, 128, D_MODEL])
    ein("pos", [128, SEQ], I32)
    ein("ident", [128, 128], BF16)
    ein("ones", [128, 128], BF16)
    ein("identf", [128, 128])
    ein("mask", [64, 512])
    ein("rmask", [128, 512])
    ein("gains", [DEPTH * 6, 128, D_MODEL])
    for l in range(DEPTH):
        for i in range(2):
            ein("wg%d%d" % (l, i), [NJ, 128, 8, 128])
            ein("wu%d%d" % (l, i), [NJ, 128, 8, 128])
            ein("wd%d%d" % (l, i), [NJ, 128, D_MODEL])
        ein("wout%d" % l, [8, 128, D_MODEL])
    for j in range(DEPTH // 2):
        ein("wlat%d" % j, [8, 128, 768])
        ein("whg%d" % j, [2, 8, 128, 1024])
        ein("wuq%d" % j, [2, 3, 128, 512])
        ein("wukv%d" % j, [2, 2, 128, 512])
        ein("colsE%d" % j, [2, 128, 16])
        ein("wodd%d" % j, [2, 8, 128, 2560])
        ein("colsO%d" % j, [2, 128, 16])
        ein("lamp%d" % j, [128, 256])
    for g in range(NGRP):
        G["xs%d" % g] = P.dram("xs%d" % g, [8, 128, D_MODEL], F32, "Internal")
    G["hT_s"] = P.dram("hT_s", [8, 128, SEQ], BF16, "Internal")
    G["oT_s"] = P.dram("oT_s", [8, 128, SEQ], BF16, "Internal")
    G["xo"] = P.dram("xo", [SEQ // 128, 128, D_MODEL], F32, "ExternalOutput")
    B = [P.ps("B%d" % i, [128, 512]) for i in range(8)]
    for step in range(DEPTH + 1):
        emit_token_phase(P, B, G, step)
        if step < DEPTH:
            if step % 2 == 0:
                emit_even_mixer(P, B, G, step // 2)
            else:
                emit_odd_mixer(P, B, G, step)
    P.finalize()
    return nc


def rearr_ffn_w(W):
    return np.ascontiguousarray(W.reshape(8, 128, NJ, 128).transpose(2, 1, 0, 3))


def fused_common_inputs(inp):
    ones, identf, mask, rmask = const_tables()
    C = {"ident": np.eye(128, dtype=BF), "ones": ones, "identf": identf, "mask": mask, "rmask": rmask}
    g = inp["norm_g"].reshape(DEPTH * 6, 1, D_MODEL)
    C["gains"] = np.ascontiguousarray(np.broadcast_to(g, (DEPTH * 6, 128, D_MODEL)))
    for l in range(DEPTH):
        for i in range(2):
            C["wg%d%d" % (l, i)] = rearr_ffn_w(inp["ffn_w_gate"][l, i])
            C["wu%d%d" % (l, i)] = rearr_ffn_w(inp["ffn_w_up"][l, i])
            C["wd%d%d" % (l, i)] = np.ascontiguousarray(inp["ffn_w_down"][l, i].reshape(NJ, 128, D_MODEL))
        w_out = inp["ev_w_out"][l // 2] if l % 2 == 0 else inp["od_w_out"][l // 2]
        C["wout%d" % l] = kchunk(np.ascontiguousarray(w_out))
    for j in range(DEPTH // 2):
        e = [even_mixer_inputs(inp, j, r, None, None) for r in range(2)]
        C["wlat%d" % j] = e[0]["wlat"]
        for nm in ("whg", "wuq", "wukv"):
            C["%s%d" % (nm, j)] = np.ascontiguousarray(np.stack([e[0][nm], e[1][nm]], axis=0))
        C["colsE%d" % j] = np.ascontiguousarray(np.stack([e[0]["cols"], e[1]["cols"]], axis=0))
        o = [odd_mixer_inputs(inp, j, r, None, None) for r in range(2)]
        C["wodd%d" % j] = np.ascontiguousarray(np.stack([o[0]["w"], o[1]["w"]], axis=0))
        C["colsO%d" % j] = np.ascontiguousarray(np.stack([o[0]["cols"], o[1]["cols"]], axis=0))
        C["lamp%d" % j] = o[0]["lamp"]
    return C


_NC_CACHE = {}


def kernel(x, positions, norm_g, ffn_w_gate, ffn_w_up, ffn_w_down, ev_w_in, ev_g_q, ev_w_uq, ev_g_kv,
           ev_w_ukv, ev_lb_logits, ev_g_out, ev_w_out, od_w_in, od_lambda, od_g_head, od_w_out):
    inp = dict(x=x, positions=positions, norm_g=norm_g, ffn_w_gate=ffn_w_gate, ffn_w_up=ffn_w_up,
               ffn_w_down=ffn_w_down, ev_w_in=ev_w_in, ev_g_q=ev_g_q, ev_w_uq=ev_w_uq, ev_g_kv=ev_g_kv,
               ev_w_ukv=ev_w_ukv, ev_lb_logits=ev_lb_logits, ev_g_out=ev_g_out, ev_w_out=ev_w_out,
               od_w_in=od_w_in, od_lambda=od_lambda, od_g_head=od_g_head, od_w_out=od_w_out)
    inp = {k: np.asarray(v) for k, v in inp.items()}
    if "nc" not in _NC_CACHE:
        _NC_CACHE["nc"] = build_fused()
    nc = _NC_CACHE["nc"]
    C = fused_common_inputs(inp)
    cores = list(range(NCORES))
    in_maps = []
    for c in cores:
        b = c // 2
        m = dict(C)
        m["x"] = np.ascontiguousarray(inp["x"][b].astype(np.float32, copy=False)).reshape(SEQ // 128, 128, D_MODEL)
        m["pos"] = np.ascontiguousarray(np.broadcast_to(inp["positions"][b].astype(np.int32)[None, :], (128, SEQ)))
        in_maps.append(m)
    res = run_bass_kernel_spmd(nc, in_maps, core_ids=cores).results
    out = np.zeros((BATCH, SEQ, D_MODEL), dtype=np.float32)
    for b in range(BATCH):
        out[b] = np.asarray(res[2 * b]["xo"]).reshape(SEQ, D_MODEL)
    return out
```

```python
import math
import numpy as np
import ml_dtypes
import concourse.bass as bass
import concourse.mybir as mybir
from concourse.bass_utils import run_bass_kernel_spmd

F32 = mybir.dt.float32
BF16 = mybir.dt.bfloat16
I32 = mybir.dt.int32
AF = mybir.ActivationFunctionType
ALU = mybir.AluOpType
AX = mybir.AxisListType

D_MODEL = 1024
BATCH = 4
SEQ = 4096
DEPTH = 4
CHUNK = 64
ROPE_THETA = 500000.0
EPS = 1e-6
TINY = 1e-30
D_FF = 2816
NJ = D_FF // 128
MLA_NOPE, MLA_ROPE, MLA_V, MLA_Q_RANK, MLA_KV_RANK = 128, 64, 128, 384, 256
DF_DH = 64
DF_ROT = 16
NCORES = 8
TOK = SEQ // 2
NTT = TOK // 128


class View:
    __slots__ = ("tile", "ap")

    def __init__(self, tile, ap):
        self.tile = tile
        self.ap = ap

    def __getitem__(self, idx):
        return View(self.tile, self.ap[idx])

    def rearrange(self, s, **kw):
        return View(self.tile, self.ap.rearrange(s, **kw))

    def bitcast(self, dt):
        return View(self.tile, self.ap.bitcast(dt))


class Tile:
    def __init__(self, name, base_ap):
        self.name = name
        self.base = base_ap
        self.last_w = None
        self.readers = []

    def __getitem__(self, idx):
        return View(self, self.base[idx])

    def v(self):
        return View(self, self.base)


class Op:
    __slots__ = ("eng", "fn", "reads", "writes", "dma", "deps", "signal", "seq",
                 "sem", "val", "prev_dma")

    def __init__(self, eng, fn, reads, writes, dma):
        self.eng = eng
        self.fn = fn
        self.reads = reads
        self.writes = writes
        self.dma = dma
        self.deps = []
        self.signal = False
        self.seq = 0
        self.sem = None
        self.val = 0
        self.prev_dma = None


class Prog:
    ENGS = ("pe", "act", "dve", "pool", "sp")
    NDMASEM = 12

    def __init__(self, nc):
        self.nc = nc
        self.ops = []
        self.n_sb = 0
        self.stack = None
        self.uid = 0

    def open_scope(self):
        import contextlib
        self.stack = contextlib.ExitStack()

    def close_scope(self):
        self.stack.close()
        self.stack = None
        self.ops.append("BARRIER")

    def sb(self, name, shape, dtype):
        if self.stack is not None:
            self.uid += 1
            h = self.stack.enter_context(self.nc.sbuf_tensor("sb%d_%s" % (self.uid, name), list(shape), dtype))
        else:
            h = self.nc.alloc_sbuf_tensor("sb_" + name, list(shape), dtype)
        idx = tuple(slice(None) for _ in shape)
        return Tile(name, h[idx])

    def ps(self, name, shape, dtype=F32):
        h = self.nc.alloc_psum_tensor("ps_" + name, list(shape), dtype)
        idx = tuple(slice(None) for _ in shape)
        return Tile(name, h[idx])

    def dram(self, name, shape, dtype, kind):
        h = self.nc.dram_tensor(name, list(shape), dtype, kind=kind)
        return Tile(name, h.ap())

    def add(self, eng, fn, reads, writes, dma=False):
        rt = []
        for r in reads:
            if isinstance(r, View) and r.tile not in rt:
                rt.append(r.tile)
        wt = []
        for w in writes:
            if isinstance(w, View) and w.tile not in wt:
                wt.append(w.tile)
        op = Op(eng, fn, rt, wt, dma)
        self.ops.append(op)
        return op

    def dma(self, q, out, in_):
        return self.add(q, lambda e: e.dma_start(out=out.ap, in_=in_.ap), [in_], [out], dma=True)

    def matmul(self, out, lhsT, rhs, start, stop):
        return self.add("pe", lambda e: e.matmul(out.ap, lhsT.ap, rhs.ap, start=start, stop=stop),
                        [lhsT, rhs], [out])

    def transpose(self, out, in_, ident):
        return self.add("pe", lambda e: e.transpose(out.ap, in_.ap, ident.ap), [in_, ident], [out])

    def act(self, out, in_, func, bias=None, scale=None, accum=None, eng="act"):
        reads = [in_]
        writes = [out]
        kw = {}
        if bias is not None:
            if isinstance(bias, View):
                reads.append(bias)
                kw["bias"] = bias.ap
            else:
                kw["bias"] = float(bias)
        if scale is not None:
            if isinstance(scale, View):
                reads.append(scale)
                kw["scale"] = scale.ap
            else:
                kw["scale"] = float(scale)
        if accum is not None:
            writes.append(accum)
            kw["accum_out"] = accum.ap
        return self.add(eng, lambda e: e.activation(out.ap, in_.ap, func, **kw), reads, writes)

    def tt(self, eng, out, in0, in1, op):
        return self.add(eng, lambda e: e.tensor_tensor(out.ap, in0.ap, in1.ap, op), [in0, in1], [out])

    def ts(self, eng, out, in0, s1, op0, s2=None, op1=None, accum=None):
        reads = [in0]
        a1 = s1.ap if isinstance(s1, View) else float(s1)
        if isinstance(s1, View):
            reads.append(s1)
        a2 = None
        if s2 is not None:
            a2 = s2.ap if isinstance(s2, View) else float(s2)
            if isinstance(s2, View):
                reads.append(s2)
        writes = [out]
        kw = {}
        if accum is not None:
            writes.append(accum)
            kw["accum_out"] = accum.ap
        o1 = op1 if op1 is not None else ALU.bypass
        return self.add(eng, lambda e: e.tensor_scalar(out.ap, in0.ap, a1, a2, op0, o1, **kw), reads, writes)

    def stt(self, out, in0, scalar, in1, op0, op1, accum=None):
        reads = [in0, in1]
        a = scalar.ap if isinstance(scalar, View) else float(scalar)
        if isinstance(scalar, View):
            reads.append(scalar)
        writes = [out]
        kw = {}
        if accum is not None:
            writes.append(accum)
            kw["accum_out"] = accum.ap
        return self.add("dve", lambda e: e.scalar_tensor_tensor(out.ap, in0.ap, a, in1.ap, op0, op1, **kw),
                        reads, writes)

    def copy(self, eng, out, in_):
        if eng == "act":
            return self.add("act", lambda e: e.copy(out.ap, in_.ap), [in_], [out])
        return self.add(eng, lambda e: e.tensor_copy(out.ap, in_.ap), [in_], [out])

    def memset(self, eng, out, val):
        return self.add(eng, lambda e: e.memset(out.ap, val), [], [out])

    def recip(self, out, in_):
        return self.add("dve", lambda e: e.reciprocal(out.ap, in_.ap), [in_], [out])

    def finalize(self):
        nc = self.nc
        ops = self.ops
        real = []
        fence = []
        last_eng = {}
        dma_q = {}
        for op in ops:
            if isinstance(op, str):
                fence = [o for o in last_eng.values()]
                for q, lst in dma_q.items():
                    fence.extend(lst[-self.NDMASEM:])
                continue
            real.append(op)
            if op.dma:
                dma_q.setdefault(op.eng, []).append(op)
            else:
                last_eng[op.eng] = op
            op.deps.extend(fence)
        ops = real
        self.ops = real
        for op in ops:
            deps = list(op.deps)
            op.deps = []
            raw = set()
            for t in op.reads:
                if t.last_w is not None:
                    deps.append(t.last_w)
                    raw.add(id(t.last_w))
            for t in op.writes:
                if t.last_w is not None:
                    deps.append(t.last_w)
                deps.extend(t.readers)
            seen = set()
            for d in deps:
                if d is op or id(d) in seen:
                    continue
                seen.add(id(d))
                if (not d.dma) and d.eng == op.eng and op.eng == "pe":
                    continue
                op.deps.append(d)
            for t in op.reads:
                t.readers.append(op)
            for t in op.writes:
                t.last_w = op
                t.readers = []
        for op in ops:
            for d in op.deps:
                if not d.dma:
                    d.signal = True
        cnt = {e: 0 for e in self.ENGS}
        dcnt = {e: 0 for e in self.ENGS}
        dma_hist = {e: [] for e in self.ENGS}
        esem = {}
        dsem = {}
        for e in self.ENGS:
            esem[e] = nc.alloc_semaphore("s_" + e)
        for op in ops:
            if op.dma:
                q = op.eng
                if q not in dsem:
                    dsem[q] = [nc.alloc_semaphore("d_%s_%d" % (q, i)) for i in range(self.NDMASEM)]
                i = dcnt[q]
                dcnt[q] += 1
                op.sem = dsem[q][i % self.NDMASEM]
                op.val = 16 * (i // self.NDMASEM + 1)
                if i >= self.NDMASEM:
                    op.prev_dma = dma_hist[q][i - self.NDMASEM]
                dma_hist[q].append(op)
            elif op.signal:
                cnt[op.eng] += 1
                op.seq = cnt[op.eng]
                op.sem = esem[op.eng]
                op.val = op.seq
        per_eng = {e: [] for e in self.ENGS}
        for op in ops:
            per_eng[op.eng].append(op)
        last_dma = {q: h for q, h in dma_hist.items() if h}

        def emit(eng_name, e):
            waited = {}
            for op in per_eng[eng_name]:
                need = {}
                dl = list(op.deps)
                if op.prev_dma is not None:
                    dl.append(op.prev_dma)
                for d in dl:
                    k = d.sem.num
                    if waited.get(k, 0) >= d.val:
                        continue
                    if k not in need or need[k][1] < d.val:
                        need[k] = (d.sem, d.val)
                for k, (s, v) in need.items():
                    e.wait_ge(s, v)
                    waited[k] = v
                ins = op.fn(e)
                if op.dma:
                    ins.then_inc(op.sem, 16)
                elif op.signal:
                    ins.then_inc(op.sem, 1)
            if eng_name in last_dma:
                fin = {}
                for d in last_dma[eng_name]:
                    k = d.sem.num
                    if k not in fin or fin[k][1] < d.val:
                        fin[k] = (d.sem, d.val)
                for k, (s, v) in fin.items():
                    if waited.get(k, 0) < v:
                        e.wait_ge(s, v)

        with nc.Block() as block:
            @block.tensor
            def _(e):
                emit("pe", e)

            @block.scalar
            def _(e):
                emit("act", e)

            @block.vector
            def _(e):
                emit("dve", e)

            @block.gpsimd
            def _(e):
                emit("pool", e)

            @block.sync
            def _(e):
                emit("sp", e)


class Ctx:
    pass


def rms_stats(P, cx, src, d, rstd):
    P.act(cx.junk[:, 0:d], src, AF.Square, accum=cx.ss[:, 0:1])
    P.act(cx.ss[:, 1:2], cx.ss[:, 0:1], AF.Sqrt, bias=cx.epsb[:, 0:1], scale=1.0 / d)
    P.recip(rstd, cx.ss[:, 1:2])


def norm_T_stage(P, cx, xt, g, sink):
    nt = len(xt)

    def stats(t):
        rs = cx.rstdN[:, t:t + 1]
        P.act(cx.junk[:, :], xt[t][:, :], AF.Square, accum=cx.ssN[:, 2 * t:2 * t + 1])
        P.act(cx.ssN[:, 2 * t + 1:2 * t + 2], cx.ssN[:, 2 * t:2 * t + 1], AF.Sqrt,
              bias=cx.epsb[:, 0:1], scale=1.0 / D_MODEL)
        P.recip(rs, cx.ssN[:, 2 * t + 1:2 * t + 2])
        P.stt(cx.hnb[t % 2][:, :], xt[t][:, :], rs, g[:, :], ALU.mult, ALU.mult)

    def trans(t):
        pt = cx.ptr[t % 2]
        hn = cx.hnb[t % 2]
        for kc in range(8):
            P.transpose(pt[:, kc * 128:(kc + 1) * 128], hn[:, kc * 128:(kc + 1) * 128], cx.ident[:, :])
        sink(t, pt)

    for t in range(nt + 1):
        if t < nt:
            stats(t)
        if t >= 1:
            trans(t - 1)


STAGE = 9


def ffn_group(P, cx, xt, W, gpre, gpost, name):
    nt = len(xt)
    nh = nt // 4
    def sink(t, pt):
        P.copy("act", cx.hT[t // 4][:, :, (t % 4) * 128:(t % 4 + 1) * 128],
               pt.rearrange("p (k n) -> p k n", k=8))

    norm_T_stage(P, cx, xt, gpre, sink)
    if STAGE < 2:
        return
    for j in range(NJ):
        wg = cx.wg[j % len(cx.wg)]
        wu = cx.wu[j % len(cx.wu)]
        P.dma("pool", wg[:, :, :], W["wg"][j])
        P.dma("pool", wu[:, :, :], W["wu"][j])
        P.dma("pool", cx.wd[j][:, :], W["wd_d"][j])
        for h in range(nh):
            pg = cx.pg[(j * nh + h) % 2]
            pu = cx.pu[(j * nh + h) % 2]
            for kc in range(8):
                P.matmul(pg[:, :], wg[:, kc, :], cx.hT[h][:, kc, :], kc == 0, kc == 7)
            for kc in range(8):
                P.matmul(pu[:, :], wu[:, kc, :], cx.hT[h][:, kc, :], kc == 0, kc == 7)
            sg = cx.sg[(j * nh + h) % 2]
            P.act(sg[:, :], pg[:, :], AF.Silu)
            P.tt("dve", cx.aT[j][:, h * 512:(h + 1) * 512], sg[:, :], pu[:, :], ALU.mult)
    if STAGE < 3:
        return
    for t in range(nt):
        y = cx.y[t % 2]
        for n in range(2):
            pd = cx.pd[(t * 2 + n) % 2]
            for j in range(NJ):
                P.matmul(pd[:, :], cx.aT[j][:, t * 128:(t + 1) * 128],
                         W["wd"][j][:, n * 512:(n + 1) * 512], j == 0, j == NJ - 1)
            P.copy("act", y[:, n * 512:(n + 1) * 512], pd[:, :])
        norm_residual(P, cx, xt[t], y, gpost, 0.5)


def norm_residual(P, cx, x, y, g, alpha):
    rms_stats(P, cx, y[:, :], D_MODEL, cx.rstd[:, 1:2])
    P.stt(y[:, :], y[:, :], cx.rstd[:, 1:2], g[:, :], ALU.mult, ALU.mult)
    P.stt(x[:, :], y[:, :], float(alpha), x[:, :], ALU.mult, ALU.add)


def load_wd(P, cx, wd_dram):
    for j in range(NJ):
        P.dma("pool", cx.wd[j][:, :], wd_dram[j])


NGRP = SEQ // 1024


def emit_token_phase(P, B, G, step):
    do_post = step > 0
    do_pre = step < DEPTH
    P.open_scope()
    cx = Ctx()
    cx.ident = P.sb("ident_sb", [128, 128], BF16)
    cx.ss = P.sb("ss", [128, 2], F32)
    cx.rstd = P.sb("rstd", [128, 2], F32)
    cx.epsb = P.sb("epsb", [128, 1], F32)
    cx.hnb = [P.sb("hn%d" % i, [128, D_MODEL], BF16) for i in range(2)]
    cx.ssN = P.sb("ssN", [128, 32], F32)
    cx.rstdN = P.sb("rstdN", [128, 16], F32)
    cx.hT = [P.sb("hT%d" % i, [128, 8, 512], BF16) for i in range(2)]
    cx.wg = [P.sb("wg%d" % i, [128, 8, 128], BF16) for i in range(3)]
    cx.wu = [P.sb("wu%d" % i, [128, 8, 128], BF16) for i in range(3)]
    cx.wd = [P.sb("wd%d" % j, [128, D_MODEL], BF16) for j in range(NJ)]
    cx.aT = [P.sb("aT%d" % j, [128, 1024], BF16) for j in range(NJ)]
    cx.sg = [P.sb("sg%d" % i, [128, 512], F32) for i in range(2)]
    cx.junk = cx.sg[0][:, :].bitcast(BF16)
    cx.y = [P.sb("y%d" % i, [128, D_MODEL], F32) for i in range(2)]
    gt = [P.sb("g%d" % i, [128, D_MODEL], F32) for i in range(6)]
    xt = [P.sb("x%d" % i, [128, D_MODEL], F32) for i in range(8)]
    if do_post:
        wout = [P.sb("wout%d" % k, [128, D_MODEL], BF16) for k in range(8)]
    if do_pre:
        hTo = [P.sb("hTo0", [128, 8, 128], BF16),
               cx.sg[1][:, :].bitcast(BF16).rearrange("p (k n) -> p k n", k=8)]
    cx.pg = [B[0], B[1]]
    cx.pu = [B[2], B[3]]
    cx.pd = [B[4], B[5]]
    cx.ptr = [B[6][:, :].bitcast(BF16), B[7][:, :].bitcast(BF16)]

    P.dma("sp", cx.ident[:, :], G["ident"][:, :])
    P.memset("dve", cx.epsb[:, :], EPS)
    Wpost = Wpre = None
    if do_post:
        l = step - 1
        for i in range(3):
            P.dma("sp", gt[i][:, :], G["gains"][l * 6 + 3 + i])
        for k in range(8):
            P.dma("pool", wout[k][:, :], G["wout%d" % l][k])
        Wpost = {"wg": G["wg%d1" % l], "wu": G["wu%d1" % l], "wd": cx.wd, "wd_d": G["wd%d1" % l]}
    if do_pre:
        l = step
        for i in range(3):
            P.dma("sp", gt[3 + i][:, :], G["gains"][l * 6 + i])
        Wpre = {"wg": G["wg%d0" % l], "wu": G["wu%d0" % l], "wd": cx.wd, "wd_d": G["wd%d0" % l]}

    for grp in range(NGRP):
        for t in range(8):
            src = G["x"][grp * 8 + t] if step == 0 else G["xs%d" % grp][t]
            P.dma("sp", xt[t][:, :], src)
        if do_post:
            l = step - 1
            for h2 in range(2):
                P.dma("sp", cx.hT[h2][:, :, :],
                      G["oT_s"][:, :, grp * 1024 + h2 * 512:grp * 1024 + (h2 + 1) * 512].rearrange("k p n -> p k n"))
            for t in range(8):
                y = cx.y[t % 2]
                for n in range(2):
                    pd = cx.pd[(t * 2 + n) % 2]
                    for k in range(8):
                        P.matmul(pd[:, :], cx.hT[t // 4][:, k, (t % 4) * 128:(t % 4 + 1) * 128],
                                 wout[k][:, n * 512:(n + 1) * 512], k == 0, k == 7)
                    P.copy("act", y[:, n * 512:(n + 1) * 512], pd[:, :])
                norm_residual(P, cx, xt[t], y, gt[0], 1.0)
            ffn_group(P, cx, xt, Wpost, gt[1], gt[2], "f2")
        if do_pre:
            l = step
            ffn_group(P, cx, xt, Wpre, gt[3], gt[4], "f1")
            def hsink(t, pt, grp=grp):
                ho = hTo[t % 2]
                P.copy("act", ho[:, :, :], pt.rearrange("p (k n) -> p k n", k=8))
                tok0 = grp * 1024 + t * 128
                P.dma("sp", G["hT_s"][:, :, tok0:tok0 + 128].rearrange("k p n -> p k n"), ho[:, :, :])

            norm_T_stage(P, cx, xt, gt[5], hsink)
        for t in range(8):
            dst = G["xo"][grp * 8 + t] if step == DEPTH else G["xs%d" % grp][t]
            P.dma("sp", dst, xt[t][:, :])
    P.close_scope()


def mm(P, out, lhsT, rhs, start, stop, skip=False):
    if skip:
        return P.add("pe", lambda e: e.matmul(out.ap, lhsT.ap, rhs.ap, start=start, stop=stop,
                                              skip_group_check=True), [lhsT, rhs], [out])
    return P.matmul(out, lhsT, rhs, start, stop)


def build_rope_tables(P, cx, pos_d, freq_col, sgn_col, nrows, Ct, St):
    PI = math.pi
    PIS = 3.1415925
    n = nrows
    for b in range(8):
        sl = slice(b * 512, (b + 1) * 512)
        P.dma("sp", cx.posi[0:n, :], pos_d[0:n, sl])
        P.copy("dve", cx.posf[0:n, :], cx.posi[0:n, :])
        P.ts("dve", cx.ang[0:n, :], cx.posf[0:n, :], freq_col, ALU.mult)
        for shift, dst, sg in ((0.0, St, sgn_col), (0.5 * PI, Ct, None)):
            if shift != 0.0:
                P.ts("dve", cx.ang[0:n, :], cx.ang[0:n, :], shift, ALU.add)
            P.ts("dve", cx.rtmp[0:n, :], cx.ang[0:n, :], 1.0 / (2 * PI), ALU.mult)
            P.copy("dve", cx.ki[0:n, :], cx.rtmp[0:n, :])
            P.copy("dve", cx.rtmp[0:n, :], cx.ki[0:n, :])
            P.stt(cx.rtmp[0:n, :], cx.rtmp[0:n, :], -2 * PI, cx.ang[0:n, :], ALU.mult, ALU.add)
            P.ts("dve", cx.posf[0:n, :], cx.rtmp[0:n, :], PI, ALU.is_gt, 2 * PI, ALU.mult)
            P.tt("dve", cx.rtmp[0:n, :], cx.rtmp[0:n, :], cx.posf[0:n, :], ALU.subtract)
            P.ts("dve", cx.rtmp[0:n, :], cx.rtmp[0:n, :], PIS, ALU.min, -PIS, ALU.max)
            if sg is not None:
                P.act(cx.rtmp[0:n, :], cx.rtmp[0:n, :], AF.Sin)
                P.ts("dve", dst[0:n, sl], cx.rtmp[0:n, :], sg, ALU.mult)
            else:
                P.act(dst[0:n, sl], cx.rtmp[0:n, :], AF.Sin)


def attention(P, cx, qk_pairs, V, scale, out_cb, ncomp=1):
    its = []
    for g in range(8):
        nk = 4 * (g + 1)
        for kt in range(nk):
            for c in range(ncomp):
                its.append((g, kt, c, nk))
    nb = len(cx.pst)

    def front(i):
        g, kt, c, nk = its[i]
        a = kt - 4 * g
        q0 = 128 * a if a > 0 else 0
        st = cx.pst[i % nb]
        pT = cx.pT[i % nb]
        prs = qk_pairs[c]
        for n_, (kT, qT) in enumerate(prs):
            P.matmul(st[:, q0:512], kT[:, kt * 128:(kt + 1) * 128],
                     qT[:, g * 512 + q0:(g + 1) * 512], n_ == 0, n_ == len(prs) - 1)
        P.act(pT[:, q0:512], st[:, q0:512], AF.Exp, scale=scale)
        if a >= 0:
            P.memset("dve", pT[64:128, q0:q0 + 64], 0.0)

    def back(i):
        g, kt, c, nk = its[i]
        a = kt - 4 * g
        q0 = 128 * a if a > 0 else 0
        pT = cx.pT[i % nb]
        mm(P, cx.pso[c][:, q0:512], V[:, kt, :], pT[:, q0:512], kt == 0, kt == nk - 1, skip=True)
        acc = cx.acc[c][g % 2]
        if kt == 0:
            P.copy("dve", acc[:, :], pT[:, :])
        else:
            P.tt("dve", acc[:, q0:512], acc[:, q0:512], pT[:, q0:512], ALU.add)
        if kt == nk - 1 and c == ncomp - 1:
            for c2 in range(ncomp):
                P.matmul(cx.psd[c2][:, :], cx.onesf[:, :], cx.acc[c2][g % 2][:, :], True, True)
            out_cb(g)

    SK = nb - 1
    n = len(its)
    for i in range(n + SK):
        if i < n:
            front(i)
        if i - SK >= 0:
            back(i - SK)


def colnorm(P, cx, raws, sqs, ps_ss, d, rstd_out):
    for i, sq in enumerate(sqs):
        P.matmul(ps_ss, cx.ones[:, :], sq, i == 0, i == len(sqs) - 1)
    P.act(rstd_out, ps_ss, AF.Sqrt, bias=cx.epsb[:, 0:1], scale=1.0 / d)
    P.recip(rstd_out, rstd_out)


def emit_even_mixer(P, B, G, j):
    P.open_scope()
    cx = Ctx()
    hT_d = G["hT_s"]
    oT_d = G["oT_s"]
    pos_d = G["pos"]
    cols_d = G["colsE%d" % j]
    ones_d, identf_d, mask_d, rmask_d = G["ones"], G["identf"], G["mask"], G["rmask"]
    wlat_d, whg_d, wuq_d, wukv_d = G["wlat%d" % j], G["whg%d" % j], G["wuq%d" % j], G["wukv%d" % j]

    hT = P.sb("hT_sb", [128, 8, SEQ], BF16)
    colsr = [P.sb("cols_sb%d" % r, [128, 16], F32) for r in range(2)]
    cols = colsr[0]
    cx.ones = P.sb("ones_sb", [128, 128], BF16)
    identf = P.sb("identf_sb", [128, 128], F32)
    mask = P.sb("mask_sb", [64, 512], F32)
    rmask = P.sb("rmask_sb", [128, 512], F32)
    wlat = P.sb("wlat_sb", [128, 8, 768], BF16)
    whg = P.sb("whg_sb", [128, 8, 1024], BF16)
    wuq = P.sb("wuq_sb", [128, 3, 512], BF16)
    wukv = P.sb("wukv_sb", [128, 2, 512], BF16)
    cx.epsb = P.sb("epsb", [128, 1], F32)
    cx.ki = P.sb("ki", [128, 512], I32)
    cx.posi = P.sb("posi", [128, 512], I32)
    sig = P.sb("sig", [128, 512], F32)
    ff = P.sb("ff", [128, 512], F32)
    kk = P.sb("kk", [128, 512], F32)
    bcum = P.sb("bcum", [128, 512], F32)
    eb = P.sb("eb", [128, 512], F32)
    enb = P.sb("enb", [128, 512], F32)
    kdT = P.sb("kdT", [128, 512], F32)
    gate = P.sb("gate", [128, 512], F32)
    cx.posf = sig
    cx.ang = ff
    cx.rtmp = kk
    Ct = P.sb("Ct", [64, SEQ], BF16)
    St = P.sb("St", [64, SEQ], BF16)
    cqn = P.sb("cqn", [128, 3, SEQ], BF16)
    ckvn = P.sb("ckvn", [128, 2, SEQ], BF16)
    kpeT = P.sb("kpeT", [64, SEQ], BF16)
    raw = [bcum, eb, enb, kdT, gate]
    rstdA = P.sb("rstdA", [128, 512], F32)
    rstdB = ff
    t1 = kk
    t2 = sig
    qnT = hT[:, 0, :]
    knT = hT[:, 1, :]
    qrT = hT[0:64, 2, :]
    V = hT[:, 3, :].rearrange("p (t n) -> p t n", t=32)
    cx.pT = [P.sb("pT%d" % i, [128, 512], BF16) for i in range(3)]
    osb = [P.sb("osb%d" % i, [128, 512], BF16) for i in range(2)]
    sq = [cx.pT[0], cx.pT[1], cx.pT[2], osb[0], osb[1]]
    cx.acc = [[P.sb("acc%d" % i, [128, 512], F32) for i in range(2)]]
    cx.onesf = P.sb("onesf", [128, 128], F32)
    P.memset("dve", cx.onesf[:, :], 1.0)

    for r in range(2):
        P.dma("sp", colsr[r][:, :], cols_d[r])
    P.dma("sp", cx.ones[:, :], ones_d[:, :])
    P.dma("sp", identf[:, :], identf_d[:, :])
    P.dma("sp", mask[:, :], mask_d[:, :])
    P.dma("sp", rmask[:, :], rmask_d[:, :])
    P.memset("dve", cx.epsb[:, :], EPS)
    P.dma("pool", wlat[:, :, :], wlat_d[:, :, :].rearrange("k p n -> p k n"))
    for k in range(8):
        P.dma("sp", hT[:, k, :], hT_d[k])
    build_rope_tables(P, cx, pos_d, cols[0:64, 5:6], cols[0:64, 6:7], 64, Ct, St)

    for b in range(8):
        sl = slice(b * 512, (b + 1) * 512)
        for oc in range(5):
            pb = B[oc % 4]
            for k in range(8):
                P.matmul(pb[:, :], wlat[:, k, oc * 128:(oc + 1) * 128], hT[:, k, sl], k == 0, k == 7)
            P.copy("act", raw[oc][:, :], pb[:, :])
            P.act(sq[oc][:, :], pb[:, :], AF.Square)
        colnorm(P, cx, raw[0:3], [s[:, :] for s in sq[0:3]], B[4][:, :], MLA_Q_RANK, rstdA[:, :])
        colnorm(P, cx, raw[3:5], [s[:, :] for s in sq[3:5]], B[5][:, :], MLA_KV_RANK, rstdB[:, :])
        for c in range(3):
            P.stt(cqn[:, c, sl], raw[c][:, :], cols[:, c:c + 1], rstdA[:, :], ALU.mult, ALU.mult)
        for c in range(2):
            P.stt(ckvn[:, c, sl], raw[3 + c][:, :], cols[:, 3 + c:4 + c], rstdB[:, :], ALU.mult, ALU.mult)
        for k in range(8):
            P.matmul(B[6][0:64, :], wlat[:, k, 640:704], hT[:, k, sl], k == 0, k == 7)
        for k in range(8):
            P.matmul(B[7][0:64, :], wlat[:, k, 704:768], hT[:, k, sl], k == 0, k == 7)
        P.tt("dve", t1[0:64, :], B[6][0:64, :], Ct[:, sl], ALU.mult)
        P.tt("dve", t2[0:64, :], B[7][0:64, :], St[:, sl], ALU.mult)
        P.tt("dve", kpeT[:, sl], t1[0:64, :], t2[0:64, :], ALU.add)

    lbc = P.sb("lbc", [128, 4], F32)
    state = P.sb("state", [128, 128], F32)
    state_bf = P.sb("state_bf", [128, 128], BF16)
    qd = P.sb("qd", [128, 512], BF16)
    ktl = P.sb("ktl", [128, 512], BF16)
    kdec = P.sb("kdec", [64, 8, 128], BF16)
    V64 = P.sb("V64", [64, 8, 128], BF16)
    attn = P.sb("attn", [64, 512], BF16)
    oraw = bcum
    osq = P.sb("osq", [128, 512], BF16)
    for r in range(2):
        P.dma("pool", whg[:, :, :], whg_d[r].rearrange("k p n -> p k n"))
        for h in range(2):
            wofs = h * 512
            if j == 0:
                P.memset("dve", lbc[:, 0:1], 0.0)
            else:
                P.tt("dve", lbc[:, 3:4], colsr[r][:, 9 + h:10 + h], colsr[r][:, 7 + h:8 + h], ALU.subtract)
                P.act(lbc[:, 0:1], lbc[:, 3:4], AF.Sigmoid)
            P.ts("dve", lbc[:, 1:2], lbc[:, 0:1], -1.0, ALU.mult, 1.0, ALU.add)
            P.ts("dve", lbc[:, 2:3], lbc[:, 1:2], -1.0, ALU.mult)
            P.memset("dve", state[:, :], 0.0)
            P.memset("dve", state_bf[:, :], 0.0)
            for b in range(8):
                sl = slice(b * 512, (b + 1) * 512)
                for i, pb in enumerate((B[0], B[1], B[2])):
                    for k in range(8):
                        P.matmul(pb[:, :], whg[:, k, wofs + i * 128:wofs + (i + 1) * 128], hT[:, k, sl], k == 0, k == 7)
                for c in range(8):
                    pb = B[3 + c // 4]
                    tok = slice(b * 512 + c * 64, b * 512 + (c + 1) * 64)
                    for k in range(8):
                        mm(P, pb[0:64, (c % 4) * 128:(c % 4 + 1) * 128], hT[:, k, tok],
                           whg[:, k, wofs + 384:wofs + 512], (c % 4 == 0 and k == 0), k == 7, skip=True)
                P.copy("act", V64[:, 0:4, :], B[3][0:64, :].rearrange("p (c n) -> p c n", c=4))
                P.copy("act", V64[:, 4:8, :], B[4][0:64, :].rearrange("p (c n) -> p c n", c=4))
                P.act(sig[:, :], B[1][:, :], AF.Sigmoid)
                P.act(gate[:, :], B[2][:, :], AF.Silu)
                P.ts("dve", ff[:, :], sig[:, :], lbc[:, 1:2], ALU.mult, lbc[:, 0:1], ALU.add)
                P.ts("dve", ff[:, :], ff[:, :], TINY, ALU.max)
                P.act(ff[:, :], ff[:, :], AF.Ln)
                P.ts("dve", kk[:, :], sig[:, :], lbc[:, 2:3], ALU.mult, lbc[:, 1:2], ALU.add)
                P.add("dve", lambda e: e.tensor_tensor_scan(bcum[:, :].ap, rmask[:, :].ap, ff[:, :].ap, 0.0,
                                                            ALU.mult, ALU.add), [rmask[:, :], ff[:, :]], [bcum[:, :]])
                P.act(eb[:, :], bcum[:, :], AF.Exp)
                P.act(enb[:, :], bcum[:, :], AF.Exp, scale=-1.0)
                P.tt("dve", qd[:, :], B[0][:, :], eb[:, :], ALU.mult)
                P.tt("dve", ktl[:, :], kk[:, :], enb[:, :], ALU.mult)
                P.tt("dve", kdT[:, :], kk[:, :], enb[:, :], ALU.mult)
                for c in range(8):
                    cs = slice(c * 64, (c + 1) * 64)
                    P.ts("dve", kdT[:, cs], kdT[:, cs], eb[:, c * 64 + 63:c * 64 + 64], ALU.mult)
                for half in range(2):
                    pb = B[5]
                    for c4 in range(4):
                        c = half * 4 + c4
                        P.transpose(pb[0:64, c4 * 128:(c4 + 1) * 128], kdT[:, c * 64:(c + 1) * 64], identf[:, :])
                    P.copy("act", kdec[:, half * 4:(half + 1) * 4, :], pb[0:64, :].rearrange("p (c n) -> p c n", c=4))
                for c in range(8):
                    cs = slice(c * 64, (c + 1) * 64)
                    mm(P, B[6][0:64, cs], ktl[:, cs], qd[:, cs], c == 0, True, skip=True)
                P.tt("dve", attn[:, :], B[6][0:64, :], mask[:, :], ALU.mult)
                for c in range(8):
                    cs = slice(c * 64, (c + 1) * 64)
                    mm(P, B[7][:, cs], V64[:, c, :], attn[:, cs], c == 0, False, skip=True)
                    mm(P, B[7][:, cs], state_bf[:, :], qd[:, cs], False, True, skip=True)
                    su = B[3] if c % 2 == 0 else B[4]
                    P.matmul(su[:, 0:128], kdec[:, c, :], V64[:, c, :], True, True)
                    P.stt(state[:, :], state[:, :], eb[:, c * 64 + 63:c * 64 + 64], su[:, 0:128], ALU.mult, ALU.add)
                    P.copy("act", state_bf[:, :], state[:, :])
                P.copy("act", oraw[:, :], B[7][:, :])
                P.act(osq[:, :], B[7][:, :], AF.Square)
                colnorm(P, cx, None, [osq[:, :]], B[6][:, :], 128, rstdA[:, :])
                P.stt(oraw[:, :], oraw[:, :], colsr[r][:, 11 + h:12 + h], rstdA[:, :], ALU.mult, ALU.mult)
                o = osb[b % 2]
                P.tt("dve", o[:, :], oraw[:, :], gate[:, :], ALU.mult)
                P.dma("sp", oT_d[4 + 2 * r + h][:, sl], o[:, :])
    cx.pst = [B[0], B[1], B[5]]
    cx.pso = [B[2]]
    cx.psd = [B[3]]
    scale = (MLA_NOPE + MLA_ROPE) ** -0.5
    for r in range(2):
        P.dma("pool", wuq[:, :, :], wuq_d[r].rearrange("k p n -> p k n"))
        P.dma("pool", wukv[:, :, :], wukv_d[r].rearrange("k p n -> p k n"))
        for h in range(2):
            for b in range(8):
                sl = slice(b * 512, (b + 1) * 512)
                for k in range(3):
                    P.matmul(B[4][:, :], wuq[:, k, h * 256:h * 256 + 128], cqn[:, k, sl], k == 0, k == 2)
                P.copy("act", qnT[:, sl], B[4][:, :])
                for k in range(3):
                    P.matmul(B[6][0:64, :], wuq[:, k, h * 256 + 128:h * 256 + 192], cqn[:, k, sl], k == 0, k == 2)
                for k in range(3):
                    P.matmul(B[7][0:64, :], wuq[:, k, h * 256 + 192:h * 256 + 256], cqn[:, k, sl], k == 0, k == 2)
                P.tt("dve", t1[0:64, :], B[6][0:64, :], Ct[:, sl], ALU.mult)
                P.tt("dve", t2[0:64, :], B[7][0:64, :], St[:, sl], ALU.mult)
                P.tt("dve", qrT[:, sl], t1[0:64, :], t2[0:64, :], ALU.add)
                for k in range(2):
                    P.matmul(B[5][:, :], wukv[:, k, h * 256:h * 256 + 128], ckvn[:, k, sl], k == 0, k == 1)
                P.copy("act", knT[:, sl], B[5][:, :])
                for tt_ in range(4):
                    tok = slice(b * 512 + tt_ * 128, b * 512 + (tt_ + 1) * 128)
                    for k in range(2):
                        mm(P, B[4][:, tt_ * 128:(tt_ + 1) * 128], ckvn[:, k, tok],
                           wukv[:, k, h * 256 + 128:h * 256 + 256], (tt_ == 0 and k == 0), k == 1, skip=True)
                P.copy("act", V[:, b * 4:(b + 1) * 4, :], B[4][:, :].rearrange("p (t n) -> p t n", t=4))

            def out_cb(g, h=h, r=r):
                o = osb[g % 2]
                P.recip(t1[:, :], cx.psd[0][:, :])
                P.tt("dve", o[:, :], cx.pso[0][:, :], t1[:, :], ALU.mult)
                P.dma("sp", oT_d[2 * r + h][:, g * 512:(g + 1) * 512], o[:, :])

            attention(P, cx, [[(knT, qnT), (kpeT, qrT)]], V, scale, out_cb)

    P.close_scope()


BF = ml_dtypes.bfloat16


def to_hT(h):
    s = h.shape[0]
    return np.ascontiguousarray(h.reshape(s, 8, 128).transpose(1, 2, 0))


def kchunk(w):
    return np.ascontiguousarray(w.reshape(w.shape[0] // 128, 128, w.shape[1]))


def const_tables():
    ones = np.ones((128, 128), dtype=BF)
    identf = np.eye(128, dtype=np.float32)
    mask = np.zeros((64, 512), dtype=np.float32)
    for c in range(8):
        mask[:, c * 64:(c + 1) * 64] = np.triu(np.ones((64, 64), dtype=np.float32))
    rmask = np.ones((128, 512), dtype=np.float32)
    rmask[:, ::64] = 0.0
    return ones, identf, mask, rmask


def inv_freq(dim):
    return (np.float32(ROPE_THETA) ** (-(np.arange(0, dim, 2, dtype=np.float32)) / np.float32(dim))).astype(np.float32)


def even_mixer_inputs(inp, j, r, h_T, pos):
    w_in = inp["ev_w_in"][j]
    kpe = w_in[:, 640:704]
    kpe_sw = np.concatenate([kpe[:, 32:64], kpe[:, 0:32]], axis=1)
    wlat = np.concatenate([w_in[:, 0:640], kpe, kpe_sw], axis=1)
    hg_parts = []
    for i in range(2):
        gh = 2 * r + i
        hg_parts += [w_in[:, 704 + gh * 128:704 + (gh + 1) * 128], w_in[:, 1216 + gh * 128:1216 + (gh + 1) * 128],
                     w_in[:, 2240 + gh * 128:2240 + (gh + 1) * 128], w_in[:, 1728 + gh * 128:1728 + (gh + 1) * 128]]
    whg = np.concatenate(hg_parts, axis=1)
    w_uq = inp["ev_w_uq"][j]
    w_ukv = inp["ev_w_ukv"][j]
    uq_parts, ukv_parts = [], []
    for i in range(2):
        gh = 2 * r + i
        rp = w_uq[:, gh * 192 + 128:gh * 192 + 192]
        uq_parts += [w_uq[:, gh * 192:gh * 192 + 128], rp, np.concatenate([rp[:, 32:64], rp[:, 0:32]], axis=1)]
        ukv_parts += [w_ukv[:, gh * 256:gh * 256 + 256]]
    wuq = np.concatenate(uq_parts, axis=1)
    wukv = np.concatenate(ukv_parts, axis=1)
    cols = np.zeros((128, 16), dtype=np.float32)
    cols[:, 0:3] = inp["ev_g_q"][j].reshape(3, 128).T
    cols[:, 3:5] = inp["ev_g_kv"][j].reshape(2, 128).T
    f = inv_freq(MLA_ROPE)
    cols[0:32, 5] = f
    cols[32:64, 5] = f
    cols[0:32, 6] = -1.0
    cols[32:64, 6] = 1.0
    for i in range(2):
        gh = 2 * r + i
        cols[:, 7 + i] = inp["ev_lb_logits"][0][gh * 128:(gh + 1) * 128]
        cols[:, 9 + i] = inp["ev_lb_logits"][1][gh * 128:(gh + 1) * 128]
        cols[:, 11 + i] = inp["ev_g_out"][j][gh]
    ones, identf, mask, rmask = const_tables()
    return {"cols": cols, "ones": ones, "identf": identf, "mask": mask, "rmask": rmask,
            "wlat": kchunk(np.ascontiguousarray(wlat)), "whg": kchunk(np.ascontiguousarray(whg)),
            "wuq": kchunk(np.ascontiguousarray(wuq)), "wukv": kchunk(np.ascontiguousarray(wukv))}


def emit_odd_mixer(P, B, G, layer):
    lambda_init = 0.8 - 0.6 * math.exp(-0.3 * layer)
    jj = layer // 2
    P.open_scope()
    cx = Ctx()
    hT_d = G["hT_s"]
    oT_d = G["oT_s"]
    pos_d = G["pos"]
    cols_d = G["colsO%d" % jj]
    lamp_d = G["lamp%d" % jj]
    ones_d = G["ones"]
    w_d = G["wodd%d" % jj]

    hT = P.sb("hT_sb", [128, 8, SEQ], BF16)
    w = P.sb("w_sb", [128, 8, 2560], BF16)
    colsr = [P.sb("cols_sb%d" % r, [128, 16], F32) for r in range(2)]
    cols = colsr[0]
    lamp = P.sb("lamp_sb", [128, 256], F32)
    lam = P.sb("lam", [128, 8], F32)
    cx.ones = P.sb("ones_sb", [128, 128], BF16)
    cx.epsb = P.sb("epsb", [128, 1], F32)
    cx.ki = P.sb("ki", [128, 512], I32)
    cx.posi = P.sb("posi", [128, 512], I32)
    cx.posf = P.sb("posf", [128, 512], F32)
    cx.ang = P.sb("ang", [128, 512], F32)
    cx.rtmp = P.sb("rtmp", [128, 512], F32)
    t1 = cx.posf
    t2 = cx.ang
    oraw = cx.rtmp
    Ct = P.sb("Ct", [128, SEQ], BF16)
    St = P.sb("St", [128, SEQ], BF16)
    qT = P.sb("qT", [128, SEQ], BF16)
    kT = P.sb("kT", [128, SEQ], BF16)
    V = P.sb("V", [128, 32, 128], BF16)
    cx.pT = [P.sb("pT%d" % i, [128, 512], BF16) for i in range(3)]
    osb = [P.sb("osb%d" % i, [128, 512], BF16) for i in range(2)]
    cx.acc = [[P.sb("acc%d_%d" % (c, i), [128, 512], F32) for i in range(2)] for c in range(2)]
    cx.onesf = P.sb("onesf", [128, 128], F32)
    P.memset("dve", cx.onesf[:, :], 1.0)
    osq = P.sb("osq", [128, 512], BF16)
    rstd = P.sb("rstd", [128, 512], F32)

    for r in range(2):
        P.dma("sp", colsr[r][:, :], cols_d[r])
    P.dma("sp", lamp[:, :], lamp_d[:, :])
    P.dma("sp", cx.ones[:, :], ones_d[:, :])
    P.memset("dve", cx.epsb[:, :], EPS)
    for k in range(8):
        P.dma("sp", hT[:, k, :], hT_d[k])
    P.stt(lamp[:, 0:64], lamp[:, 0:64], 1.0, lamp[:, 64:128], ALU.mult, ALU.mult, accum=lam[:, 0:1])
    P.stt(lamp[:, 128:192], lamp[:, 128:192], 1.0, lamp[:, 192:256], ALU.mult, ALU.mult, accum=lam[:, 1:2])
    P.act(lam[:, 2:3], lam[:, 0:1], AF.Exp)
    P.act(lam[:, 3:4], lam[:, 1:2], AF.Exp)
    P.tt("dve", lam[:, 4:5], lam[:, 3:4], lam[:, 2:3], ALU.subtract)
    P.ts("dve", lam[:, 5:6], lam[:, 4:5], -lambda_init, ALU.add)
    for r in range(2):
        P.ts("dve", colsr[r][:, 6:10], colsr[r][:, 2:6], 1.0 - lambda_init, ALU.mult)
    build_rope_tables(P, cx, pos_d, cols[:, 0:1], cols[:, 1:2], 128, Ct, St)

    cx.pst = [B[0], B[1], B[7]]
    cx.pso = [B[2], B[3]]
    cx.psd = [B[4], B[5]]
    scale = DF_DH ** -0.5
    for r in range(2):
        for k in range(8):
            P.dma("pool", w[:, k, :], w_d[r][k])
        for hh in range(4):
            wo = hh * 640
            for b in range(8):
                sl = slice(b * 512, (b + 1) * 512)
                for (dst, o0) in ((qT, 0), (kT, 256)):
                    for k in range(8):
                        P.matmul(B[6][:, :], w[:, k, wo + o0:wo + o0 + 128], hT[:, k, sl], k == 0, k == 7)
                    for k in range(8):
                        P.matmul(B[7][:, :], w[:, k, wo + o0 + 128:wo + o0 + 256], hT[:, k, sl], k == 0, k == 7)
                    P.tt("dve", t1[:, :], B[6][:, :], Ct[:, sl], ALU.mult)
                    P.tt("dve", t2[:, :], B[7][:, :], St[:, sl], ALU.mult)
                    P.tt("dve", dst[:, sl], t1[:, :], t2[:, :], ALU.add)
                for tt_ in range(4):
                    tok = slice(b * 512 + tt_ * 128, b * 512 + (tt_ + 1) * 128)
                    for k in range(8):
                        mm(P, B[6][:, tt_ * 128:(tt_ + 1) * 128], hT[:, k, tok],
                           w[:, k, wo + 512:wo + 640], (tt_ == 0 and k == 0), k == 7, skip=True)
                P.copy("act", V[:, b * 4:(b + 1) * 4, :], B[6][:, :].rearrange("p (t n) -> p t n", t=4))

            def out_cb(g, hh=hh, r=r):
                o = osb[g % 2]
                P.recip(t1[:, :], cx.psd[0][:, :])
                P.recip(t2[:, :], cx.psd[1][:, :])
                P.tt("dve", t1[:, :], cx.pso[0][:, :], t1[:, :], ALU.mult)
                P.tt("dve", t2[:, :], cx.pso[1][:, :], t2[:, :], ALU.mult)
                P.stt(oraw[:, :], t2[:, :], lam[:, 5:6], t1[:, :], ALU.mult, ALU.add)
                P.act(osq[:, :], oraw[:, :], AF.Square)
                colnorm(P, cx, None, [osq[:, :]], B[6][:, :], 128, rstd[:, :])
                P.stt(o[:, :], oraw[:, :], colsr[r][:, 6 + hh:7 + hh], rstd[:, :], ALU.mult, ALU.mult)
                P.dma("sp", oT_d[4 * r + hh][:, g * 512:(g + 1) * 512], o[:, :])

            pairs = [[(kT[0:64, :], qT[0:64, :])], [(kT[64:128, :], qT[64:128, :])]]
            attention(P, cx, pairs, V, scale, out_cb, ncomp=2)
    P.close_scope()


def odd_mixer_inputs(inp, jj, r, h_T, pos):
    w_in = inp["od_w_in"][jj]

    def sw(wc):
        out = wc.copy()
        for c0 in range(0, wc.shape[1], 64):
            out[:, c0:c0 + 8] = wc[:, c0 + 8:c0 + 16]
            out[:, c0 + 8:c0 + 16] = wc[:, c0:c0 + 8]
        return out

    parts = []
    for hh in range(4):
        gh = 4 * r + hh
        q = w_in[:, gh * 128:(gh + 1) * 128]
        k = w_in[:, 1024 + gh * 128:1024 + (gh + 1) * 128]
        v = w_in[:, 2048 + gh * 128:2048 + (gh + 1) * 128]
        parts += [q, sw(q), k, sw(k), v]
    w = np.concatenate(parts, axis=1)
    cols = np.zeros((128, 16), dtype=np.float32)
    f = inv_freq(DF_ROT)
    for blk in (0, 64):
        cols[blk:blk + 8, 0] = f
        cols[blk + 8:blk + 16, 0] = f
        cols[blk:blk + 8, 1] = -1.0
        cols[blk + 8:blk + 16, 1] = 1.0
    for hh in range(4):
        cols[:, 2 + hh] = inp["od_g_head"][jj][4 * r + hh]
    lamp = np.ascontiguousarray(np.broadcast_to(inp["od_lambda"][jj].reshape(1, 256), (128, 256)))
    ones = np.ones((128, 128), dtype=BF)
    return {"cols": cols, "lamp": lamp, "ones": ones, "w": kchunk(np.ascontiguousarray(w))}


def build_fused():
    nc = bass.Bass("TRN2", target_bir_lowering=False)
    P = Prog(nc)
    G = {}

    def ein(name, shape, dt=F32):
        G[name] = P.dram(name, shape, dt, "ExternalInput")

    ein("x", [SEQ // 128, 128, D_MODEL])
    ein("pos", [128, SEQ], I32)
    ein("ident", [128, 128], BF16)
    ein("ones", [128, 128], BF16)
    ein("identf", [128, 128])
    ein("mask", [64, 512])
    ein("rmask", [128, 512])
    ein("gains", [DEPTH * 6, 128, D_MODEL])
    for l in range(DEPTH):
        for i in range(2):
            ein("wg%d%d" % (l, i), [NJ, 128, 8, 128])
            ein("wu%d%d" % (l, i), [NJ, 128, 8, 128])
            ein("wd%d%d" % (l, i), [NJ, 128, D_MODEL])
        ein("wout%d" % l, [8, 128, D_MODEL])
    for j in range(DEPTH // 2):
        ein("wlat%d" % j, [8, 128, 768])
        ein("whg%d" % j, [2, 8, 128, 1024])
        ein("wuq%d" % j, [2, 3, 128, 512])
        ein("wukv%d" % j, [2, 2, 128, 512])
        ein("colsE%d" % j, [2, 128, 16])
        ein("wodd%d" % j, [2, 8, 128, 2560])
        ein("colsO%d" % j, [2, 128, 16])
        ein("lamp%d" % j, [128, 256])
    for g in range(NGRP):
        G["xs%d" % g] = P.dram("xs%d" % g, [8, 128, D_MODEL], F32, "Internal")
    G["hT_s"] = P.dram("hT_s", [8, 128, SEQ], BF16, "Internal")
    G["oT_s"] = P.dram("oT_s", [8, 128, SEQ], BF16, "Internal")
    G["xo"] = P.dram("xo", [SEQ // 128, 128, D_MODEL], F32, "ExternalOutput")
    B = [P.ps("B%d" % i, [128, 512]) for i in range(8)]
    for step in range(DEPTH + 1):
        emit_token_phase(P, B, G, step)
        if step < DEPTH:
            if step % 2 == 0:
                emit_even_mixer(P, B, G, step // 2)
            else:
                emit_odd_mixer(P, B, G, step)
    P.finalize()
    return nc


def rearr_ffn_w(W):
    return np.ascontiguousarray(W.reshape(8, 128, NJ, 128).transpose(2, 1, 0, 3))


def fused_common_inputs(inp):
    ones, identf, mask, rmask = const_tables()
    C = {"ident": np.eye(128, dtype=BF), "ones": ones, "identf": identf, "mask": mask, "rmask": rmask}
    g = inp["norm_g"].reshape(DEPTH * 6, 1, D_MODEL)
    C["gains"] = np.ascontiguousarray(np.broadcast_to(g, (DEPTH * 6, 128, D_MODEL)))
    for l in range(DEPTH):
        for i in range(2):
            C["wg%d%d" % (l, i)] = rearr_ffn_w(inp["ffn_w_gate"][l, i])
            C["wu%d%d" % (l, i)] = rearr_ffn_w(inp["ffn_w_up"][l, i])
            C["wd%d%d" % (l, i)] = np.ascontiguousarray(inp["ffn_w_down"][l, i].reshape(NJ, 128, D_MODEL))
        w_out = inp["ev_w_out"][l // 2] if l % 2 == 0 else inp["od_w_out"][l // 2]
        C["wout%d" % l] = kchunk(np.ascontiguousarray(w_out))
    for j in range(DEPTH // 2):
        e = [even_mixer_inputs(inp, j, r, None, None) for r in range(2)]
        C["wlat%d" % j] = e[0]["wlat"]
        for nm in ("whg", "wuq", "wukv"):
            C["%s%d" % (nm, j)] = np.ascontiguousarray(np.stack([e[0][nm], e[1][nm]], axis=0))
        C["colsE%d" % j] = np.ascontiguousarray(np.stack([e[0]["cols"], e[1]["cols"]], axis=0))
        o = [odd_mixer_inputs(inp, j, r, None, None) for r in range(2)]
        C["wodd%d" % j] = np.ascontiguousarray(np.stack([o[0]["w"], o[1]["w"]], axis=0))
        C["colsO%d" % j] = np.ascontiguousarray(np.stack([o[0]["cols"], o[1]["cols"]], axis=0))
        C["lamp%d" % j] = o[0]["lamp"]
    return C


_NC_CACHE = {}


def kernel(x, positions, norm_g, ffn_w_gate, ffn_w_up, ffn_w_down, ev_w_in, ev_g_q, ev_w_uq, ev_g_kv,
           ev_w_ukv, ev_lb_logits, ev_g_out, ev_w_out, od_w_in, od_lambda, od_g_head, od_w_out):
    inp = dict(x=x, positions=positions, norm_g=norm_g, ffn_w_gate=ffn_w_gate, ffn_w_up=ffn_w_up,
               ffn_w_down=ffn_w_down, ev_w_in=ev_w_in, ev_g_q=ev_g_q, ev_w_uq=ev_w_uq, ev_g_kv=ev_g_kv,
               ev_w_ukv=ev_w_ukv, ev_lb_logits=ev_lb_logits, ev_g_out=ev_g_out, ev_w_out=ev_w_out,
               od_w_in=od_w_in, od_lambda=od_lambda, od_g_head=od_g_head, od_w_out=od_w_out)
    inp = {k: np.asarray(v) for k, v in inp.items()}
    if "nc" not in _NC_CACHE:
        _NC_CACHE["nc"] = build_fused()
    nc = _NC_CACHE["nc"]
    C = fused_common_inputs(inp)
    cores = list(range(NCORES))
    in_maps = []
    for c in cores:
        b = c // 2
        m = dict(C)
        m["x"] = np.ascontiguousarray(inp["x"][b].astype(np.float32, copy=False)).reshape(SEQ // 128, 128, D_MODEL)
        m["pos"] = np.ascontiguousarray(np.broadcast_to(inp["positions"][b].astype(np.int32)[None, :], (128, SEQ)))
        in_maps.append(m)
    res = run_bass_kernel_spmd(nc, in_maps, core_ids=cores).results
    out = np.zeros((BATCH, SEQ, D_MODEL), dtype=np.float32)
    for b in range(BATCH):
        out[b] = np.asarray(res[2 * b]["xo"]).reshape(SEQ, D_MODEL)
    return out
```

```python
import math
import numpy as np
import ml_dtypes
import concourse.bass as bass
import concourse.mybir as mybir
from concourse.bass_utils import run_bass_kernel_spmd

F32 = mybir.dt.float32
BF16 = mybir.dt.bfloat16
I32 = mybir.dt.int32
AF = mybir.ActivationFunctionType
ALU = mybir.AluOpType
AX = mybir.AxisListType

D_MODEL = 1024
BATCH = 4
SEQ = 4096
DEPTH = 4
CHUNK = 64
ROPE_THETA = 500000.0
EPS = 1e-6
TINY = 1e-30
D_FF = 2816
NJ = D_FF // 128
MLA_NOPE, MLA_ROPE, MLA_V, MLA_Q_RANK, MLA_KV_RANK = 128, 64, 128, 384, 256
DF_DH = 64
DF_ROT = 16
NCORES = 8
TOK = SEQ // 2
NTT = TOK // 128


class View:
    __slots__ = ("tile", "ap")

    def __init__(self, tile, ap):
        self.tile = tile
        self.ap = ap

    def __getitem__(self, idx):
        return View(self.tile, self.ap[idx])

    def rearrange(self, s, **kw):
        return View(self.tile, self.ap.rearrange(s, **kw))

    def bitcast(self, dt):
        return View(self.tile, self.ap.bitcast(dt))


class Tile:
    def __init__(self, name, base_ap):
        self.name = name
        self.base = base_ap
        self.last_w = None
        self.readers = []

    def __getitem__(self, idx):
        return View(self, self.base[idx])

    def v(self):
        return View(self, self.base)


class Op:
    __slots__ = ("eng", "fn", "reads", "writes", "dma", "deps", "signal", "seq",
                 "sem", "val", "prev_dma")

    def __init__(self, eng, fn, reads, writes, dma):
        self.eng = eng
        self.fn = fn
        self.reads = reads
        self.writes = writes
        self.dma = dma
        self.deps = []
        self.signal = False
        self.seq = 0
        self.sem = None
        self.val = 0
        self.prev_dma = None


class Prog:
    ENGS = ("pe", "act", "dve", "pool", "sp")
    NDMASEM = 12

    def __init__(self, nc):
        self.nc = nc
        self.ops = []
        self.n_sb = 0
        self.stack = None
        self.uid = 0

    def open_scope(self):
        import contextlib
        self.stack = contextlib.ExitStack()

    def close_scope(self):
        self.stack.close()
        self.stack = None
        self.ops.append("BARRIER")

    def sb(self, name, shape, dtype):
        if self.stack is not None:
            self.uid += 1
            h = self.stack.enter_context(self.nc.sbuf_tensor("sb%d_%s" % (self.uid, name), list(shape), dtype))
        else:
            h = self.nc.alloc_sbuf_tensor("sb_" + name, list(shape), dtype)
        idx = tuple(slice(None) for _ in shape)
        return Tile(name, h[idx])

    def ps(self, name, shape, dtype=F32):
        h = self.nc.alloc_psum_tensor("ps_" + name, list(shape), dtype)
        idx = tuple(slice(None) for _ in shape)
        return Tile(name, h[idx])

    def dram(self, name, shape, dtype, kind):
        h = self.nc.dram_tensor(name, list(shape), dtype, kind=kind)
        return Tile(name, h.ap())

    def add(self, eng, fn, reads, writes, dma=False):
        rt = []
        for r in reads:
            if isinstance(r, View) and r.tile not in rt:
                rt.append(r.tile)
        wt = []
        for w in writes:
            if isinstance(w, View) and w.tile not in wt:
                wt.append(w.tile)
        op = Op(eng, fn, rt, wt, dma)
        self.ops.append(op)
        return op

    def dma(self, q, out, in_):
        return self.add(q, lambda e: e.dma_start(out=out.ap, in_=in_.ap), [in_], [out], dma=True)

    def matmul(self, out, lhsT, rhs, start, stop):
        return self.add("pe", lambda e: e.matmul(out.ap, lhsT.ap, rhs.ap, start=start, stop=stop),
                        [lhsT, rhs], [out])

    def transpose(self, out, in_, ident):
        return self.add("pe", lambda e: e.transpose(out.ap, in_.ap, ident.ap), [in_, ident], [out])

    def act(self, out, in_, func, bias=None, scale=None, accum=None, eng="act"):
        reads = [in_]
        writes = [out]
        kw = {}
        if bias is not None:
            if isinstance(bias, View):
                reads.append(bias)
                kw["bias"] = bias.ap
            else:
                kw["bias"] = float(bias)
        if scale is not None:
            if isinstance(scale, View):
                reads.append(scale)
                kw["scale"] = scale.ap
            else:
                kw["scale"] = float(scale)
        if accum is not None:
            writes.append(accum)
            kw["accum_out"] = accum.ap
        return self.add(eng, lambda e: e.activation(out.ap, in_.ap, func, **kw), reads, writes)

    def tt(self, eng, out, in0, in1, op):
        return self.add(eng, lambda e: e.tensor_tensor(out.ap, in0.ap, in1.ap, op), [in0, in1], [out])

    def ts(self, eng, out, in0, s1, op0, s2=None, op1=None, accum=None):
        reads = [in0]
        a1 = s1.ap if isinstance(s1, View) else float(s1)
        if isinstance(s1, View):
            reads.append(s1)
        a2 = None
        if s2 is not None:
            a2 = s2.ap if isinstance(s2, View) else float(s2)
            if isinstance(s2, View):
                reads.append(s2)
        writes = [out]
        kw = {}
        if accum is not None:
            writes.append(accum)
            kw["accum_out"] = accum.ap
        o1 = op1 if op1 is not None else ALU.bypass
        return self.add(eng, lambda e: e.tensor_scalar(out.ap, in0.ap, a1, a2, op0, o1, **kw), reads, writes)

    def stt(self, out, in0, scalar, in1, op0, op1, accum=None):
        reads = [in0, in1]
        a = scalar.ap if isinstance(scalar, View) else float(scalar)
        if isinstance(scalar, View):
            reads.append(scalar)
        writes = [out]
        kw = {}
        if accum is not None:
            writes.append(accum)
            kw["accum_out"] = accum.ap
        return self.add("dve", lambda e: e.scalar_tensor_tensor(out.ap, in0.ap, a, in1.ap, op0, op1, **kw),
                        reads, writes)

    def copy(self, eng, out, in_):
        if eng == "act":
            return self.add("act", lambda e: e.copy(out.ap, in_.ap), [in_], [out])
        return self.add(eng, lambda e: e.tensor_copy(out.ap, in_.ap), [in_], [out])

    def memset(self, eng, out, val):
        return self.add(eng, lambda e: e.memset(out.ap, val), [], [out])

    def recip(self, out, in_):
        return self.add("dve", lambda e: e.reciprocal(out.ap, in_.ap), [in_], [out])

    def finalize(self):
        nc = self.nc
        ops = self.ops
        real = []
        fence = []
        last_eng = {}
        dma_q = {}
        for op in ops:
            if isinstance(op, str):
                fence = [o for o in last_eng.values()]
                for q, lst in dma_q.items():
                    fence.extend(lst[-self.NDMASEM:])
                continue
            real.append(op)
            if op.dma:
                dma_q.setdefault(op.eng, []).append(op)
            else:
                last_eng[op.eng] = op
            op.deps.extend(fence)
        ops = real
        self.ops = real
        for op in ops:
            deps = list(op.deps)
            op.deps = []
            raw = set()
            for t in op.reads:
                if t.last_w is not None:
                    deps.append(t.last_w)
                    raw.add(id(t.last_w))
            for t in op.writes:
                if t.last_w is not None:
                    deps.append(t.last_w)
                deps.extend(t.readers)
            seen = set()
            for d in deps:
                if d is op or id(d) in seen:
                    continue
                seen.add(id(d))
                if (not d.dma) and d.eng == op.eng and op.eng == "pe":
                    continue
                op.deps.append(d)
            for t in op.reads:
                t.readers.append(op)
            for t in op.writes:
                t.last_w = op
                t.readers = []
        for op in ops:
            for d in op.deps:
                if not d.dma:
                    d.signal = True
        cnt = {e: 0 for e in self.ENGS}
        dcnt = {e: 0 for e in self.ENGS}
        dma_hist = {e: [] for e in self.ENGS}
        esem = {}
        dsem = {}
        for e in self.ENGS:
            esem[e] = nc.alloc_semaphore("s_" + e)
        for op in ops:
            if op.dma:
                q = op.eng
                if q not in dsem:
                    dsem[q] = [nc.alloc_semaphore("d_%s_%d" % (q, i)) for i in range(self.NDMASEM)]
                i = dcnt[q]
                dcnt[q] += 1
                op.sem = dsem[q][i % self.NDMASEM]
                op.val = 16 * (i // self.NDMASEM + 1)
                if i >= self.NDMASEM:
                    op.prev_dma = dma_hist[q][i - self.NDMASEM]
                dma_hist[q].append(op)
            elif op.signal:
                cnt[op.eng] += 1
                op.seq = cnt[op.eng]
                op.sem = esem[op.eng]
                op.val = op.seq
        per_eng = {e: [] for e in self.ENGS}
        for op in ops:
            per_eng[op.eng].append(op)
        last_dma = {q: h for q, h in dma_hist.items() if h}

        def emit(eng_name, e):
            waited = {}
            for op in per_eng[eng_name]:
                need = {}
                dl = list(op.deps)
                if op.prev_dma is not None:
                    dl.append(op.prev_dma)
                for d in dl:
                    k = d.sem.num
                    if waited.get(k, 0) >= d.val:
                        continue
                    if k not in need or need[k][1] < d.val:
                        need[k] = (d.sem, d.val)
                for k, (s, v) in need.items():
                    e.wait_ge(s, v)
                    waited[k] = v
                ins = op.fn(e)
                if op.dma:
                    ins.then_inc(op.sem, 16)
                elif op.signal:
                    ins.then_inc(op.sem, 1)
            if eng_name in last_dma:
                fin = {}
                for d in last_dma[eng_name]:
                    k = d.sem.num
                    if k not in fin or fin[k][1] < d.val:
                        fin[k] = (d.sem, d.val)
                for k, (s, v) in fin.items():
                    if waited.get(k, 0) < v:
                        e.wait_ge(s, v)

        with nc.Block() as block:
            @block.tensor
            def _(e):
                emit("pe", e)

            @block.scalar
            def _(e):
                emit("act", e)

            @block.vector
            def _(e):
                emit("dve", e)

            @block.gpsimd
            def _(e):
                emit("pool", e)

            @block.sync
            def _(e):
                emit("sp", e)


class Ctx:
    pass


def rms_stats(P, cx, src, d, rstd):
    P.act(cx.junk[:, 0:d], src, AF.Square, accum=cx.ss[:, 0:1])
    P.act(cx.ss[:, 1:2], cx.ss[:, 0:1], AF.Sqrt, bias=cx.epsb[:, 0:1], scale=1.0 / d)
    P.recip(rstd, cx.ss[:, 1:2])


def norm_T_stage(P, cx, xt, g, sink):
    nt = len(xt)

    def stats(t):
        rs = cx.rstdN[:, t:t + 1]
        P.act(cx.junk[:, :], xt[t][:, :], AF.Square, accum=cx.ssN[:, 2 * t:2 * t + 1])
        P.act(cx.ssN[:, 2 * t + 1:2 * t + 2], cx.ssN[:, 2 * t:2 * t + 1], AF.Sqrt,
              bias=cx.epsb[:, 0:1], scale=1.0 / D_MODEL)
        P.recip(rs, cx.ssN[:, 2 * t + 1:2 * t + 2])
        P.stt(cx.hnb[t % 2][:, :], xt[t][:, :], rs, g[:, :], ALU.mult, ALU.mult)

    def trans(t):
        pt = cx.ptr[t % 2]
        hn = cx.hnb[t % 2]
        for kc in range(8):
            P.transpose(pt[:, kc * 128:(kc + 1) * 128], hn[:, kc * 128:(kc + 1) * 128], cx.ident[:, :])
        sink(t, pt)

    for t in range(nt + 1):
        if t < nt:
            stats(t)
        if t >= 1:
            trans(t - 1)


STAGE = 9


def ffn_group(P, cx, xt, W, gpre, gpost, name):
    nt = len(xt)
    nh = nt // 4
    def sink(t, pt):
        P.copy("act", cx.hT[t // 4][:, :, (t % 4) * 128:(t % 4 + 1) * 128],
               pt.rearrange("p (k n) -> p k n", k=8))

    norm_T_stage(P, cx, xt, gpre, sink)
    if STAGE < 2:
        return
    for j in range(NJ):
        wg = cx.wg[j % len(cx.wg)]
        wu = cx.wu[j % len(cx.wu)]
        P.dma("pool", wg[:, :, :], W["wg"][j])
        P.dma("pool", wu[:, :, :], W["wu"][j])
        P.dma("pool", cx.wd[j][:, :], W["wd_d"][j])
        for h in range(nh):
            pg = cx.pg[(j * nh + h) % 2]
            pu = cx.pu[(j * nh + h) % 2]
            for kc in range(8):
                P.matmul(pg[:, :], wg[:, kc, :], cx.hT[h][:, kc, :], kc == 0, kc == 7)
            for kc in range(8):
                P.matmul(pu[:, :], wu[:, kc, :], cx.hT[h][:, kc, :], kc == 0, kc == 7)
            sg = cx.sg[(j * nh + h) % 2]
            P.act(sg[:, :], pg[:, :], AF.Silu)
            P.tt("dve", cx.aT[j][:, h * 512:(h + 1) * 512], sg[:, :], pu[:, :], ALU.mult)
    if STAGE < 3:
        return
    for t in range(nt):
        y = cx.y[t % 2]
        for n in range(2):
            pd = cx.pd[(t * 2 + n) % 2]
            for j in range(NJ):
                P.matmul(pd[:, :], cx.aT[j][:, t * 128:(t + 1) * 128],
                         W["wd"][j][:, n * 512:(n + 1) * 512], j == 0, j == NJ - 1)
            P.copy("act", y[:, n * 512:(n + 1) * 512], pd[:, :])
        norm_residual(P, cx, xt[t], y, gpost, 0.5)


def norm_residual(P, cx, x, y, g, alpha):
    rms_stats(P, cx, y[:, :], D_MODEL, cx.rstd[:, 1:2])
    P.stt(y[:, :], y[:, :], cx.rstd[:, 1:2], g[:, :], ALU.mult, ALU.mult)
    P.stt(x[:, :], y[:, :], float(alpha), x[:, :], ALU.mult, ALU.add)


def load_wd(P, cx, wd_dram):
    for j in range(NJ):
        P.dma("pool", cx.wd[j][:, :], wd_dram[j])


NGRP = SEQ // 1024


def emit_token_phase(P, B, G, step):
    do_post = step > 0
    do_pre = step < DEPTH
    P.open_scope()
    cx = Ctx()
    cx.ident = P.sb("ident_sb", [128, 128], BF16)
    cx.ss = P.sb("ss", [128, 2], F32)
    cx.rstd = P.sb("rstd", [128, 2], F32)
    cx.epsb = P.sb("epsb", [128, 1], F32)
    cx.hnb = [P.sb("hn%d" % i, [128, D_MODEL], BF16) for i in range(2)]
    cx.ssN = P.sb("ssN", [128, 32], F32)
    cx.rstdN = P.sb("rstdN", [128, 16], F32)
    cx.hT = [P.sb("hT%d" % i, [128, 8, 512], BF16) for i in range(2)]
    cx.wg = [P.sb("wg%d" % i, [128, 8, 128], BF16) for i in range(3)]
    cx.wu = [P.sb("wu%d" % i, [128, 8, 128], BF16) for i in range(3)]
    cx.wd = [P.sb("wd%d" % j, [128, D_MODEL], BF16) for j in range(NJ)]
    cx.aT = [P.sb("aT%d" % j, [128, 1024], BF16) for j in range(NJ)]
    cx.sg = [P.sb("sg%d" % i, [128, 512], F32) for i in range(2)]
    cx.junk = cx.sg[0][:, :].bitcast(BF16)
    cx.y = [P.sb("y%d" % i, [128, D_MODEL], F32) for i in range(2)]
    gt = [P.sb("g%d" % i, [128, D_MODEL], F32) for i in range(6)]
    xt = [P.sb("x%d" % i, [128, D_MODEL], F32) for i in range(8)]
    if do_post:
        wout = [P.sb("wout%d" % k, [128, D_MODEL], BF16) for k in range(8)]
    if do_pre:
        hTo = [P.sb("hTo0", [128, 8, 128], BF16),
               cx.sg[1][:, :].bitcast(BF16).rearrange("p (k n) -> p k n", k=8)]
    cx.pg = [B[0], B[1]]
    cx.pu = [B[2], B[3]]
    cx.pd = [B[4], B[5]]
    cx.ptr = [B[6][:, :].bitcast(BF16), B[7][:, :].bitcast(BF16)]

    P.dma("sp", cx.ident[:, :], G["ident"][:, :])
    P.memset("dve", cx.epsb[:, :], EPS)
    Wpost = Wpre = None
    if do_post:
        l = step - 1
        for i in range(3):
            P.dma("sp", gt[i][:, :], G["gains"][l * 6 + 3 + i])
        for k in range(8):
            P.dma("pool", wout[k][:, :], G["wout%d" % l][k])
        Wpost = {"wg": G["wg%d1" % l], "wu": G["wu%d1" % l], "wd": cx.wd, "wd_d": G["wd%d1" % l]}
    if do_pre:
        l = step
        for i in range(3):
            P.dma("sp", gt[3 + i][:, :], G["gains"][l * 6 + i])
        Wpre = {"wg": G["wg%d0" % l], "wu": G["wu%d0" % l], "wd": cx.wd, "wd_d": G["wd%d0" % l]}

    for grp in range(NGRP):
        for t in range(8):
            src = G["x"][grp * 8 + t] if step == 0 else G["xs%d" % grp][t]
            P.dma("sp", xt[t][:, :], src)
        if do_post:
            l = step - 1
            for h2 in range(2):
                P.dma("sp", cx.hT[h2][:, :, :],
                      G["oT_s"][:, :, grp * 1024 + h2 * 512:grp * 1024 + (h2 + 1) * 512].rearrange("k p n -> p k n"))
            for t in range(8):
                y = cx.y[t % 2]
                for n in range(2):
                    pd = cx.pd[(t * 2 + n) % 2]
                    for k in range(8):
                        P.matmul(pd[:, :], cx.hT[t // 4][:, k, (t % 4) * 128:(t % 4 + 1) * 128],
                                 wout[k][:, n * 512:(n + 1) * 512], k == 0, k == 7)
                    P.copy("act", y[:, n * 512:(n + 1) * 512], pd[:, :])
                norm_residual(P, cx, xt[t], y, gt[0], 1.0)
            ffn_group(P, cx, xt, Wpost, gt[1], gt[2], "f2")
        if do_pre:
            l = step
            ffn_group(P, cx, xt, Wpre, gt[3], gt[4], "f1")
            def hsink(t, pt, grp=grp):
                ho = hTo[t % 2]
                P.copy("act", ho[:, :, :], pt.rearrange("p (k n) -> p k n", k=8))
                tok0 = grp * 1024 + t * 128
                P.dma("sp", G["hT_s"][:, :, tok0:tok0 + 128].rearrange("k p n -> p k n"), ho[:, :, :])

            norm_T_stage(P, cx, xt, gt[5], hsink)
        for t in range(8):
            dst = G["xo"][grp * 8 + t] if step == DEPTH else G["xs%d" % grp][t]
            P.dma("sp", dst, xt[t][:, :])
    P.close_scope()


def mm(P, out, lhsT, rhs, start, stop, skip=False):
    if skip:
        return P.add("pe", lambda e: e.matmul(out.ap, lhsT.ap, rhs.ap, start=start, stop=stop,
                                              skip_group_check=True), [lhsT, rhs], [out])
    return P.matmul(out, lhsT, rhs, start, stop)


def build_rope_tables(P, cx, pos_d, freq_col, sgn_col, nrows, Ct, St):
    PI = math.pi
    PIS = 3.1415925
    n = nrows
    for b in range(8):
        sl = slice(b * 512, (b + 1) * 512)
        P.dma("sp", cx.posi[0:n, :], pos_d[0:n, sl])
        P.copy("dve", cx.posf[0:n, :], cx.posi[0:n, :])
        P.ts("dve", cx.ang[0:n, :], cx.posf[0:n, :], freq_col, ALU.mult)
        for shift, dst, sg in ((0.0, St, sgn_col), (0.5 * PI, Ct, None)):
            if shift != 0.0:
                P.ts("dve", cx.ang[0:n, :], cx.ang[0:n, :], shift, ALU.add)
            P.ts("dve", cx.rtmp[0:n, :], cx.ang[0:n, :], 1.0 / (2 * PI), ALU.mult)
            P.copy("dve", cx.ki[0:n, :], cx.rtmp[0:n, :])
            P.copy("dve", cx.rtmp[0:n, :], cx.ki[0:n, :])
            P.stt(cx.rtmp[0:n, :], cx.rtmp[0:n, :], -2 * PI, cx.ang[0:n, :], ALU.mult, ALU.add)
            P.ts("dve", cx.posf[0:n, :], cx.rtmp[0:n, :], PI, ALU.is_gt, 2 * PI, ALU.mult)
            P.tt("dve", cx.rtmp[0:n, :], cx.rtmp[0:n, :], cx.posf[0:n, :], ALU.subtract)
            P.ts("dve", cx.rtmp[0:n, :], cx.rtmp[0:n, :], PIS, ALU.min, -PIS, ALU.max)
            if sg is not None:
                P.act(cx.rtmp[0:n, :], cx.rtmp[0:n, :], AF.Sin)
                P.ts("dve", dst[0:n, sl], cx.rtmp[0:n, :], sg, ALU.mult)
            else:
                P.act(dst[0:n, sl], cx.rtmp[0:n, :], AF.Sin)


def attention(P, cx, qk_pairs, V, scale, out_cb, ncomp=1):
    its = []
    for g in range(8):
        nk = 4 * (g + 1)
        for kt in range(nk):
            for c in range(ncomp):
                its.append((g, kt, c, nk))
    nb = len(cx.pst)

    def front(i):
        g, kt, c, nk = its[i]
        a = kt - 4 * g
        q0 = 128 * a if a > 0 else 0
        st = cx.pst[i % nb]
        pT = cx.pT[i % nb]
        prs = qk_pairs[c]
        for n_, (kT, qT) in enumerate(prs):
            P.matmul(st[:, q0:512], kT[:, kt * 128:(kt + 1) * 128],
                     qT[:, g * 512 + q0:(g + 1) * 512], n_ == 0, n_ == len(prs) - 1)
        P.act(pT[:, q0:512], st[:, q0:512], AF.Exp, scale=scale)
        if a >= 0:
            P.memset("dve", pT[64:128, q0:q0 + 64], 0.0)

    def back(i):
        g, kt, c, nk = its[i]
        a = kt - 4 * g
        q0 = 128 * a if a > 0 else 0
        pT = cx.pT[i % nb]
        mm(P, cx.pso[c][:, q0:512], V[:, kt, :], pT[:, q0:512], kt == 0, kt == nk - 1, skip=True)
        if cx.sum_pe[c]:
            mm(P, cx.psd[c][:, q0:512], cx.ones[:, :], pT[:, q0:512], kt == 0, kt == nk - 1, skip=True)
        else:
            acc = cx.acc[c][g % 2]
            if kt == 0:
                P.copy("dve", acc[:, :], pT[:, :])
            else:
                P.tt("dve", acc[:, q0:512], acc[:, q0:512], pT[:, q0:512], ALU.add)
        if kt == nk - 1 and c == ncomp - 1:
            for c2 in range(ncomp):
                if not cx.sum_pe[c2]:
                    P.matmul(cx.psd[c2][:, :], cx.onesf[:, :], cx.acc[c2][g % 2][:, :], True, True)
            out_cb(g)

    SK = nb - 1
    n = len(its)
    for i in range(n + SK):
        if i < n:
            front(i)
        if i - SK >= 0:
            back(i - SK)


def colnorm(P, cx, raws, sqs, ps_ss, d, rstd_out):
    for i, sq in enumerate(sqs):
        P.matmul(ps_ss, cx.ones[:, :], sq, i == 0, i == len(sqs) - 1)
    P.act(rstd_out, ps_ss, AF.Sqrt, bias=cx.epsb[:, 0:1], scale=1.0 / d)
    P.recip(rstd_out, rstd_out)


def emit_even_mixer(P, B, G, j):
    P.open_scope()
    cx = Ctx()
    hT_d = G["hT_s"]
    oT_d = G["oT_s"]
    pos_d = G["pos"]
    cols_d = G["colsE%d" % j]
    ones_d, identf_d, mask_d, rmask_d = G["ones"], G["identf"], G["mask"], G["rmask"]
    wlat_d, whg_d, wuq_d, wukv_d = G["wlat%d" % j], G["whg%d" % j], G["wuq%d" % j], G["wukv%d" % j]

    hT = P.sb("hT_sb", [128, 8, SEQ], BF16)
    colsr = [P.sb("cols_sb%d" % r, [128, 16], F32) for r in range(2)]
    cols = colsr[0]
    cx.ones = P.sb("ones_sb", [128, 128], BF16)
    identf = P.sb("identf_sb", [128, 128], F32)
    mask = P.sb("mask_sb", [64, 512], F32)
    rmask = P.sb("rmask_sb", [128, 512], F32)
    wlat = P.sb("wlat_sb", [128, 8, 768], BF16)
    whg = P.sb("whg_sb", [128, 8, 1024], BF16)
    wuq = P.sb("wuq_sb", [128, 3, 512], BF16)
    wukv = P.sb("wukv_sb", [128, 2, 512], BF16)
    cx.epsb = P.sb("epsb", [128, 1], F32)
    cx.ki = P.sb("ki", [128, 512], I32)
    cx.posi = P.sb("posi", [128, 512], I32)
    sig = P.sb("sig", [128, 512], F32)
    ff = P.sb("ff", [128, 512], F32)
    kk = P.sb("kk", [128, 512], F32)
    bcum = P.sb("bcum", [128, 512], F32)
    eb = P.sb("eb", [128, 512], F32)
    enb = P.sb("enb", [128, 512], F32)
    kdT = P.sb("kdT", [128, 512], F32)
    gate = P.sb("gate", [128, 512], F32)
    cx.posf = sig
    cx.ang = ff
    cx.rtmp = kk
    Ct = P.sb("Ct", [64, SEQ], BF16)
    St = P.sb("St", [64, SEQ], BF16)
    cqn = P.sb("cqn", [128, 3, SEQ], BF16)
    ckvn = P.sb("ckvn", [128, 2, SEQ], BF16)
    kpeT = P.sb("kpeT", [128, SEQ], BF16)
    raw = [bcum, eb, enb, kdT, gate]
    rstdA = P.sb("rstdA", [128, 512], F32)
    rstdB = ff
    t1 = kk
    t2 = sig
    qnT = hT[:, 0, :]
    knT = hT[:, 1, :]
    qrT = hT[:, 2, :]
    V = hT[:, 3, :].rearrange("p (t n) -> p t n", t=32)
    cx.pT = [P.sb("pT%d" % i, [128, 512], BF16) for i in range(3)]
    osb = [P.sb("osb%d" % i, [128, 512], BF16) for i in range(2)]
    sq = [cx.pT[0], cx.pT[1], cx.pT[2], osb[0], osb[1]]
    cx.acc = [[P.sb("acc%d" % i, [128, 512], F32) for i in range(2)]]
    cx.onesf = P.sb("onesf", [128, 128], F32)
    P.memset("dve", cx.onesf[:, :], 1.0)

    for r in range(2):
        P.dma("sp", colsr[r][:, :], cols_d[r])
    P.dma("sp", cx.ones[:, :], ones_d[:, :])
    P.dma("sp", identf[:, :], identf_d[:, :])
    P.dma("sp", mask[:, :], mask_d[:, :])
    P.dma("sp", rmask[:, :], rmask_d[:, :])
    P.memset("dve", cx.epsb[:, :], EPS)
    P.dma("pool", wlat[:, :, :], wlat_d[:, :, :].rearrange("k p n -> p k n"))
    for k in range(8):
        P.dma("sp", hT[:, k, :], hT_d[k])
    build_rope_tables(P, cx, pos_d, cols[0:64, 5:6], cols[0:64, 6:7], 64, Ct, St)

    P.memset("dve", kpeT[64:128, :], 0.0)
    for b in range(8):
        sl = slice(b * 512, (b + 1) * 512)
        for oc in range(5):
            pb = B[oc % 4]
            for k in range(8):
                P.matmul(pb[:, :], wlat[:, k, oc * 128:(oc + 1) * 128], hT[:, k, sl], k == 0, k == 7)
            P.copy("act", raw[oc][:, :], pb[:, :])
            P.act(sq[oc][:, :], pb[:, :], AF.Square)
        colnorm(P, cx, raw[0:3], [s[:, :] for s in sq[0:3]], B[4][:, :], MLA_Q_RANK, rstdA[:, :])
        colnorm(P, cx, raw[3:5], [s[:, :] for s in sq[3:5]], B[5][:, :], MLA_KV_RANK, rstdB[:, :])
        for c in range(3):
            P.stt(cqn[:, c, sl], raw[c][:, :], cols[:, c:c + 1], rstdA[:, :], ALU.mult, ALU.mult)
        for c in range(2):
            P.stt(ckvn[:, c, sl], raw[3 + c][:, :], cols[:, 3 + c:4 + c], rstdB[:, :], ALU.mult, ALU.mult)
        for k in range(8):
            P.matmul(B[6][0:64, :], wlat[:, k, 640:704], hT[:, k, sl], k == 0, k == 7)
        for k in range(8):
            P.matmul(B[7][0:64, :], wlat[:, k, 704:768], hT[:, k, sl], k == 0, k == 7)
        P.tt("dve", t1[0:64, :], B[6][0:64, :], Ct[:, sl], ALU.mult)
        P.tt("dve", t2[0:64, :], B[7][0:64, :], St[:, sl], ALU.mult)
        P.tt("dve", kpeT[0:64, sl], t1[0:64, :], t2[0:64, :], ALU.add)

    lbc = P.sb("lbc", [128, 4], F32)
    state = P.sb("state", [128, 128], F32)
    state_bf = P.sb("state_bf", [128, 128], BF16)
    qd = P.sb("qd", [128, 512], BF16)
    ktl = P.sb("ktl", [128, 512], BF16)
    kdec = P.sb("kdec", [128, 8, 128], BF16)
    V64 = P.sb("V64", [128, 8, 128], BF16)
    attn = P.sb("attn", [128, 512], BF16)
    P.memset("dve", kdec[64:128, :, :], 0.0)
    P.memset("dve", V64[64:128, :, :], 0.0)
    P.memset("dve", attn[64:128, :], 0.0)
    oraw = bcum
    osq = P.sb("osq", [128, 512], BF16)
    for r in range(2):
        P.dma("pool", whg[:, :, :], whg_d[r].rearrange("k p n -> p k n"))
        for h in range(2):
            wofs = h * 512
            if j == 0:
                P.memset("dve", lbc[:, 0:1], 0.0)
            else:
                P.tt("dve", lbc[:, 3:4], colsr[r][:, 9 + h:10 + h], colsr[r][:, 7 + h:8 + h], ALU.subtract)
                P.act(lbc[:, 0:1], lbc[:, 3:4], AF.Sigmoid)
            P.ts("dve", lbc[:, 1:2], lbc[:, 0:1], -1.0, ALU.mult, 1.0, ALU.add)
            P.ts("dve", lbc[:, 2:3], lbc[:, 1:2], -1.0, ALU.mult)
            P.memset("dve", state[:, :], 0.0)
            P.memset("dve", state_bf[:, :], 0.0)
            for b in range(8):
                sl = slice(b * 512, (b + 1) * 512)
                for i, pb in enumerate((B[0], B[1], B[2])):
                    for k in range(8):
                        P.matmul(pb[:, :], whg[:, k, wofs + i * 128:wofs + (i + 1) * 128], hT[:, k, sl], k == 0, k == 7)
                for c in range(8):
                    pb = B[3 + c // 4]
                    tok = slice(b * 512 + c * 64, b * 512 + (c + 1) * 64)
                    for k in range(8):
                        mm(P, pb[0:64, (c % 4) * 128:(c % 4 + 1) * 128], hT[:, k, tok],
                           whg[:, k, wofs + 384:wofs + 512], (c % 4 == 0 and k == 0), k == 7, skip=True)
                P.copy("act", V64[0:64, 0:4, :], B[3][0:64, :].rearrange("p (c n) -> p c n", c=4))
                P.copy("act", V64[0:64, 4:8, :], B[4][0:64, :].rearrange("p (c n) -> p c n", c=4))
                P.act(sig[:, :], B[1][:, :], AF.Sigmoid)
                P.act(gate[:, :], B[2][:, :], AF.Silu)
                P.ts("dve", ff[:, :], sig[:, :], lbc[:, 1:2], ALU.mult, lbc[:, 0:1], ALU.add)
                P.ts("dve", ff[:, :], ff[:, :], TINY, ALU.max)
                P.act(ff[:, :], ff[:, :], AF.Ln)
                P.ts("dve", kk[:, :], sig[:, :], lbc[:, 2:3], ALU.mult, lbc[:, 1:2], ALU.add)
                P.add("dve", lambda e: e.tensor_tensor_scan(bcum[:, :].ap, rmask[:, :].ap, ff[:, :].ap, 0.0,
                                                            ALU.mult, ALU.add), [rmask[:, :], ff[:, :]], [bcum[:, :]])
                P.act(eb[:, :], bcum[:, :], AF.Exp)
                P.act(enb[:, :], bcum[:, :], AF.Exp, scale=-1.0)
                P.tt("dve", qd[:, :], B[0][:, :], eb[:, :], ALU.mult)
                P.tt("dve", ktl[:, :], kk[:, :], enb[:, :], ALU.mult)
                P.tt("dve", kdT[:, :], kk[:, :], enb[:, :], ALU.mult)
                for c in range(8):
                    cs = slice(c * 64, (c + 1) * 64)
                    P.ts("dve", kdT[:, cs], kdT[:, cs], eb[:, c * 64 + 63:c * 64 + 64], ALU.mult)
                for half in range(2):
                    pb = B[5]
                    for c4 in range(4):
                        c = half * 4 + c4
                        P.transpose(pb[0:64, c4 * 128:(c4 + 1) * 128], kdT[:, c * 64:(c + 1) * 64], identf[:, :])
                    P.copy("act", kdec[0:64, half * 4:(half + 1) * 4, :], pb[0:64, :].rearrange("p (c n) -> p c n", c=4))
                for c in range(8):
                    cs = slice(c * 64, (c + 1) * 64)
                    mm(P, B[6][0:64, cs], ktl[:, cs], qd[:, cs], c == 0, True, skip=True)
                P.tt("dve", attn[0:64, :], B[6][0:64, :], mask[:, :], ALU.mult)
                for c in range(8):
                    cs = slice(c * 64, (c + 1) * 64)
                    mm(P, B[7][:, cs], V64[:, c, :], attn[:, cs], c == 0, False, skip=True)
                    mm(P, B[7][:, cs], state_bf[:, :], qd[:, cs], False, True, skip=True)
                    su = B[3] if c % 2 == 0 else B[4]
                    P.matmul(su[:, 0:128], kdec[:, c, :], V64[:, c, :], True, True)
                    P.stt(state[:, :], state[:, :], eb[:, c * 64 + 63:c * 64 + 64], su[:, 0:128], ALU.mult, ALU.add)
                    P.copy("act", state_bf[:, :], state[:, :])
                P.copy("act", oraw[:, :], B[7][:, :])
                P.act(osq[:, :], B[7][:, :], AF.Square)
                colnorm(P, cx, None, [osq[:, :]], B[6][:, :], 128, rstdA[:, :])
                P.stt(oraw[:, :], oraw[:, :], colsr[r][:, 11 + h:12 + h], rstdA[:, :], ALU.mult, ALU.mult)
                o = osb[b % 2]
                P.tt("dve", o[:, :], oraw[:, :], gate[:, :], ALU.mult)
                P.dma("sp", oT_d[4 + 2 * r + h][:, sl], o[:, :])
    cx.pst = [B[0], B[1], B[5]]
    cx.pso = [B[2]]
    cx.psd = [B[3]]
    cx.sum_pe = [False]
    P.memset("dve", hT[64:128, 2, :], 0.0)
    scale = (MLA_NOPE + MLA_ROPE) ** -0.5
    for r in range(2):
        P.dma("pool", wuq[:, :, :], wuq_d[r].rearrange("k p n -> p k n"))
        P.dma("pool", wukv[:, :, :], wukv_d[r].rearrange("k p n -> p k n"))
        for h in range(2):
            for b in range(8):
                sl = slice(b * 512, (b + 1) * 512)
                for k in range(3):
                    P.matmul(B[4][:, :], wuq[:, k, h * 256:h * 256 + 128], cqn[:, k, sl], k == 0, k == 2)
                P.copy("act", qnT[:, sl], B[4][:, :])
                for k in range(3):
                    P.matmul(B[6][0:64, :], wuq[:, k, h * 256 + 128:h * 256 + 192], cqn[:, k, sl], k == 0, k == 2)
                for k in range(3):
                    P.matmul(B[7][0:64, :], wuq[:, k, h * 256 + 192:h * 256 + 256], cqn[:, k, sl], k == 0, k == 2)
                P.tt("dve", t1[0:64, :], B[6][0:64, :], Ct[:, sl], ALU.mult)
                P.tt("dve", t2[0:64, :], B[7][0:64, :], St[:, sl], ALU.mult)
                P.tt("dve", qrT[0:64, sl], t1[0:64, :], t2[0:64, :], ALU.add)
                for k in range(2):
                    P.matmul(B[5][:, :], wukv[:, k, h * 256:h * 256 + 128], ckvn[:, k, sl], k == 0, k == 1)
                P.copy("act", knT[:, sl], B[5][:, :])
                for tt_ in range(4):
                    tok = slice(b * 512 + tt_ * 128, b * 512 + (tt_ + 1) * 128)
                    for k in range(2):
                        mm(P, B[4][:, tt_ * 128:(tt_ + 1) * 128], ckvn[:, k, tok],
                           wukv[:, k, h * 256 + 128:h * 256 + 256], (tt_ == 0 and k == 0), k == 1, skip=True)
                P.copy("act", V[:, b * 4:(b + 1) * 4, :], B[4][:, :].rearrange("p (t n) -> p t n", t=4))

            def out_cb(g, h=h, r=r):
                o = osb[g % 2]
                P.recip(t1[:, :], cx.psd[0][:, :])
                P.tt("dve", o[:, :], cx.pso[0][:, :], t1[:, :], ALU.mult)
                P.dma("sp", oT_d[2 * r + h][:, g * 512:(g + 1) * 512], o[:, :])

            attention(P, cx, [[(knT, qnT), (kpeT, qrT)]], V, scale, out_cb)

    P.close_scope()


BF = ml_dtypes.bfloat16


def to_hT(h):
    s = h.shape[0]
    return np.ascontiguousarray(h.reshape(s, 8, 128).transpose(1, 2, 0))


def kchunk(w):
    return np.ascontiguousarray(w.reshape(w.shape[0] // 128, 128, w.shape[1]))


def const_tables():
    ones = np.ones((128, 128), dtype=BF)
    identf = np.eye(128, dtype=np.float32)
    mask = np.zeros((64, 512), dtype=np.float32)
    for c in range(8):
        mask[:, c * 64:(c + 1) * 64] = np.triu(np.ones((64, 64), dtype=np.float32))
    rmask = np.ones((128, 512), dtype=np.float32)
    rmask[:, ::64] = 0.0
    return ones, identf, mask, rmask


def inv_freq(dim):
    return (np.float32(ROPE_THETA) ** (-(np.arange(0, dim, 2, dtype=np.float32)) / np.float32(dim))).astype(np.float32)


def even_mixer_inputs(inp, j, r, h_T, pos):
    w_in = inp["ev_w_in"][j]
    kpe = w_in[:, 640:704]
    kpe_sw = np.concatenate([kpe[:, 32:64], kpe[:, 0:32]], axis=1)
    wlat = np.concatenate([w_in[:, 0:640], kpe, kpe_sw], axis=1)
    hg_parts = []
    for i in range(2):
        gh = 2 * r + i
        hg_parts += [w_in[:, 704 + gh * 128:704 + (gh + 1) * 128], w_in[:, 1216 + gh * 128:1216 + (gh + 1) * 128],
                     w_in[:, 2240 + gh * 128:2240 + (gh + 1) * 128], w_in[:, 1728 + gh * 128:1728 + (gh + 1) * 128]]
    whg = np.concatenate(hg_parts, axis=1)
    w_uq = inp["ev_w_uq"][j]
    w_ukv = inp["ev_w_ukv"][j]
    uq_parts, ukv_parts = [], []
    for i in range(2):
        gh = 2 * r + i
        rp = w_uq[:, gh * 192 + 128:gh * 192 + 192]
        uq_parts += [w_uq[:, gh * 192:gh * 192 + 128], rp, np.concatenate([rp[:, 32:64], rp[:, 0:32]], axis=1)]
        ukv_parts += [w_ukv[:, gh * 256:gh * 256 + 256]]
    wuq = np.concatenate(uq_parts, axis=1)
    wukv = np.concatenate(ukv_parts, axis=1)
    cols = np.zeros((128, 16), dtype=np.float32)
    cols[:, 0:3] = inp["ev_g_q"][j].reshape(3, 128).T
    cols[:, 3:5] = inp["ev_g_kv"][j].reshape(2, 128).T
    f = inv_freq(MLA_ROPE)
    cols[0:32, 5] = f
    cols[32:64, 5] = f
    cols[0:32, 6] = -1.0
    cols[32:64, 6] = 1.0
    for i in range(2):
        gh = 2 * r + i
        cols[:, 7 + i] = inp["ev_lb_logits"][0][gh * 128:(gh + 1) * 128]
        cols[:, 9 + i] = inp["ev_lb_logits"][1][gh * 128:(gh + 1) * 128]
        cols[:, 11 + i] = inp["ev_g_out"][j][gh]
    ones, identf, mask, rmask = const_tables()
    return {"cols": cols, "ones": ones, "identf": identf, "mask": mask, "rmask": rmask,
            "wlat": kchunk(np.ascontiguousarray(wlat)), "whg": kchunk(np.ascontiguousarray(whg)),
            "wuq": kchunk(np.ascontiguousarray(wuq)), "wukv": kchunk(np.ascontiguousarray(wukv))}


def emit_odd_mixer(P, B, G, layer):
    lambda_init = 0.8 - 0.6 * math.exp(-0.3 * layer)
    jj = layer // 2
    P.open_scope()
    cx = Ctx()
    hT_d = G["hT_s"]
    oT_d = G["oT_s"]
    pos_d = G["pos"]
    cols_d = G["colsO%d" % jj]
    lamp_d = G["lamp%d" % jj]
    ones_d = G["ones"]
    w_d = G["wodd%d" % jj]

    hT = P.sb("hT_sb", [128, 8, SEQ], BF16)
    w = P.sb("w_sb", [128, 8, 2560], BF16)
    colsr = [P.sb("cols_sb%d" % r, [128, 16], F32) for r in range(2)]
    cols = colsr[0]
    lamp = P.sb("lamp_sb", [128, 256], F32)
    lam = P.sb("lam", [128, 8], F32)
    cx.ones = P.sb("ones_sb", [128, 128], BF16)
    cx.epsb = P.sb("epsb", [128, 1], F32)
    cx.ki = P.sb("ki", [128, 512], I32)
    cx.posi = P.sb("posi", [128, 512], I32)
    cx.posf = P.sb("posf", [128, 512], F32)
    cx.ang = P.sb("ang", [128, 512], F32)
    cx.rtmp = P.sb("rtmp", [128, 512], F32)
    t1 = cx.posf
    t2 = cx.ang
    oraw = cx.rtmp
    Ct = P.sb("Ct", [128, SEQ], BF16)
    St = P.sb("St", [128, SEQ], BF16)
    qT = P.sb("qT", [128, SEQ], BF16)
    kT0 = P.sb("kT0", [128, SEQ], BF16)
    kT1 = P.sb("kT1", [128, SEQ], BF16)
    V = P.sb("V", [128, 32, 128], BF16)
    cx.pT = [P.sb("pT%d" % i, [128, 512], BF16) for i in range(3)]
    osb = [P.sb("osb%d" % i, [128, 512], BF16) for i in range(2)]
    cx.acc = [[P.sb("acc%d_%d" % (c, i), [128, 512], F32) for i in range(2)] for c in range(2)]
    cx.onesf = P.sb("onesf", [128, 128], F32)
    P.memset("dve", cx.onesf[:, :], 1.0)
    osq = P.sb("osq", [128, 512], BF16)
    rstd = P.sb("rstd", [128, 512], F32)

    for r in range(2):
        P.dma("sp", colsr[r][:, :], cols_d[r])
    P.dma("sp", lamp[:, :], lamp_d[:, :])
    P.dma("sp", cx.ones[:, :], ones_d[:, :])
    P.memset("dve", cx.epsb[:, :], EPS)
    for k in range(8):
        P.dma("sp", hT[:, k, :], hT_d[k])
    P.stt(lamp[:, 0:64], lamp[:, 0:64], 1.0, lamp[:, 64:128], ALU.mult, ALU.mult, accum=lam[:, 0:1])
    P.stt(lamp[:, 128:192], lamp[:, 128:192], 1.0, lamp[:, 192:256], ALU.mult, ALU.mult, accum=lam[:, 1:2])
    P.act(lam[:, 2:3], lam[:, 0:1], AF.Exp)
    P.act(lam[:, 3:4], lam[:, 1:2], AF.Exp)
    P.tt("dve", lam[:, 4:5], lam[:, 3:4], lam[:, 2:3], ALU.subtract)
    P.ts("dve", lam[:, 5:6], lam[:, 4:5], -lambda_init, ALU.add)
    for r in range(2):
        P.ts("dve", colsr[r][:, 6:10], colsr[r][:, 2:6], 1.0 - lambda_init, ALU.mult)
    build_rope_tables(P, cx, pos_d, cols[:, 0:1], cols[:, 1:2], 128, Ct, St)

    cx.pst = [B[0], B[1], B[7]]
    cx.pso = [B[2], B[3]]
    cx.psd = [B[4], B[5]]
    cx.sum_pe = [True, False]
    P.memset("dve", kT0[64:128, :], 0.0)
    P.memset("dve", kT1[0:64, :], 0.0)
    scale = DF_DH ** -0.5
    for r in range(2):
        for k in range(8):
            P.dma("pool", w[:, k, :], w_d[r][k])
        for hh in range(4):
            wo = hh * 640
            for b in range(8):
                sl = slice(b * 512, (b + 1) * 512)
                for (dst, o0) in ((qT, 0), (None, 256)):
                    for k in range(8):
                        P.matmul(B[6][:, :], w[:, k, wo + o0:wo + o0 + 128], hT[:, k, sl], k == 0, k == 7)
                    for k in range(8):
                        P.matmul(B[7][:, :], w[:, k, wo + o0 + 128:wo + o0 + 256], hT[:, k, sl], k == 0, k == 7)
                    P.tt("dve", t1[:, :], B[6][:, :], Ct[:, sl], ALU.mult)
                    P.tt("dve", t2[:, :], B[7][:, :], St[:, sl], ALU.mult)
                    if dst is not None:
                        P.tt("dve", dst[:, sl], t1[:, :], t2[:, :], ALU.add)
                    else:
                        P.tt("dve", kT0[0:64, sl], t1[0:64, :], t2[0:64, :], ALU.add)
                        P.tt("dve", kT1[64:128, sl], t1[64:128, :], t2[64:128, :], ALU.add)
                for tt_ in range(4):
                    tok = slice(b * 512 + tt_ * 128, b * 512 + (tt_ + 1) * 128)
                    for k in range(8):
                        mm(P, B[6][:, tt_ * 128:(tt_ + 1) * 128], hT[:, k, tok],
                           w[:, k, wo + 512:wo + 640], (tt_ == 0 and k == 0), k == 7, skip=True)
                P.copy("act", V[:, b * 4:(b + 1) * 4, :], B[6][:, :].rearrange("p (t n) -> p t n", t=4))

            def out_cb(g, hh=hh, r=r):
                o = osb[g % 2]
                P.recip(t1[:, :], cx.psd[0][:, :])
                P.recip(t2[:, :], cx.psd[1][:, :])
                P.tt("dve", t1[:, :], cx.pso[0][:, :], t1[:, :], ALU.mult)
                P.tt("dve", t2[:, :], cx.pso[1][:, :], t2[:, :], ALU.mult)
                P.stt(oraw[:, :], t2[:, :], lam[:, 5:6], t1[:, :], ALU.mult, ALU.add)
                P.act(osq[:, :], oraw[:, :], AF.Square)
                colnorm(P, cx, None, [osq[:, :]], B[6][:, :], 128, rstd[:, :])
                P.stt(o[:, :], oraw[:, :], colsr[r][:, 6 + hh:7 + hh], rstd[:, :], ALU.mult, ALU.mult)
                P.dma("sp", oT_d[4 * r + hh][:, g * 512:(g + 1) * 512], o[:, :])

            pairs = [[(kT0, qT)], [(kT1, qT)]]
            attention(P, cx, pairs, V, scale, out_cb, ncomp=2)
    P.close_scope()


def odd_mixer_inputs(inp, jj, r, h_T, pos):
    w_in = inp["od_w_in"][jj]

    def sw(wc):
        out = wc.copy()
        for c0 in range(0, wc.shape[1], 64):
            out[:, c0:c0 + 8] = wc[:, c0 + 8:c0 + 16]
            out[:, c0 + 8:c0 + 16] = wc[:, c0:c0 + 8]
        return out

    parts = []
    for hh in range(4):
        gh = 4 * r + hh
        q = w_in[:, gh * 128:(gh + 1) * 128]
        k = w_in[:, 1024 + gh * 128:1024 + (gh + 1) * 128]
        v = w_in[:, 2048 + gh * 128:2048 + (gh + 1) * 128]
        parts += [q, sw(q), k, sw(k), v]
    w = np.concatenate(parts, axis=1)
    cols = np.zeros((128, 16), dtype=np.float32)
    f = inv_freq(DF_ROT)
    for blk in (0, 64):
        cols[blk:blk + 8, 0] = f
        cols[blk + 8:blk + 16, 0] = f
        cols[blk:blk + 8, 1] = -1.0
        cols[blk + 8:blk + 16, 1] = 1.0
    for hh in range(4):
        cols[:, 2 + hh] = inp["od_g_head"][jj][4 * r + hh]
    lamp = np.ascontiguousarray(np.broadcast_to(inp["od_lambda"][jj].reshape(1, 256), (128, 256)))
    ones = np.ones((128, 128), dtype=BF)
    return {"cols": cols, "lamp": lamp, "ones": ones, "w": kchunk(np.ascontiguousarray(w))}


def build_fused():
    nc = bass.Bass("TRN2", target_bir_lowering=False)
    P = Prog(nc)
    G = {}

    def ein(name, shape, dt=F32):
        G[name] = P.dram(name, shape, dt, "ExternalInput")

    ein("x", [SEQ // 128, 128, D_MODEL])
    ein("pos", [128, SEQ], I32)
    ein("ident", [128, 128], BF16)
    ein("ones", [128, 128], BF16)
    ein("identf", [128, 128])
    ein("mask", [64, 512])
    ein("rmask", [128, 512])
    ein("gains", [DEPTH * 6, 128, D_MODEL])
    for l in range(DEPTH):
        for i in range(2):
            ein("wg%d%d" % (l, i), [NJ, 128, 8, 128])
            ein("wu%d%d" % (l, i), [NJ, 128, 8, 128])
            ein("wd%d%d" % (l, i), [NJ, 128, D_MODEL])
        ein("wout%d" % l, [8, 128, D_MODEL])
    for j in range(DEPTH // 2):
        ein("wlat%d" % j, [8, 128, 768])
        ein("whg%d" % j, [2, 8, 128, 1024])
        ein("wuq%d" % j, [2, 3, 128, 512])
        ein("wukv%d" % j, [2, 2, 128, 512])
        ein("colsE%d" % j, [2, 128, 16])
        ein("wodd%d" % j, [2, 8, 128, 2560])
        ein("colsO%d" % j, [2, 128, 16])
        ein("lamp%d" % j, [128, 256])
    for g in range(NGRP):
        G["xs%d" % g] = P.dram("xs%d" % g, [8, 128, D_MODEL], F32, "Internal")
    G["hT_s"] = P.dram("hT_s", [8, 128, SEQ], BF16, "Internal")
    G["oT_s"] = P.dram("oT_s", [8, 128, SEQ], BF16, "Internal")
    G["xo"] = P.dram("xo", [SEQ // 128, 128, D_MODEL], F32, "ExternalOutput")
    B = [P.ps("B%d" % i, [128, 512]) for i in range(8)]
    for step in range(DEPTH + 1):
        emit_token_phase(P, B, G, step)
        if step < DEPTH:
            if step % 2 == 0:
                emit_even_mixer(P, B, G, step // 2)
            else:
                emit_odd_mixer(P, B, G, step)
    P.finalize()
    return nc


def rearr_ffn_w(W):
    return np.ascontiguousarray(W.reshape(8, 128, NJ, 128).transpose(2, 1, 0, 3))


def fused_common_inputs(inp):
    ones, identf, mask, rmask = const_tables()
    C = {"ident": np.eye(128, dtype=BF), "ones": ones, "identf": identf, "mask": mask, "rmask": rmask}
    g = inp["norm_g"].reshape(DEPTH * 6, 1, D_MODEL)
    C["gains"] = np.ascontiguousarray(np.broadcast_to(g, (DEPTH * 6, 128, D_MODEL)))
    for l in range(DEPTH):
        for i in range(2):
            C["wg%d%d" % (l, i)] = rearr_ffn_w(inp["ffn_w_gate"][l, i])
            C["wu%d%d" % (l, i)] = rearr_ffn_w(inp["ffn_w_up"][l, i])
            C["wd%d%d" % (l, i)] = np.ascontiguousarray(inp["ffn_w_down"][l, i].reshape(NJ, 128, D_MODEL))
        w_out = inp["ev_w_out"][l // 2] if l % 2 == 0 else inp["od_w_out"][l // 2]
        C["wout%d" % l] = kchunk(np.ascontiguousarray(w_out))
    for j in range(DEPTH // 2):
        e = [even_mixer_inputs(inp, j, r, None, None) for r in range(2)]
        C["wlat%d" % j] = e[0]["wlat"]
        for nm in ("whg", "wuq", "wukv"):
            C["%s%d" % (nm, j)] = np.ascontiguousarray(np.stack([e[0][nm], e[1][nm]], axis=0))
        C["colsE%d" % j] = np.ascontiguousarray(np.stack([e[0]["cols"], e[1]["cols"]], axis=0))
        o = [odd_mixer_inputs(inp, j, r, None, None) for r in range(2)]
        C["wodd%d" % j] = np.ascontiguousarray(np.stack([o[0]["w"], o[1]["w"]], axis=0))
        C["colsO%d" % j] = np.ascontiguousarray(np.stack([o[0]["cols"], o[1]["cols"]], axis=0))
        C["lamp%d" % j] = o[0]["lamp"]
    return C


_NC_CACHE = {}


def kernel(x, positions, norm_g, ffn_w_gate, ffn_w_up, ffn_w_down, ev_w_in, ev_g_q, ev_w_uq, ev_g_kv,
           ev_w_ukv, ev_lb_logits, ev_g_out, ev_w_out, od_w_in, od_lambda, od_g_head, od_w_out):
    inp = dict(x=x, positions=positions, norm_g=norm_g, ffn_w_gate=ffn_w_gate, ffn_w_up=ffn_w_up,
               ffn_w_down=ffn_w_down, ev_w_in=ev_w_in, ev_g_q=ev_g_q, ev_w_uq=ev_w_uq, ev_g_kv=ev_g_kv,
               ev_w_ukv=ev_w_ukv, ev_lb_logits=ev_lb_logits, ev_g_out=ev_g_out, ev_w_out=ev_w_out,
               od_w_in=od_w_in, od_lambda=od_lambda, od_g_head=od_g_head, od_w_out=od_w_out)
    inp = {k: np.asarray(v) for k, v in inp.items()}
    if "nc" not in _NC_CACHE:
        _NC_CACHE["nc"] = build_fused()
    nc = _NC_CACHE["nc"]
    C = fused_common_inputs(inp)
    cores = list(range(NCORES))
    in_maps = []
    for c in cores:
        b = c // 2
        m = dict(C)
        m["x"] = np.ascontiguousarray(inp["x"][b].astype(np.float32, copy=False)).reshape(SEQ // 128, 128, D_MODEL)
        m["pos"] = np.ascontiguousarray(np.broadcast_to(inp["positions"][b].astype(np.int32)[None, :], (128, SEQ)))
        in_maps.append(m)
    res = run_bass_kernel_spmd(nc, in_maps, core_ids=cores).results
    out = np.zeros((BATCH, SEQ, D_MODEL), dtype=np.float32)
    for b in range(BATCH):
        out[b] = np.asarray(res[2 * b]["xo"]).reshape(SEQ, D_MODEL)
    return out
```

```python
import math
import numpy as np
import ml_dtypes
import concourse.bass as bass
import concourse.mybir as mybir
from concourse.bass_utils import run_bass_kernel_spmd

F32 = mybir.dt.float32
BF16 = mybir.dt.bfloat16
I32 = mybir.dt.int32
AF = mybir.ActivationFunctionType
ALU = mybir.AluOpType
AX = mybir.AxisListType

D_MODEL = 1024
BATCH = 4
SEQ = 4096
DEPTH = 4
CHUNK = 64
ROPE_THETA = 500000.0
EPS = 1e-6
TINY = 1e-30
D_FF = 2816
NJ = D_FF // 128
MLA_NOPE, MLA_ROPE, MLA_V, MLA_Q_RANK, MLA_KV_RANK = 128, 64, 128, 384, 256
DF_DH = 64
DF_ROT = 16
NCORES = 8
TOK = SEQ // 2
NTT = TOK // 128


class View:
    __slots__ = ("tile", "ap")

    def __init__(self, tile, ap):
        self.tile = tile
        self.ap = ap

    def __getitem__(self, idx):
        return View(self.tile, self.ap[idx])

    def rearrange(self, s, **kw):
        return View(self.tile, self.ap.rearrange(s, **kw))

    def bitcast(self, dt):
        return View(self.tile, self.ap.bitcast(dt))


class Tile:
    def __init__(self, name, base_ap):
        self.name = name
        self.base = base_ap
        self.last_w = None
        self.readers = []

    def __getitem__(self, idx):
        return View(self, self.base[idx])

    def v(self):
        return View(self, self.base)


class Op:
    __slots__ = ("eng", "fn", "reads", "writes", "dma", "deps", "signal", "seq",
                 "sem", "val", "prev_dma")

    def __init__(self, eng, fn, reads, writes, dma):
        self.eng = eng
        self.fn = fn
        self.reads = reads
        self.writes = writes
        self.dma = dma
        self.deps = []
        self.signal = False
        self.seq = 0
        self.sem = None
        self.val = 0
        self.prev_dma = None


class Prog:
    ENGS = ("pe", "act", "dve", "pool", "sp")
    NDMASEM = 12

    def __init__(self, nc):
        self.nc = nc
        self.ops = []
        self.n_sb = 0
        self.stack = None
        self.uid = 0

    def open_scope(self):
        import contextlib
        self.stack = contextlib.ExitStack()

    def close_scope(self):
        self.stack.close()
        self.stack = None
        self.ops.append("BARRIER")

    def sb(self, name, shape, dtype):
        if self.stack is not None:
            self.uid += 1
            h = self.stack.enter_context(self.nc.sbuf_tensor("sb%d_%s" % (self.uid, name), list(shape), dtype))
        else:
            h = self.nc.alloc_sbuf_tensor("sb_" + name, list(shape), dtype)
        idx = tuple(slice(None) for _ in shape)
        return Tile(name, h[idx])

    def ps(self, name, shape, dtype=F32):
        h = self.nc.alloc_psum_tensor("ps_" + name, list(shape), dtype)
        idx = tuple(slice(None) for _ in shape)
        return Tile(name, h[idx])

    def dram(self, name, shape, dtype, kind):
        h = self.nc.dram_tensor(name, list(shape), dtype, kind=kind)
        return Tile(name, h.ap())

    def add(self, eng, fn, reads, writes, dma=False):
        rt = []
        for r in reads:
            if isinstance(r, View) and r.tile not in rt:
                rt.append(r.tile)
        wt = []
        for w in writes:
            if isinstance(w, View) and w.tile not in wt:
                wt.append(w.tile)
        op = Op(eng, fn, rt, wt, dma)
        self.ops.append(op)
        return op

    def dma(self, q, out, in_):
        return self.add(q, lambda e: e.dma_start(out=out.ap, in_=in_.ap), [in_], [out], dma=True)

    def matmul(self, out, lhsT, rhs, start, stop):
        return self.add("pe", lambda e: e.matmul(out.ap, lhsT.ap, rhs.ap, start=start, stop=stop),
                        [lhsT, rhs], [out])

    def transpose(self, out, in_, ident):
        return self.add("pe", lambda e: e.transpose(out.ap, in_.ap, ident.ap), [in_, ident], [out])

    def act(self, out, in_, func, bias=None, scale=None, accum=None, eng="act"):
        reads = [in_]
        writes = [out]
        kw = {}
        if bias is not None:
            if isinstance(bias, View):
                reads.append(bias)
                kw["bias"] = bias.ap
            else:
                kw["bias"] = float(bias)
        if scale is not None:
            if isinstance(scale, View):
                reads.append(scale)
                kw["scale"] = scale.ap
            else:
                kw["scale"] = float(scale)
        if accum is not None:
            writes.append(accum)
            kw["accum_out"] = accum.ap
        return self.add(eng, lambda e: e.activation(out.ap, in_.ap, func, **kw), reads, writes)

    def tt(self, eng, out, in0, in1, op):
        return self.add(eng, lambda e: e.tensor_tensor(out.ap, in0.ap, in1.ap, op), [in0, in1], [out])

    def ts(self, eng, out, in0, s1, op0, s2=None, op1=None, accum=None):
        reads = [in0]
        a1 = s1.ap if isinstance(s1, View) else float(s1)
        if isinstance(s1, View):
            reads.append(s1)
        a2 = None
        if s2 is not None:
            a2 = s2.ap if isinstance(s2, View) else float(s2)
            if isinstance(s2, View):
                reads.append(s2)
        writes = [out]
        kw = {}
        if accum is not None:
            writes.append(accum)
            kw["accum_out"] = accum.ap
        o1 = op1 if op1 is not None else ALU.bypass
        return self.add(eng, lambda e: e.tensor_scalar(out.ap, in0.ap, a1, a2, op0, o1, **kw), reads, writes)

    def stt(self, out, in0, scalar, in1, op0, op1, accum=None):
        reads = [in0, in1]
        a = scalar.ap if isinstance(scalar, View) else float(scalar)
        if isinstance(scalar, View):
            reads.append(scalar)
        writes = [out]
        kw = {}
        if accum is not None:
            writes.append(accum)
            kw["accum_out"] = accum.ap
        return self.add("dve", lambda e: e.scalar_tensor_tensor(out.ap, in0.ap, a, in1.ap, op0, op1, **kw),
                        reads, writes)

    def copy(self, eng, out, in_):
        if eng == "act":
            return self.add("act", lambda e: e.copy(out.ap, in_.ap), [in_], [out])
        return self.add(eng, lambda e: e.tensor_copy(out.ap, in_.ap), [in_], [out])

    def memset(self, eng, out, val):
        return self.add(eng, lambda e: e.memset(out.ap, val), [], [out])

    def recip(self, out, in_):
        return self.add("dve", lambda e: e.reciprocal(out.ap, in_.ap), [in_], [out])

    def finalize(self):
        nc = self.nc
        ops = self.ops
        real = []
        fence = []
        last_eng = {}
        dma_q = {}
        for op in ops:
            if isinstance(op, str):
                fence = [o for o in last_eng.values()]
                for q, lst in dma_q.items():
                    fence.extend(lst[-self.NDMASEM:])
                continue
            real.append(op)
            if op.dma:
                dma_q.setdefault(op.eng, []).append(op)
            else:
                last_eng[op.eng] = op
            op.deps.extend(fence)
        ops = real
        self.ops = real
        for op in ops:
            deps = list(op.deps)
            op.deps = []
            raw = set()
            for t in op.reads:
                if t.last_w is not None:
                    deps.append(t.last_w)
                    raw.add(id(t.last_w))
            for t in op.writes:
                if t.last_w is not None:
                    deps.append(t.last_w)
                deps.extend(t.readers)
            seen = set()
            for d in deps:
                if d is op or id(d) in seen:
                    continue
                seen.add(id(d))
                if (not d.dma) and d.eng == op.eng and op.eng == "pe":
                    continue
                op.deps.append(d)
            for t in op.reads:
                t.readers.append(op)
            for t in op.writes:
                t.last_w = op
                t.readers = []
        for op in ops:
            for d in op.deps:
                if not d.dma:
                    d.signal = True
        cnt = {e: 0 for e in self.ENGS}
        dcnt = {e: 0 for e in self.ENGS}
        dma_hist = {e: [] for e in self.ENGS}
        esem = {}
        dsem = {}
        for e in self.ENGS:
            esem[e] = nc.alloc_semaphore("s_" + e)
        for op in ops:
            if op.dma:
                q = op.eng
                if q not in dsem:
                    dsem[q] = [nc.alloc_semaphore("d_%s_%d" % (q, i)) for i in range(self.NDMASEM)]
                i = dcnt[q]
                dcnt[q] += 1
                op.sem = dsem[q][i % self.NDMASEM]
                op.val = 16 * (i // self.NDMASEM + 1)
                if i >= self.NDMASEM:
                    op.prev_dma = dma_hist[q][i - self.NDMASEM]
                dma_hist[q].append(op)
            elif op.signal:
                cnt[op.eng] += 1
                op.seq = cnt[op.eng]
                op.sem = esem[op.eng]
                op.val = op.seq
        per_eng = {e: [] for e in self.ENGS}
        for op in ops:
            per_eng[op.eng].append(op)
        last_dma = {q: h for q, h in dma_hist.items() if h}

        def emit(eng_name, e):
            waited = {}
            for op in per_eng[eng_name]:
                need = {}
                dl = list(op.deps)
                if op.prev_dma is not None:
                    dl.append(op.prev_dma)
                for d in dl:
                    k = d.sem.num
                    if waited.get(k, 0) >= d.val:
                        continue
                    if k not in need or need[k][1] < d.val:
                        need[k] = (d.sem, d.val)
                for k, (s, v) in need.items():
                    e.wait_ge(s, v)
                    waited[k] = v
                ins = op.fn(e)
                if op.dma:
                    ins.then_inc(op.sem, 16)
                elif op.signal:
                    ins.then_inc(op.sem, 1)
            if eng_name in last_dma:
                fin = {}
                for d in last_dma[eng_name]:
                    k = d.sem.num
                    if k not in fin or fin[k][1] < d.val:
                        fin[k] = (d.sem, d.val)
                for k, (s, v) in fin.items():
                    if waited.get(k, 0) < v:
                        e.wait_ge(s, v)

        with nc.Block() as block:
            @block.tensor
            def _(e):
                emit("pe", e)

            @block.scalar
            def _(e):
                emit("act", e)

            @block.vector
            def _(e):
                emit("dve", e)

            @block.gpsimd
            def _(e):
                emit("pool", e)

            @block.sync
            def _(e):
                emit("sp", e)


class Ctx:
    pass


def rms_stats(P, cx, src, d, rstd):
    P.act(cx.junk[:, 0:d], src, AF.Square, accum=cx.ss[:, 0:1])
    P.act(cx.ss[:, 1:2], cx.ss[:, 0:1], AF.Sqrt, bias=cx.epsb[:, 0:1], scale=1.0 / d)
    P.recip(rstd, cx.ss[:, 1:2])


def norm_T_stage(P, cx, xt, g, sink):
    nt = len(xt)

    def stats(t):
        rs = cx.rstdN[:, t:t + 1]
        P.act(cx.junk[:, :], xt[t][:, :], AF.Square, accum=cx.ssN[:, 2 * t:2 * t + 1])
        P.act(cx.ssN[:, 2 * t + 1:2 * t + 2], cx.ssN[:, 2 * t:2 * t + 1], AF.Sqrt,
              bias=cx.epsb[:, 0:1], scale=1.0 / D_MODEL)
        P.recip(rs, cx.ssN[:, 2 * t + 1:2 * t + 2])
        P.stt(cx.hnb[t % 2][:, :], xt[t][:, :], rs, g[:, :], ALU.mult, ALU.mult)

    def trans(t):
        pt = cx.ptr[t % 2]
        hn = cx.hnb[t % 2]
        for kc in range(8):
            P.transpose(pt[:, kc * 128:(kc + 1) * 128], hn[:, kc * 128:(kc + 1) * 128], cx.ident[:, :])
        sink(t, pt)

    for t in range(nt + 1):
        if t < nt:
            stats(t)
        if t >= 1:
            trans(t - 1)


STAGE = 9


def ffn_group(P, cx, xt, W, gpre, gpost, name):
    nt = len(xt)
    nh = nt // 4
    def sink(t, pt):
        P.copy("act", cx.hT[t // 4][:, :, (t % 4) * 128:(t % 4 + 1) * 128],
               pt.rearrange("p (k n) -> p k n", k=8))

    norm_T_stage(P, cx, xt, gpre, sink)
    if STAGE < 2:
        return
    for j in range(NJ):
        wg = cx.wg[j % len(cx.wg)]
        wu = cx.wu[j % len(cx.wu)]
        P.dma("pool", wg[:, :, :], W["wg"][j])
        P.dma("pool", wu[:, :, :], W["wu"][j])
        P.dma("pool", cx.wd[j][:, :], W["wd_d"][j])
        for h in range(nh):
            pg = cx.pg[(j * nh + h) % 2]
            pu = cx.pu[(j * nh + h) % 2]
            for kc in range(8):
                P.matmul(pg[:, :], wg[:, kc, :], cx.hT[h][:, kc, :], kc == 0, kc == 7)
            for kc in range(8):
                P.matmul(pu[:, :], wu[:, kc, :], cx.hT[h][:, kc, :], kc == 0, kc == 7)
            sg = cx.sg[(j * nh + h) % 2]
            P.act(sg[:, :], pg[:, :], AF.Silu)
            P.tt("dve", cx.aT[j][:, h * 512:(h + 1) * 512], sg[:, :], pu[:, :], ALU.mult)
    if STAGE < 3:
        return
    for t in range(nt):
        y = cx.y[t % 2]
        for n in range(2):
            pd = cx.pd[(t * 2 + n) % 2]
            for j in range(NJ):
                P.matmul(pd[:, :], cx.aT[j][:, t * 128:(t + 1) * 128],
                         W["wd"][j][:, n * 512:(n + 1) * 512], j == 0, j == NJ - 1)
            P.copy("act", y[:, n * 512:(n + 1) * 512], pd[:, :])
        norm_residual(P, cx, xt[t], y, gpost, 0.5)


def norm_residual(P, cx, x, y, g, alpha):
    rms_stats(P, cx, y[:, :], D_MODEL, cx.rstd[:, 1:2])
    P.stt(y[:, :], y[:, :], cx.rstd[:, 1:2], g[:, :], ALU.mult, ALU.mult)
    P.stt(x[:, :], y[:, :], float(alpha), x[:, :], ALU.mult, ALU.add)


def load_wd(P, cx, wd_dram):
    for j in range(NJ):
        P.dma("pool", cx.wd[j][:, :], wd_dram[j])


NGRP = SEQ // 1024


def emit_token_phase(P, B, G, step):
    do_post = step > 0
    do_pre = step < DEPTH
    P.open_scope()
    cx = Ctx()
    cx.ident = P.sb("ident_sb", [128, 128], BF16)
    cx.ss = P.sb("ss", [128, 2], F32)
    cx.rstd = P.sb("rstd", [128, 2], F32)
    cx.epsb = P.sb("epsb", [128, 1], F32)
    cx.hnb = [P.sb("hn%d" % i, [128, D_MODEL], BF16) for i in range(2)]
    cx.ssN = P.sb("ssN", [128, 32], F32)
    cx.rstdN = P.sb("rstdN", [128, 16], F32)
    cx.hT = [P.sb("hT%d" % i, [128, 8, 512], BF16) for i in range(2)]
    cx.wg = [P.sb("wg%d" % i, [128, 8, 128], BF16) for i in range(3)]
    cx.wu = [P.sb("wu%d" % i, [128, 8, 128], BF16) for i in range(3)]
    cx.wd = [P.sb("wd%d" % j, [128, D_MODEL], BF16) for j in range(NJ)]
    cx.aT = [P.sb("aT%d" % j, [128, 1024], BF16) for j in range(NJ)]
    cx.sg = [P.sb("sg%d" % i, [128, 512], F32) for i in range(2)]
    cx.junk = cx.sg[0][:, :].bitcast(BF16)
    cx.y = [P.sb("y%d" % i, [128, D_MODEL], F32) for i in range(2)]
    gt = [P.sb("g%d" % i, [128, D_MODEL], F32) for i in range(6)]
    xt = [P.sb("x%d" % i, [128, D_MODEL], F32) for i in range(8)]
    if do_post:
        wout = [P.sb("wout%d" % k, [128, D_MODEL], BF16) for k in range(8)]
    if do_pre:
        hTo = [P.sb("hTo0", [128, 8, 128], BF16),
               cx.sg[1][:, :].bitcast(BF16).rearrange("p (k n) -> p k n", k=8)]
    cx.pg = [B[0], B[1]]
    cx.pu = [B[2], B[3]]
    cx.pd = [B[4], B[5]]
    cx.ptr = [B[6][:, :].bitcast(BF16), B[7][:, :].bitcast(BF16)]

    P.dma("sp", cx.ident[:, :], G["ident"][:, :])
    P.memset("dve", cx.epsb[:, :], EPS)
    Wpost = Wpre = None
    if do_post:
        l = step - 1
        for i in range(3):
            P.dma("sp", gt[i][:, :], G["gains"][l * 6 + 3 + i])
        for k in range(8):
            P.dma("pool", wout[k][:, :], G["wout%d" % l][k])
        Wpost = {"wg": G["wg%d1" % l], "wu": G["wu%d1" % l], "wd": cx.wd, "wd_d": G["wd%d1" % l]}
    if do_pre:
        l = step
        for i in range(3):
            P.dma("sp", gt[3 + i][:, :], G["gains"][l * 6 + i])
        Wpre = {"wg": G["wg%d0" % l], "wu": G["wu%d0" % l], "wd": cx.wd, "wd_d": G["wd%d0" % l]}

    for grp in range(NGRP):
        for t in range(8):
            src = G["x"][grp * 8 + t] if step == 0 else G["xs%d" % grp][t]
            P.dma("sp", xt[t][:, :], src)
        if do_post:
            l = step - 1
            for h2 in range(2):
                P.dma("sp", cx.hT[h2][:, :, :],
                      G["oT_s"][:, :, grp * 1024 + h2 * 512:grp * 1024 + (h2 + 1) * 512].rearrange("k p n -> p k n"))
            for t in range(8):
                y = cx.y[t % 2]
                for n in range(2):
                    pd = cx.pd[(t * 2 + n) % 2]
                    for k in range(8):
                        P.matmul(pd[:, :], cx.hT[t // 4][:, k, (t % 4) * 128:(t % 4 + 1) * 128],
                                 wout[k][:, n * 512:(n + 1) * 512], k == 0, k == 7)
                    P.copy("act", y[:, n * 512:(n + 1) * 512], pd[:, :])
                norm_residual(P, cx, xt[t], y, gt[0], 1.0)
            ffn_group(P, cx, xt, Wpost, gt[1], gt[2], "f2")
        if do_pre:
            l = step
            ffn_group(P, cx, xt, Wpre, gt[3], gt[4], "f1")
            def hsink(t, pt, grp=grp):
                ho = hTo[t % 2]
                P.copy("act", ho[:, :, :], pt.rearrange("p (k n) -> p k n", k=8))
                tok0 = grp * 1024 + t * 128
                P.dma("sp", G["hT_s"][:, :, tok0:tok0 + 128].rearrange("k p n -> p k n"), ho[:, :, :])

            norm_T_stage(P, cx, xt, gt[5], hsink)
        for t in range(8):
            dst = G["xo"][grp * 8 + t] if step == DEPTH else G["xs%d" % grp][t]
            P.dma("sp", dst, xt[t][:, :])
    P.close_scope()


def mm(P, out, lhsT, rhs, start, stop, skip=False):
    if skip:
        return P.add("pe", lambda e: e.matmul(out.ap, lhsT.ap, rhs.ap, start=start, stop=stop,
                                              skip_group_check=True), [lhsT, rhs], [out])
    return P.matmul(out, lhsT, rhs, start, stop)


def build_rope_tables(P, cx, pos_d, freq_col, sgn_col, nrows, Ct, St):
    PI = math.pi
    PIS = 3.1415925
    n = nrows
    for b in range(8):
        sl = slice(b * 512, (b + 1) * 512)
        P.dma("sp", cx.posi[0:n, :], pos_d[0:n, sl])
        P.copy("dve", cx.posf[0:n, :], cx.posi[0:n, :])
        P.ts("dve", cx.ang[0:n, :], cx.posf[0:n, :], freq_col, ALU.mult)
        for shift, dst, sg in ((0.0, St, sgn_col), (0.5 * PI, Ct, None)):
            if shift != 0.0:
                P.ts("dve", cx.ang[0:n, :], cx.ang[0:n, :], shift, ALU.add)
            P.ts("dve", cx.rtmp[0:n, :], cx.ang[0:n, :], 1.0 / (2 * PI), ALU.mult)
            P.copy("dve", cx.ki[0:n, :], cx.rtmp[0:n, :])
            P.copy("dve", cx.rtmp[0:n, :], cx.ki[0:n, :])
            P.stt(cx.rtmp[0:n, :], cx.rtmp[0:n, :], -2 * PI, cx.ang[0:n, :], ALU.mult, ALU.add)
            P.ts("dve", cx.posf[0:n, :], cx.rtmp[0:n, :], PI, ALU.is_gt, 2 * PI, ALU.mult)
            P.tt("dve", cx.rtmp[0:n, :], cx.rtmp[0:n, :], cx.posf[0:n, :], ALU.subtract)
            P.ts("dve", cx.rtmp[0:n, :], cx.rtmp[0:n, :], PIS, ALU.min, -PIS, ALU.max)
            if sg is not None:
                P.act(cx.rtmp[0:n, :], cx.rtmp[0:n, :], AF.Sin)
                P.ts("dve", dst[0:n, sl], cx.rtmp[0:n, :], sg, ALU.mult)
            else:
                P.act(dst[0:n, sl], cx.rtmp[0:n, :], AF.Sin)


def attention(P, cx, qk_pairs, V, scale, out_cb, ncomp=1):
    its = []
    for g in range(8):
        nk = 4 * (g + 1)
        for kt in range(nk):
            for c in range(ncomp):
                its.append((g, kt, c, nk))
    nb = len(cx.pst)

    def front(i):
        g, kt, c, nk = its[i]
        a = kt - 4 * g
        q0 = 128 * a if a > 0 else 0
        st = cx.pst[i % nb]
        pT = cx.pT[i % nb]
        prs = qk_pairs[c]
        for n_, (kT, qT) in enumerate(prs):
            P.matmul(st[:, q0:512], kT[:, kt * 128:(kt + 1) * 128],
                     qT[:, g * 512 + q0:(g + 1) * 512], n_ == 0, n_ == len(prs) - 1)
        P.act(pT[:, q0:512], st[:, q0:512], AF.Exp, scale=scale)
        if a >= 0:
            P.memset("dve", pT[64:128, q0:q0 + 64], 0.0)

    def back(i):
        g, kt, c, nk = its[i]
        a = kt - 4 * g
        q0 = 128 * a if a > 0 else 0
        pT = cx.pT[i % nb]
        mm(P, cx.pso[c][:, q0:512], V[:, kt, :], pT[:, q0:512], kt == 0, kt == nk - 1, skip=True)
        if cx.sum_pe[c]:
            mm(P, cx.psd[c][:, q0:512], cx.ones[:, :], pT[:, q0:512], kt == 0, kt == nk - 1, skip=True)
        else:
            acc = cx.acc[c][g % 2]
            if kt == 0:
                P.copy("dve", acc[:, :], pT[:, :])
            else:
                P.tt("dve", acc[:, q0:512], acc[:, q0:512], pT[:, q0:512], ALU.add)
        if kt == nk - 1 and c == ncomp - 1:
            for c2 in range(ncomp):
                if not cx.sum_pe[c2]:
                    P.matmul(cx.psd[c2][:, :], cx.onesf[:, :], cx.acc[c2][g % 2][:, :], True, True)
            out_cb(g)

    SK = nb - 1
    n = len(its)
    for i in range(n + SK):
        if i < n:
            front(i)
        if i - SK >= 0:
            back(i - SK)


def attention_diff(P, cx, kT0, kT1, qT, V, scale, out_cb):
    its = []
    for g in range(8):
        nk = 4 * (g + 1)
        for kt in range(nk):
            its.append((g, kt, nk))
    nb = len(cx.pst2)

    def front(i):
        g, kt, nk = its[i]
        a = kt - 4 * g
        q0 = 128 * a if a > 0 else 0
        st = cx.pst2[i % nb]
        pT = cx.pT2[i % nb]
        ks = slice(kt * 128, (kt + 1) * 128)
        qs = slice(g * 512 + q0, (g + 1) * 512)
        P.matmul(st[:, q0:512], kT0[:, ks], qT[:, qs], True, True)
        P.matmul(st[:, 512 + q0:1024], kT1[:, ks], qT[:, qs], True, True)
        P.act(pT[:, :].rearrange("p (c n) -> p c n", c=2)[:, :, q0:512],
              st[:, :].rearrange("p (c n) -> p c n", c=2)[:, :, q0:512], AF.Exp, scale=scale)
        if a >= 0:
            P.memset("dve", pT[:, :].rearrange("p (c n) -> p c n", c=2)[64:128, :, q0:q0 + 64], 0.0)

    def back(i):
        g, kt, nk = its[i]
        a = kt - 4 * g
        q0 = 128 * a if a > 0 else 0
        pT = cx.pT2[i % nb]
        first, last = kt == 0, kt == nk - 1
        mm(P, cx.pso[0][:, q0:512], V[:, kt, :], pT[:, q0:512], first, last, skip=True)
        mm(P, cx.pso[1][:, q0:512], V[:, kt, :], pT[:, 512 + q0:1024], first, last, skip=True)
        mm(P, cx.psd[0][:, q0:512], cx.ones[:, :], pT[:, q0:512], first, last, skip=True)
        acc = cx.acc[1][g % 2]
        if first:
            P.copy("dve", acc[:, :], pT[:, 512:1024])
        else:
            P.tt("dve", acc[:, q0:512], acc[:, q0:512], pT[:, 512 + q0:1024], ALU.add)
        if last:
            P.matmul(cx.psd[1][:, :], cx.onesf[:, :], acc[:, :], True, True)
            out_cb(g)

    SK = nb - 1
    n = len(its)
    for i in range(n + SK):
        if i < n:
            front(i)
        if i - SK >= 0:
            back(i - SK)


def colnorm(P, cx, raws, sqs, ps_ss, d, rstd_out):
    for i, sq in enumerate(sqs):
        P.matmul(ps_ss, cx.ones[:, :], sq, i == 0, i == len(sqs) - 1)
    P.act(rstd_out, ps_ss, AF.Sqrt, bias=cx.epsb[:, 0:1], scale=1.0 / d)
    P.recip(rstd_out, rstd_out)


def emit_even_mixer(P, B, G, j):
    P.open_scope()
    cx = Ctx()
    hT_d = G["hT_s"]
    oT_d = G["oT_s"]
    pos_d = G["pos"]
    cols_d = G["colsE%d" % j]
    ones_d, identf_d, mask_d, rmask_d = G["ones"], G["identf"], G["mask"], G["rmask"]
    wlat_d, whg_d, wuq_d, wukv_d = G["wlat%d" % j], G["whg%d" % j], G["wuq%d" % j], G["wukv%d" % j]

    hT = P.sb("hT_sb", [128, 8, SEQ], BF16)
    colsr = [P.sb("cols_sb%d" % r, [128, 16], F32) for r in range(2)]
    cols = colsr[0]
    cx.ones = P.sb("ones_sb", [128, 128], BF16)
    identf = P.sb("identf_sb", [128, 128], F32)
    mask = P.sb("mask_sb", [64, 512], F32)
    rmask = P.sb("rmask_sb", [128, 512], F32)
    wlat = P.sb("wlat_sb", [128, 8, 768], BF16)
    whg = P.sb("whg_sb", [128, 8, 1024], BF16)
    wuq = P.sb("wuq_sb", [128, 3, 512], BF16)
    wukv = P.sb("wukv_sb", [128, 2, 512], BF16)
    cx.epsb = P.sb("epsb", [128, 1], F32)
    cx.ki = P.sb("ki", [128, 512], I32)
    cx.posi = P.sb("posi", [128, 512], I32)
    sig = P.sb("sig", [128, 512], F32)
    ff = P.sb("ff", [128, 512], F32)
    kk = P.sb("kk", [128, 512], F32)
    bcum = P.sb("bcum", [128, 512], F32)
    eb = P.sb("eb", [128, 512], F32)
    enb = P.sb("enb", [128, 512], F32)
    kdT = P.sb("kdT", [128, 512], F32)
    gate = P.sb("gate", [128, 512], F32)
    cx.posf = sig
    cx.ang = ff
    cx.rtmp = kk
    Ct = P.sb("Ct", [64, SEQ], BF16)
    St = P.sb("St", [64, SEQ], BF16)
    cqn = P.sb("cqn", [128, 3, SEQ], BF16)
    ckvn = P.sb("ckvn", [128, 2, SEQ], BF16)
    kpeT = P.sb("kpeT", [128, SEQ], BF16)
    raw = [bcum, eb, enb, kdT, gate]
    rstdA = P.sb("rstdA", [128, 512], F32)
    rstdB = ff
    t1 = kk
    t2 = sig
    qnT = hT[:, 0, :]
    knT = hT[:, 1, :]
    qrT = hT[:, 2, :]
    V = hT[:, 3, :].rearrange("p (t n) -> p t n", t=32)
    cx.pT = [P.sb("pT%d" % i, [128, 512], BF16) for i in range(3)]
    osb = [P.sb("osb%d" % i, [128, 512], BF16) for i in range(2)]
    sq = [cx.pT[0], cx.pT[1], cx.pT[2], osb[0], osb[1]]
    cx.acc = [[P.sb("acc%d" % i, [128, 512], F32) for i in range(2)]]
    cx.onesf = P.sb("onesf", [128, 128], F32)
    P.memset("dve", cx.onesf[:, :], 1.0)

    for r in range(2):
        P.dma("sp", colsr[r][:, :], cols_d[r])
    P.dma("sp", cx.ones[:, :], ones_d[:, :])
    P.dma("sp", identf[:, :], identf_d[:, :])
    P.dma("sp", mask[:, :], mask_d[:, :])
    P.dma("sp", rmask[:, :], rmask_d[:, :])
    P.memset("dve", cx.epsb[:, :], EPS)
    P.dma("pool", wlat[:, :, :], wlat_d[:, :, :].rearrange("k p n -> p k n"))
    for k in range(8):
        P.dma("sp", hT[:, k, :], hT_d[k])
    build_rope_tables(P, cx, pos_d, cols[0:64, 5:6], cols[0:64, 6:7], 64, Ct, St)

    P.memset("dve", kpeT[64:128, :], 0.0)
    for b in range(8):
        sl = slice(b * 512, (b + 1) * 512)
        for oc in range(5):
            pb = B[oc % 4]
            for k in range(8):
                P.matmul(pb[:, :], wlat[:, k, oc * 128:(oc + 1) * 128], hT[:, k, sl], k == 0, k == 7)
            P.copy("act", raw[oc][:, :], pb[:, :])
            P.act(sq[oc][:, :], pb[:, :], AF.Square)
        colnorm(P, cx, raw[0:3], [s[:, :] for s in sq[0:3]], B[4][:, :], MLA_Q_RANK, rstdA[:, :])
        colnorm(P, cx, raw[3:5], [s[:, :] for s in sq[3:5]], B[5][:, :], MLA_KV_RANK, rstdB[:, :])
        for c in range(3):
            P.stt(cqn[:, c, sl], raw[c][:, :], cols[:, c:c + 1], rstdA[:, :], ALU.mult, ALU.mult)
        for c in range(2):
            P.stt(ckvn[:, c, sl], raw[3 + c][:, :], cols[:, 3 + c:4 + c], rstdB[:, :], ALU.mult, ALU.mult)
        for k in range(8):
            P.matmul(B[6][0:64, :], wlat[:, k, 640:704], hT[:, k, sl], k == 0, k == 7)
        for k in range(8):
            P.matmul(B[7][0:64, :], wlat[:, k, 704:768], hT[:, k, sl], k == 0, k == 7)
        P.tt("dve", t1[0:64, :], B[6][0:64, :], Ct[:, sl], ALU.mult)
        P.tt("dve", t2[0:64, :], B[7][0:64, :], St[:, sl], ALU.mult)
        P.tt("dve", kpeT[0:64, sl], t1[0:64, :], t2[0:64, :], ALU.add)

    lbc = P.sb("lbc", [128, 4], F32)
    state = P.sb("state", [128, 128], F32)
    state_bf = P.sb("state_bf", [128, 128], BF16)
    qd = P.sb("qd", [128, 512], BF16)
    ktl = P.sb("ktl", [128, 512], BF16)
    kdec = P.sb("kdec", [128, 8, 128], BF16)
    V64 = P.sb("V64", [128, 8, 128], BF16)
    attn = P.sb("attn", [128, 512], BF16)
    P.memset("dve", kdec[64:128, :, :], 0.0)
    P.memset("dve", V64[64:128, :, :], 0.0)
    P.memset("dve", attn[64:128, :], 0.0)
    oraw = bcum
    osq = P.sb("osq", [128, 512], BF16)
    for r in range(2):
        P.dma("pool", whg[:, :, :], whg_d[r].rearrange("k p n -> p k n"))
        for h in range(2):
            wofs = h * 512
            if j == 0:
                P.memset("dve", lbc[:, 0:1], 0.0)
            else:
                P.tt("dve", lbc[:, 3:4], colsr[r][:, 9 + h:10 + h], colsr[r][:, 7 + h:8 + h], ALU.subtract)
                P.act(lbc[:, 0:1], lbc[:, 3:4], AF.Sigmoid)
            P.ts("dve", lbc[:, 1:2], lbc[:, 0:1], -1.0, ALU.mult, 1.0, ALU.add)
            P.ts("dve", lbc[:, 2:3], lbc[:, 1:2], -1.0, ALU.mult)
            P.memset("dve", state[:, :], 0.0)
            P.memset("dve", state_bf[:, :], 0.0)
            for b in range(8):
                sl = slice(b * 512, (b + 1) * 512)
                for i, pb in enumerate((B[0], B[1], B[2])):
                    for k in range(8):
                        P.matmul(pb[:, :], whg[:, k, wofs + i * 128:wofs + (i + 1) * 128], hT[:, k, sl], k == 0, k == 7)
                for c in range(8):
                    pb = B[3 + c // 4]
                    tok = slice(b * 512 + c * 64, b * 512 + (c + 1) * 64)
                    for k in range(8):
                        mm(P, pb[0:64, (c % 4) * 128:(c % 4 + 1) * 128], hT[:, k, tok],
                           whg[:, k, wofs + 384:wofs + 512], (c % 4 == 0 and k == 0), k == 7, skip=True)
                P.copy("act", V64[0:64, 0:4, :], B[3][0:64, :].rearrange("p (c n) -> p c n", c=4))
                P.copy("act", V64[0:64, 4:8, :], B[4][0:64, :].rearrange("p (c n) -> p c n", c=4))
                P.act(sig[:, :], B[1][:, :], AF.Sigmoid)
                P.act(gate[:, :], B[2][:, :], AF.Silu)
                P.ts("dve", ff[:, :], sig[:, :], lbc[:, 1:2], ALU.mult, lbc[:, 0:1], ALU.add)
                P.ts("dve", ff[:, :], ff[:, :], TINY, ALU.max)
                P.act(ff[:, :], ff[:, :], AF.Ln)
                P.ts("dve", kk[:, :], sig[:, :], lbc[:, 2:3], ALU.mult, lbc[:, 1:2], ALU.add)
                P.add("dve", lambda e: e.tensor_tensor_scan(bcum[:, :].ap, rmask[:, :].ap, ff[:, :].ap, 0.0,
                                                            ALU.mult, ALU.add), [rmask[:, :], ff[:, :]], [bcum[:, :]])
                P.act(eb[:, :], bcum[:, :], AF.Exp)
                P.act(enb[:, :], bcum[:, :], AF.Exp, scale=-1.0)
                P.tt("dve", qd[:, :], B[0][:, :], eb[:, :], ALU.mult)
                P.tt("dve", ktl[:, :], kk[:, :], enb[:, :], ALU.mult)
                P.tt("dve", kdT[:, :], kk[:, :], enb[:, :], ALU.mult)
                for c in range(8):
                    cs = slice(c * 64, (c + 1) * 64)
                    P.ts("dve", kdT[:, cs], kdT[:, cs], eb[:, c * 64 + 63:c * 64 + 64], ALU.mult)
                for half in range(2):
                    pb = B[5]
                    for c4 in range(4):
                        c = half * 4 + c4
                        P.transpose(pb[0:64, c4 * 128:(c4 + 1) * 128], kdT[:, c * 64:(c + 1) * 64], identf[:, :])
                    P.copy("act", kdec[0:64, half * 4:(half + 1) * 4, :], pb[0:64, :].rearrange("p (c n) -> p c n", c=4))
                for c in range(8):
                    cs = slice(c * 64, (c + 1) * 64)
                    mm(P, B[6][0:64, cs], ktl[:, cs], qd[:, cs], c == 0, True, skip=True)
                P.tt("dve", attn[0:64, :], B[6][0:64, :], mask[:, :], ALU.mult)
                for c in range(8):
                    cs = slice(c * 64, (c + 1) * 64)
                    mm(P, B[7][:, cs], V64[:, c, :], attn[:, cs], c == 0, False, skip=True)
                    mm(P, B[7][:, cs], state_bf[:, :], qd[:, cs], False, True, skip=True)
                    su = B[3] if c % 2 == 0 else B[4]
                    P.matmul(su[:, 0:128], kdec[:, c, :], V64[:, c, :], True, True)
                    P.stt(state[:, :], state[:, :], eb[:, c * 64 + 63:c * 64 + 64], su[:, 0:128], ALU.mult, ALU.add)
                    P.copy("act", state_bf[:, :], state[:, :])
                P.copy("act", oraw[:, :], B[7][:, :])
                P.act(osq[:, :], B[7][:, :], AF.Square)
                colnorm(P, cx, None, [osq[:, :]], B[6][:, :], 128, rstdA[:, :])
                P.stt(oraw[:, :], oraw[:, :], colsr[r][:, 11 + h:12 + h], rstdA[:, :], ALU.mult, ALU.mult)
                o = osb[b % 2]
                P.tt("dve", o[:, :], oraw[:, :], gate[:, :], ALU.mult)
                P.dma("sp", oT_d[4 + 2 * r + h][:, sl], o[:, :])
    cx.pst = [B[0], B[1], B[5]]
    cx.pso = [B[2]]
    cx.psd = [B[3]]
    cx.sum_pe = [False]
    P.memset("dve", hT[64:128, 2, :], 0.0)
    scale = (MLA_NOPE + MLA_ROPE) ** -0.5
    for r in range(2):
        P.dma("pool", wuq[:, :, :], wuq_d[r].rearrange("k p n -> p k n"))
        P.dma("pool", wukv[:, :, :], wukv_d[r].rearrange("k p n -> p k n"))
        for h in range(2):
            for b in range(8):
                sl = slice(b * 512, (b + 1) * 512)
                for k in range(3):
                    P.matmul(B[4][:, :], wuq[:, k, h * 256:h * 256 + 128], cqn[:, k, sl], k == 0, k == 2)
                P.copy("act", qnT[:, sl], B[4][:, :])
                for k in range(3):
                    P.matmul(B[6][0:64, :], wuq[:, k, h * 256 + 128:h * 256 + 192], cqn[:, k, sl], k == 0, k == 2)
                for k in range(3):
                    P.matmul(B[7][0:64, :], wuq[:, k, h * 256 + 192:h * 256 + 256], cqn[:, k, sl], k == 0, k == 2)
                P.tt("dve", t1[0:64, :], B[6][0:64, :], Ct[:, sl], ALU.mult)
                P.tt("dve", t2[0:64, :], B[7][0:64, :], St[:, sl], ALU.mult)
                P.tt("dve", qrT[0:64, sl], t1[0:64, :], t2[0:64, :], ALU.add)
                for k in range(2):
                    P.matmul(B[5][:, :], wukv[:, k, h * 256:h * 256 + 128], ckvn[:, k, sl], k == 0, k == 1)
                P.copy("act", knT[:, sl], B[5][:, :])
                for tt_ in range(4):
                    tok = slice(b * 512 + tt_ * 128, b * 512 + (tt_ + 1) * 128)
                    for k in range(2):
                        mm(P, B[4][:, tt_ * 128:(tt_ + 1) * 128], ckvn[:, k, tok],
                           wukv[:, k, h * 256 + 128:h * 256 + 256], (tt_ == 0 and k == 0), k == 1, skip=True)
                P.copy("act", V[:, b * 4:(b + 1) * 4, :], B[4][:, :].rearrange("p (t n) -> p t n", t=4))

            def out_cb(g, h=h, r=r):
                o = osb[g % 2]
                P.recip(t1[:, :], cx.psd[0][:, :])
                P.tt("dve", o[:, :], cx.pso[0][:, :], t1[:, :], ALU.mult)
                P.dma("sp", oT_d[2 * r + h][:, g * 512:(g + 1) * 512], o[:, :])

            attention(P, cx, [[(knT, qnT), (kpeT, qrT)]], V, scale, out_cb)

    P.close_scope()


BF = ml_dtypes.bfloat16


def to_hT(h):
    s = h.shape[0]
    return np.ascontiguousarray(h.reshape(s, 8, 128).transpose(1, 2, 0))


def kchunk(w):
    return np.ascontiguousarray(w.reshape(w.shape[0] // 128, 128, w.shape[1]))


def const_tables():
    ones = np.ones((128, 128), dtype=BF)
    identf = np.eye(128, dtype=np.float32)
    mask = np.zeros((64, 512), dtype=np.float32)
    for c in range(8):
        mask[:, c * 64:(c + 1) * 64] = np.triu(np.ones((64, 64), dtype=np.float32))
    rmask = np.ones((128, 512), dtype=np.float32)
    rmask[:, ::64] = 0.0
    return ones, identf, mask, rmask


def inv_freq(dim):
    return (np.float32(ROPE_THETA) ** (-(np.arange(0, dim, 2, dtype=np.float32)) / np.float32(dim))).astype(np.float32)


def even_mixer_inputs(inp, j, r, h_T, pos):
    w_in = inp["ev_w_in"][j]
    kpe = w_in[:, 640:704]
    kpe_sw = np.concatenate([kpe[:, 32:64], kpe[:, 0:32]], axis=1)
    wlat = np.concatenate([w_in[:, 0:640], kpe, kpe_sw], axis=1)
    hg_parts = []
    for i in range(2):
        gh = 2 * r + i
        hg_parts += [w_in[:, 704 + gh * 128:704 + (gh + 1) * 128], w_in[:, 1216 + gh * 128:1216 + (gh + 1) * 128],
                     w_in[:, 2240 + gh * 128:2240 + (gh + 1) * 128], w_in[:, 1728 + gh * 128:1728 + (gh + 1) * 128]]
    whg = np.concatenate(hg_parts, axis=1)
    w_uq = inp["ev_w_uq"][j]
    w_ukv = inp["ev_w_ukv"][j]
    uq_parts, ukv_parts = [], []
    for i in range(2):
        gh = 2 * r + i
        rp = w_uq[:, gh * 192 + 128:gh * 192 + 192]
        uq_parts += [w_uq[:, gh * 192:gh * 192 + 128], rp, np.concatenate([rp[:, 32:64], rp[:, 0:32]], axis=1)]
        ukv_parts += [w_ukv[:, gh * 256:gh * 256 + 256]]
    wuq = np.concatenate(uq_parts, axis=1)
    wukv = np.concatenate(ukv_parts, axis=1)
    cols = np.zeros((128, 16), dtype=np.float32)
    cols[:, 0:3] = inp["ev_g_q"][j].reshape(3, 128).T
    cols[:, 3:5] = inp["ev_g_kv"][j].reshape(2, 128).T
    f = inv_freq(MLA_ROPE)
    cols[0:32, 5] = f
    cols[32:64, 5] = f
    cols[0:32, 6] = -1.0
    cols[32:64, 6] = 1.0
    for i in range(2):
        gh = 2 * r + i
        cols[:, 7 + i] = inp["ev_lb_logits"][0][gh * 128:(gh + 1) * 128]
        cols[:, 9 + i] = inp["ev_lb_logits"][1][gh * 128:(gh + 1) * 128]
        cols[:, 11 + i] = inp["ev_g_out"][j][gh]
    ones, identf, mask, rmask = const_tables()
    return {"cols": cols, "ones": ones, "identf": identf, "mask": mask, "rmask": rmask,
            "wlat": kchunk(np.ascontiguousarray(wlat)), "whg": kchunk(np.ascontiguousarray(whg)),
            "wuq": kchunk(np.ascontiguousarray(wuq)), "wukv": kchunk(np.ascontiguousarray(wukv))}


def emit_odd_mixer(P, B, G, layer):
    lambda_init = 0.8 - 0.6 * math.exp(-0.3 * layer)
    jj = layer // 2
    P.open_scope()
    cx = Ctx()
    hT_d = G["hT_s"]
    oT_d = G["oT_s"]
    pos_d = G["pos"]
    cols_d = G["colsO%d" % jj]
    lamp_d = G["lamp%d" % jj]
    ones_d = G["ones"]
    w_d = G["wodd%d" % jj]

    hT = P.sb("hT_sb", [128, 8, SEQ], BF16)
    w = P.sb("w_sb", [128, 8, 2560], BF16)
    colsr = [P.sb("cols_sb%d" % r, [128, 16], F32) for r in range(2)]
    cols = colsr[0]
    lamp = P.sb("lamp_sb", [128, 256], F32)
    lam = P.sb("lam", [128, 8], F32)
    cx.ones = P.sb("ones_sb", [128, 128], BF16)
    cx.epsb = P.sb("epsb", [128, 1], F32)
    cx.ki = P.sb("ki", [128, 512], I32)
    cx.posi = P.sb("posi", [128, 512], I32)
    cx.posf = P.sb("posf", [128, 512], F32)
    cx.ang = P.sb("ang", [128, 512], F32)
    cx.rtmp = P.sb("rtmp", [128, 512], F32)
    t1 = cx.posf
    t2 = cx.ang
    oraw = cx.rtmp
    Ct = P.sb("Ct", [128, SEQ], BF16)
    St = P.sb("St", [128, SEQ], BF16)
    qT = P.sb("qT", [128, SEQ], BF16)
    kT0 = P.sb("kT0", [128, SEQ], BF16)
    kT1 = P.sb("kT1", [128, SEQ], BF16)
    V = P.sb("V", [128, 32, 128], BF16)
    osb = [P.sb("osb%d" % i, [128, 512], BF16) for i in range(2)]
    cx.acc = [[P.sb("acc%d_%d" % (c, i), [128, 512], F32) for i in range(2)] for c in range(2)]
    cx.onesf = P.sb("onesf", [128, 128], F32)
    P.memset("dve", cx.onesf[:, :], 1.0)
    osq = P.sb("osq", [128, 512], BF16)
    rstd = P.sb("rstd", [128, 512], F32)

    for r in range(2):
        P.dma("sp", colsr[r][:, :], cols_d[r])
    P.dma("sp", lamp[:, :], lamp_d[:, :])
    P.dma("sp", cx.ones[:, :], ones_d[:, :])
    P.memset("dve", cx.epsb[:, :], EPS)
    for k in range(8):
        P.dma("sp", hT[:, k, :], hT_d[k])
    P.stt(lamp[:, 0:64], lamp[:, 0:64], 1.0, lamp[:, 64:128], ALU.mult, ALU.mult, accum=lam[:, 0:1])
    P.stt(lamp[:, 128:192], lamp[:, 128:192], 1.0, lamp[:, 192:256], ALU.mult, ALU.mult, accum=lam[:, 1:2])
    P.act(lam[:, 2:3], lam[:, 0:1], AF.Exp)
    P.act(lam[:, 3:4], lam[:, 1:2], AF.Exp)
    P.tt("dve", lam[:, 4:5], lam[:, 3:4], lam[:, 2:3], ALU.subtract)
    P.ts("dve", lam[:, 5:6], lam[:, 4:5], -lambda_init, ALU.add)
    for r in range(2):
        P.ts("dve", colsr[r][:, 6:10], colsr[r][:, 2:6], 1.0 - lambda_init, ALU.mult)
    build_rope_tables(P, cx, pos_d, cols[:, 0:1], cols[:, 1:2], 128, Ct, St)

    cx.pst2 = [G["BB"][0], G["BB"][1]]
    cx.pT2 = [P.sb("pTT%d" % i, [128, 1024], BF16) for i in range(2)]
    cx.pso = [B[4], B[5]]
    cx.psd = [B[6], B[7]]
    P.memset("dve", kT0[64:128, :], 0.0)
    P.memset("dve", kT1[0:64, :], 0.0)
    scale = DF_DH ** -0.5
    for r in range(2):
        for k in range(8):
            P.dma("pool", w[:, k, :], w_d[r][k])
        for hh in range(4):
            wo = hh * 640
            for b in range(8):
                sl = slice(b * 512, (b + 1) * 512)
                for (dst, o0) in ((qT, 0), (None, 256)):
                    for k in range(8):
                        P.matmul(B[6][:, :], w[:, k, wo + o0:wo + o0 + 128], hT[:, k, sl], k == 0, k == 7)
                    for k in range(8):
                        P.matmul(B[7][:, :], w[:, k, wo + o0 + 128:wo + o0 + 256], hT[:, k, sl], k == 0, k == 7)
                    P.tt("dve", t1[:, :], B[6][:, :], Ct[:, sl], ALU.mult)
                    P.tt("dve", t2[:, :], B[7][:, :], St[:, sl], ALU.mult)
                    if dst is not None:
                        P.tt("dve", dst[:, sl], t1[:, :], t2[:, :], ALU.add)
                    else:
                        P.tt("dve", kT0[0:64, sl], t1[0:64, :], t2[0:64, :], ALU.add)
                        P.tt("dve", kT1[64:128, sl], t1[64:128, :], t2[64:128, :], ALU.add)
                for tt_ in range(4):
                    tok = slice(b * 512 + tt_ * 128, b * 512 + (tt_ + 1) * 128)
                    for k in range(8):
                        mm(P, B[6][:, tt_ * 128:(tt_ + 1) * 128], hT[:, k, tok],
                           w[:, k, wo + 512:wo + 640], (tt_ == 0 and k == 0), k == 7, skip=True)
                P.copy("act", V[:, b * 4:(b + 1) * 4, :], B[6][:, :].rearrange("p (t n) -> p t n", t=4))

            def out_cb(g, hh=hh, r=r):
                o = osb[g % 2]
                P.recip(t1[:, :], cx.psd[0][:, :])
                P.recip(t2[:, :], cx.psd[1][:, :])
                P.tt("dve", t1[:, :], cx.pso[0][:, :], t1[:, :], ALU.mult)
                P.tt("dve", t2[:, :], cx.pso[1][:, :], t2[:, :], ALU.mult)
                P.stt(oraw[:, :], t2[:, :], lam[:, 5:6], t1[:, :], ALU.mult, ALU.add)
                P.act(osq[:, :], oraw[:, :], AF.Square)
                colnorm(P, cx, None, [osq[:, :]], B[6][:, :], 128, rstd[:, :])
                P.stt(o[:, :], oraw[:, :], colsr[r][:, 6 + hh:7 + hh], rstd[:, :], ALU.mult, ALU.mult)
                P.dma("sp", oT_d[4 * r + hh][:, g * 512:(g + 1) * 512], o[:, :])

            attention_diff(P, cx, kT0, kT1, qT, V, scale, out_cb)
    P.close_scope()


def odd_mixer_inputs(inp, jj, r, h_T, pos):
    w_in = inp["od_w_in"][jj]

    def sw(wc):
        out = wc.copy()
        for c0 in range(0, wc.shape[1], 64):
            out[:, c0:c0 + 8] = wc[:, c0 + 8:c0 + 16]
            out[:, c0 + 8:c0 + 16] = wc[:, c0:c0 + 8]
        return out

    parts = []
    for hh in range(4):
        gh = 4 * r + hh
        q = w_in[:, gh * 128:(gh + 1) * 128]
        k = w_in[:, 1024 + gh * 128:1024 + (gh + 1) * 128]
        v = w_in[:, 2048 + gh * 128:2048 + (gh + 1) * 128]
        parts += [q, sw(q), k, sw(k), v]
    w = np.concatenate(parts, axis=1)
    cols = np.zeros((128, 16), dtype=np.float32)
    f = inv_freq(DF_ROT)
    for blk in (0, 64):
        cols[blk:blk + 8, 0] = f
        cols[blk + 8:blk + 16, 0] = f
        cols[blk:blk + 8, 1] = -1.0
        cols[blk + 8:blk + 16, 1] = 1.0
    for hh in range(4):
        cols[:, 2 + hh] = inp["od_g_head"][jj][4 * r + hh]
    lamp = np.ascontiguousarray(np.broadcast_to(inp["od_lambda"][jj].reshape(1, 256), (128, 256)))
    ones = np.ones((128, 128), dtype=BF)
    return {"cols": cols, "lamp": lamp, "ones": ones, "w": kchunk(np.ascontiguousarray(w))}


def build_fused():
    nc = bass.Bass("TRN2", target_bir_lowering=False)
    P = Prog(nc)
    G = {}

    def ein(name, shape, dt=F32):
        G[name] = P.dram(name, shape, dt, "ExternalInput")

    ein("x", [SEQ // 128, 128, D_MODEL])
    ein("pos", [128, SEQ], I32)
    ein("ident", [128, 128], BF16)
    ein("ones", [128, 128], BF16)
    ein("identf", [128, 128])
    ein("mask", [64, 512])
    ein("rmask", [128, 512])
    ein("gains", [DEPTH * 6, 128, D_MODEL])
    for l in range(DEPTH):
        for i in range(2):
            ein("wg%d%d" % (l, i), [NJ, 128, 8, 128])
            ein("wu%d%d" % (l, i), [NJ, 128, 8, 128])
            ein("wd%d%d" % (l, i), [NJ, 128, D_MODEL])
        ein("wout%d" % l, [8, 128, D_MODEL])
    for j in range(DEPTH // 2):
        ein("wlat%d" % j, [8, 128, 768])
        ein("whg%d" % j, [2, 8, 128, 1024])
        ein("wuq%d" % j, [2, 3, 128, 512])
        ein("wukv%d" % j, [2, 2, 128, 512])
        ein("colsE%d" % j, [2, 128, 16])
        ein("wodd%d" % j, [2, 8, 128, 2560])
        ein("colsO%d" % j, [2, 128, 16])
        ein("lamp%d" % j, [128, 256])
    for g in range(NGRP):
        G["xs%d" % g] = P.dram("xs%d" % g, [8, 128, D_MODEL], F32, "Internal")
    G["hT_s"] = P.dram("hT_s", [8, 128, SEQ], BF16, "Internal")
    G["oT_s"] = P.dram("oT_s", [8, 128, SEQ], BF16, "Internal")
    G["xo"] = P.dram("xo", [SEQ // 128, 128, D_MODEL], F32, "ExternalOutput")
    BB = [P.ps("BB%d" % i, [128, 1024]) for i in range(4)]
    B = [Tile("B%d" % i, BB[i // 2].base[:, (i % 2) * 512:(i % 2 + 1) * 512]) for i in range(8)]
    G["BB"] = BB
    for step in range(DEPTH + 1):
        emit_token_phase(P, B, G, step)
        if step < DEPTH:
            if step % 2 == 0:
                emit_even_mixer(P, B, G, step // 2)
            else:
                emit_odd_mixer(P, B, G, step)
    P.finalize()
    return nc


def rearr_ffn_w(W):
    return np.ascontiguousarray(W.reshape(8, 128, NJ, 128).transpose(2, 1, 0, 3))


def fused_common_inputs(inp):
    ones, identf, mask, rmask = const_tables()
    C = {"ident": np.eye(128, dtype=BF), "ones": ones, "identf": identf, "mask": mask, "rmask": rmask}
    g = inp["norm_g"].reshape(DEPTH * 6, 1, D_MODEL)
    C["gains"] = np.ascontiguousarray(np.broadcast_to(g, (DEPTH * 6, 128, D_MODEL)))
    for l in range(DEPTH):
        for i in range(2):
            C["wg%d%d" % (l, i)] = rearr_ffn_w(inp["ffn_w_gate"][l, i])
            C["wu%d%d" % (l, i)] = rearr_ffn_w(inp["ffn_w_up"][l, i])
            C["wd%d%d" % (l, i)] = np.ascontiguousarray(inp["ffn_w_down"][l, i].reshape(NJ, 128, D_MODEL))
        w_out = inp["ev_w_out"][l // 2] if l % 2 == 0 else inp["od_w_out"][l // 2]
        C["wout%d" % l] = kchunk(np.ascontiguousarray(w_out))
    for j in range(DEPTH // 2):
        e = [even_mixer_inputs(inp, j, r, None, None) for r in range(2)]
        C["wlat%d" % j] = e[0]["wlat"]
        for nm in ("whg", "wuq", "wukv"):
            C["%s%d" % (nm, j)] = np.ascontiguousarray(np.stack([e[0][nm], e[1][nm]], axis=0))
        C["colsE%d" % j] = np.ascontiguousarray(np.stack([e[0]["cols"], e[1]["cols"]], axis=0))
        o = [odd_mixer_inputs(inp, j, r, None, None) for r in range(2)]
        C["wodd%d" % j] = np.ascontiguousarray(np.stack([o[0]["w"], o[1]["w"]], axis=0))
        C["colsO%d" % j] = np.ascontiguousarray(np.stack([o[0]["cols"], o[1]["cols"]], axis=0))
        C["lamp%d" % j] = o[0]["lamp"]
    return C


_NC_CACHE = {}


def kernel(x, positions, norm_g, ffn_w_gate, ffn_w_up, ffn_w_down, ev_w_in, ev_g_q, ev_w_uq, ev_g_kv,
           ev_w_ukv, ev_lb_logits, ev_g_out, ev_w_out, od_w_in, od_lambda, od_g_head, od_w_out):
    inp = dict(x=x, positions=positions, norm_g=norm_g, ffn_w_gate=ffn_w_gate, ffn_w_up=ffn_w_up,
               ffn_w_down=ffn_w_down, ev_w_in=ev_w_in, ev_g_q=ev_g_q, ev_w_uq=ev_w_uq, ev_g_kv=ev_g_kv,
               ev_w_ukv=ev_w_ukv, ev_lb_logits=ev_lb_logits, ev_g_out=ev_g_out, ev_w_out=ev_w_out,
               od_w_in=od_w_in, od_lambda=od_lambda, od_g_head=od_g_head, od_w_out=od_w_out)
    inp = {k: np.asarray(v) for k, v in inp.items()}
    if "nc" not in _NC_CACHE:
        _NC_CACHE["nc"] = build_fused()
    nc = _NC_CACHE["nc"]
    C = fused_common_inputs(inp)
    cores = list(range(NCORES))
    in_maps = []
    for c in cores:
        b = c // 2
        m = dict(C)
        m["x"] = np.ascontiguousarray(inp["x"][b].astype(np.float32, copy=False)).reshape(SEQ // 128, 128, D_MODEL)
        m["pos"] = np.ascontiguousarray(np.broadcast_to(inp["positions"][b].astype(np.int32)[None, :], (128, SEQ)))
        in_maps.append(m)
    res = run_bass_kernel_spmd(nc, in_maps, core_ids=cores).results
    out = np.zeros((BATCH, SEQ, D_MODEL), dtype=np.float32)
    for b in range(BATCH):
        out[b] = np.asarray(res[2 * b]["xo"]).reshape(SEQ, D_MODEL)
    return out
```
